# Optimizing a Trainium2 kernel written in Bass

```python
import math
import jax, jax.numpy as jnp
from jax import lax
import numpy as np

D_MODEL = 1024
BATCH = 16
SEQ = 4096
DEPTH = 1
DEC_BATCH = 128
DEC_SEQ = 8
PAST_LEN = 8192
PAGE_SIZE = 128

GLA_HEADS = 4
GLA_DK = 64
GLA_DV = 128
GLA_QK = GLA_HEADS * GLA_DK
GLA_V = GLA_HEADS * GLA_DV
GLA_GATE_RANK = 16
GLA_GATE_NORM = 16.0
GLA_CHUNK = 64
DIL_HEADS = 8
DIL_HD = 64
DIL_W = DIL_HEADS * DIL_HD
DIL_PATTERNS = ((128, 1), (512, 4), (2048, 16))
DIL_WMAX = 2048
Q_BLOCK = 128
D_MIX = GLA_V + DIL_W
SPLIT_POINTS = (GLA_QK, 2 * GLA_QK, 2 * GLA_QK + GLA_V, 2 * GLA_QK + GLA_V + GLA_GATE_RANK,
                2 * GLA_QK + 2 * GLA_V + GLA_GATE_RANK, 2 * GLA_QK + 2 * GLA_V + GLA_GATE_RANK + DIL_W,
                2 * GLA_QK + 2 * GLA_V + GLA_GATE_RANK + 2 * DIL_W)
D_IN = 2 * GLA_QK + 2 * GLA_V + GLA_GATE_RANK + 3 * DIL_W
N_MEM = 256
MEM_HEADS = 4
MEM_HD = D_MODEL // MEM_HEADS
N_GROUPS = 4
EXP_PER_GROUP = 8
N_EXPERTS = N_GROUPS * EXP_PER_GROUP
TOP_K = 2
D_EXPERT = 512
MOE_BLOCK = 128
EPS = 1e-6

kernel_name = 'hybrid_gla_dilated_hmoe_decode_step'


def rmsnorm(x, g):
    xf = x.astype(jnp.float32)
    y = xf * lax.rsqrt(jnp.mean(xf * xf, axis=-1, keepdims=True) + EPS)
    return (y * g.astype(jnp.float32)).astype(x.dtype)


def alibi_slopes():
    return jnp.asarray([2.0 ** (-8.0 * (h + 1) / DIL_HEADS) for h in range(DIL_HEADS)], dtype=jnp.float32)


def gla_chunked(q, k, v, log_a, s0):
    B, T, H, DK = q.shape
    DV = v.shape[-1]
    C = math.gcd(T, GLA_CHUNK)
    n = T // C

    def to_chunks(a):
        return a.reshape(B, n, C, H, a.shape[-1]).transpose(1, 0, 3, 2, 4).astype(jnp.float32)

    qc, kc, vc, gc = to_chunks(q), to_chunks(k), to_chunks(v), to_chunks(log_a)
    causal = jnp.tril(jnp.ones((C, C), dtype=bool))

    def step(S, inp):
        qi, ki, vi, gi = inp
        b = jnp.cumsum(gi, axis=2)
        diff = jnp.where(causal[:, :, None], b[:, :, :, None, :] - b[:, :, None, :, :], -jnp.inf)
        A = jnp.einsum('bhik,bhjk,bhijk->bhij', qi, ki, jnp.exp(diff))
        o = jnp.einsum('bhij,bhjv->bhiv', A, vi) + jnp.einsum('bhik,bhkv->bhiv', qi * jnp.exp(b), S)
        bC = b[:, :, -1, :]
        S_new = jnp.exp(bC)[..., None] * S + jnp.einsum('bhjk,bhjv->bhkv', ki * jnp.exp(bC[:, :, None, :] - b), vi)
        return S_new, o

    S_fin, oc = lax.scan(step, s0.astype(jnp.float32), (qc, kc, vc, gc))
    o = oc.transpose(1, 0, 3, 2, 4).reshape(B, T, H, DV)
    return o, S_fin


def dilated_block(q, k_ext, v_ext, qidx):
    qf = q.astype(jnp.float32) * (DIL_HD ** -0.5)
    slopes = alibi_slopes()
    lses, outs = [], []
    for (w, d) in DIL_PATTERNS:
        nk = w // d + 1
        dist = jnp.arange(nk, dtype=jnp.int32) * d
        idx = qidx[:, None] - dist[None, :]
        valid = idx >= 0
        idx = jnp.maximum(idx, 0)
        kg = k_ext[:, idx].astype(jnp.float32)
        vg = v_ext[:, idx].astype(jnp.float32)
        s = jnp.einsum('bqhd,bqkhd->bqhk', qf, kg) - slopes[:, None] * dist.astype(jnp.float32)
        s = jnp.where(valid[None, :, None, :], s, -jnp.inf)
        lse = jax.nn.logsumexp(s, axis=-1)
        p = jnp.exp(s - lse[..., None])
        outs.append(jnp.einsum('bqhk,bqkhd->bqhd', p, vg))
        lses.append(lse)
    wts = jax.nn.softmax(jnp.stack(lses, axis=0), axis=0)
    return jnp.sum(wts[..., None] * jnp.stack(outs, axis=0), axis=0)


def dilated_attention(q, k_ext, v_ext, offset):
    B, T, H, Dh = q.shape
    blk = math.gcd(T, Q_BLOCK)
    nb = T // blk
    qb = q.reshape(B, nb, blk, H, Dh).transpose(1, 0, 2, 3, 4)
    qidx = (offset + jnp.arange(T, dtype=jnp.int32)).reshape(nb, blk)
    ob = lax.map(lambda a: dilated_block(a[0], k_ext, v_ext, a[1]), (qb, qidx))
    return ob.transpose(1, 0, 2, 3, 4).reshape(B, T, H, Dh)


def hybrid_mixer(n, s0, k_past, v_past, w_in, w_gk2, b_gk, g_gla, w_out):
    B, T, _ = n.shape
    proj = n @ w_in
    q_a, k_a, v_a, gk_lr, r_a, q_b, k_b, v_b = jnp.split(proj, SPLIT_POINTS, axis=-1)
    q_a = q_a.reshape(B, T, GLA_HEADS, GLA_DK) * (GLA_DK ** -0.5)
    k_a = k_a.reshape(B, T, GLA_HEADS, GLA_DK)
    v_a = v_a.reshape(B, T, GLA_HEADS, GLA_DV)
    gk = (gk_lr @ w_gk2 + b_gk).astype(jnp.float32)
    log_a = (jax.nn.log_sigmoid(gk) / GLA_GATE_NORM).reshape(B, T, GLA_HEADS, GLA_DK)
    o_a, s_new = gla_chunked(q_a, k_a, v_a, log_a, s0)
    o_a = rmsnorm(o_a, g_gla) * jax.nn.silu(r_a.astype(jnp.float32)).reshape(B, T, GLA_HEADS, GLA_DV)
    o_a = o_a.reshape(B, T, GLA_V).astype(n.dtype)
    q_b = q_b.reshape(B, T, DIL_HEADS, DIL_HD)
    k_b = k_b.reshape(B, T, DIL_HEADS, DIL_HD)
    v_b = v_b.reshape(B, T, DIL_HEADS, DIL_HD)
    k_ext = jnp.concatenate([k_past.astype(k_b.dtype), k_b], axis=1)
    v_ext = jnp.concatenate([v_past.astype(v_b.dtype), v_b], axis=1)
    o_b = dilated_attention(q_b, k_ext, v_ext, k_past.shape[1]).reshape(B, T, DIL_W).astype(n.dtype)
    y = jnp.concatenate([o_a, o_b], axis=-1) @ w_out
    return y, s_new.astype(n.dtype), k_b, v_b


def memory_kv(mem, g_mem, w_mk, w_mv):
    B = mem.shape[0]
    m = rmsnorm(mem, g_mem)
    return ((m @ w_mk).reshape(B, N_MEM, MEM_HEADS, MEM_HD), (m @ w_mv).reshape(B, N_MEM, MEM_HEADS, MEM_HD))


def cross_attend(h, mk, mv, w_cq, w_co):
    B, T, D = h.shape
    q = (h @ w_cq).reshape(B, T, MEM_HEADS, MEM_HD).astype(jnp.float32) * (MEM_HD ** -0.5)
    s = jnp.einsum('bthd,bshd->bhts', q, mk.astype(jnp.float32))
    p = jax.nn.softmax(s, axis=-1)
    o = jnp.einsum('bhts,bshd->bthd', p, mv.astype(jnp.float32)).reshape(B, T, D).astype(h.dtype)
    return o @ w_co


def hier_moe(x, w_gr, b_gr, w_er, b_er, w_e1, w_e3, w_e2):
    N, D = x.shape
    xf = x.astype(jnp.float32)
    g_logits = xf @ w_gr.astype(jnp.float32) + b_gr.astype(jnp.float32)
    g_idx = jnp.argmax(g_logits, axis=-1)
    p_g = jnp.take_along_axis(jax.nn.softmax(g_logits, axis=-1), g_idx[:, None], axis=1)[:, 0]
    e_logits = (xf @ w_er.astype(jnp.float32) + b_er.astype(jnp.float32)).reshape(N, N_GROUPS, EXP_PER_GROUP)
    sel = jnp.take_along_axis(e_logits, g_idx[:, None, None], axis=1)[:, 0]
    top_l, top_i = lax.top_k(sel, TOP_K)
    gate = p_g[:, None] * jax.nn.softmax(top_l, axis=-1)
    expert = (g_idx[:, None] * EXP_PER_GROUP + top_i).astype(jnp.int32)
    M = N * TOP_K
    flat_e = expert.reshape(M)
    flat_tok = jnp.repeat(jnp.arange(N, dtype=jnp.int32), TOP_K)
    flat_w = gate.reshape(M)
    order = jnp.argsort(flat_e)
    se = flat_e[order]
    counts = jnp.bincount(flat_e, length=N_EXPERTS).astype(jnp.int32)
    padded = (counts + MOE_BLOCK - 1) // MOE_BLOCK * MOE_BLOCK
    pad_end = jnp.cumsum(padded)
    pad_start = pad_end - padded
    start = jnp.cumsum(counts) - counts
    dest = pad_start[se] + jnp.arange(M, dtype=jnp.int32) - start[se]
    nb = -(-M // MOE_BLOCK) + N_EXPERTS
    R = nb * MOE_BLOCK
    row_tok = jnp.full((R,), N, dtype=jnp.int32).at[dest].set(flat_tok[order])
    row_w = jnp.zeros((R,), dtype=jnp.float32).at[dest].set(flat_w[order])
    block_e = jnp.minimum(jnp.searchsorted(pad_end, jnp.arange(nb, dtype=jnp.int32) * MOE_BLOCK, side='right'),
                          N_EXPERTS - 1)
    xs = jnp.concatenate([x, jnp.zeros((1, D), x.dtype)], axis=0)[row_tok].reshape(nb, MOE_BLOCK, D)

    def expert_block(a):
        xb, e = a
        return (jax.nn.silu(xb @ w_e1[e]) * (xb @ w_e3[e])) @ w_e2[e]

    yb = lax.map(expert_block, (xs, block_e)).reshape(R, D)
    y = jax.ops.segment_sum(yb.astype(jnp.float32) * row_w[:, None], row_tok, num_segments=N + 1)[:N]
    return y.astype(x.dtype)


def layer(x, mk, mv, s0, k_past, v_past, g1, w_in, w_gk2, b_gk, g_gla, w_out, g2, w_cq, w_co,
          g3, w_gr, b_gr, w_er, b_er, w_e1, w_e3, w_e2):
    B, T, D = x.shape
    mix, s_new, k_new, v_new = hybrid_mixer(rmsnorm(x, g1), s0, k_past, v_past, w_in, w_gk2, b_gk, g_gla, w_out)
    h = x + mix
    h = h + cross_attend(rmsnorm(h, g2), mk, mv, w_cq, w_co)
    h = h + hier_moe(rmsnorm(h, g3).reshape(B * T, D), w_gr, b_gr, w_er, b_er, w_e1, w_e3, w_e2).reshape(B, T, D)
    return h, s_new, k_new, v_new


def setup_inputs(seed: int = 0) -> dict:
    key = jax.random.key(seed)
    ks = jax.random.split(key, 32)

    def nrm(i, shape, scale):
        return jax.random.normal(ks[i], shape, jnp.float32) * scale

    L = DEPTH
    buf = min(DIL_WMAX, PAST_LEN)
    return {
        'x_prompt': nrm(0, (BATCH, SEQ, D_MODEL), 1.0),
        'x_sample': nrm(1, (DEC_BATCH, DEC_SEQ, D_MODEL), 1.0),
        'cache_swa_k': nrm(2, (L, DEC_BATCH, buf, DIL_HEADS, DIL_HD), 1.0),
        'cache_swa_v': nrm(3, (L, DEC_BATCH, buf, DIL_HEADS, DIL_HD), 1.0),
        'state_gla': nrm(4, (L, DEC_BATCH, GLA_HEADS, GLA_DK, GLA_DV), 0.5),
        'cache_mem_k': nrm(5, (L, DEC_BATCH, N_MEM, MEM_HEADS, MEM_HD), 1.0),
        'cache_mem_v': nrm(6, (L, DEC_BATCH, N_MEM, MEM_HEADS, MEM_HD), 1.0),
        'mem_prompt': nrm(7, (BATCH, N_MEM, D_MODEL), 1.0),
        'g_norm1': 1.0 + nrm(8, (L, D_MODEL), 0.02),
        'w_in': nrm(9, (L, D_MODEL, D_IN), D_MODEL ** -0.5),
        'w_gk2': nrm(10, (L, GLA_GATE_RANK, GLA_QK), GLA_GATE_RANK ** -0.5),
        'b_gk': nrm(11, (L, GLA_QK), 0.1),
        'g_gla_out': 1.0 + nrm(12, (L, GLA_DV), 0.02),
        'w_out': nrm(13, (L, D_MIX, D_MODEL), D_MIX ** -0.5),
        'g_norm2': 1.0 + nrm(14, (L, D_MODEL), 0.02),
        'g_mem': 1.0 + nrm(15, (L, D_MODEL), 0.02),
        'w_cq': nrm(16, (L, D_MODEL, D_MODEL), D_MODEL ** -0.5),
        'w_mk': nrm(17, (L, D_MODEL, D_MODEL), D_MODEL ** -0.5),
        'w_mv': nrm(18, (L, D_MODEL, D_MODEL), D_MODEL ** -0.5),
        'w_co': nrm(19, (L, D_MODEL, D_MODEL), D_MODEL ** -0.5),
        'g_norm3': 1.0 + nrm(20, (L, D_MODEL), 0.02),
        'w_gr': nrm(21, (L, D_MODEL, N_GROUPS), D_MODEL ** -0.5),
        'b_gr': nrm(22, (L, N_GROUPS), 0.01),
        'w_er': nrm(23, (L, D_MODEL, N_EXPERTS), D_MODEL ** -0.5),
        'b_er': nrm(24, (L, N_EXPERTS), 0.01),
        'w_e1': nrm(25, (L, N_EXPERTS, D_MODEL, D_EXPERT), D_MODEL ** -0.5),
        'w_e3': nrm(26, (L, N_EXPERTS, D_MODEL, D_EXPERT), D_MODEL ** -0.5),
        'w_e2': nrm(27, (L, N_EXPERTS, D_EXPERT, D_MODEL), D_EXPERT ** -0.5),
        'g_final': 1.0 + nrm(28, (D_MODEL,), 0.02),
    }


def reference(x_prompt, x_sample, cache_swa_k, cache_swa_v, state_gla, cache_mem_k, cache_mem_v, mem_prompt,
              g_norm1, w_in, w_gk2, b_gk, g_gla_out, w_out, g_norm2, g_mem, w_cq, w_mk, w_mv, w_co,
              g_norm3, w_gr, b_gr, w_er, b_er, w_e1, w_e3, w_e2, g_final):
    hp, hs = x_prompt, x_sample
    B, T, _ = x_prompt.shape
    keep = min(DIL_WMAX, T)
    kp_l, vp_l, sp_l, mk_l, mv_l, ks_l, vs_l, ss_l = [], [], [], [], [], [], [], []
    for l in range(DEPTH):
        lw = (g_norm1[l], w_in[l], w_gk2[l], b_gk[l], g_gla_out[l], w_out[l], g_norm2[l], w_cq[l], w_co[l],
              g_norm3[l], w_gr[l], b_gr[l], w_er[l], b_er[l], w_e1[l], w_e3[l], w_e2[l])
        mk, mv = memory_kv(mem_prompt, g_mem[l], w_mk[l], w_mv[l])
        s0 = jnp.zeros((B, GLA_HEADS, GLA_DK, GLA_DV), hp.dtype)
        empty = jnp.zeros((B, 0, DIL_HEADS, DIL_HD), hp.dtype)
        hp, sp, kp, vp = layer(hp, mk, mv, s0, empty, empty, *lw)
        kp_l.append(kp[:, T - keep:])
        vp_l.append(vp[:, T - keep:])
        sp_l.append(sp)
        mk_l.append(mk)
        mv_l.append(mv)
        hs, ss, kn, vn = layer(hs, cache_mem_k[l], cache_mem_v[l], state_gla[l], cache_swa_k[l], cache_swa_v[l], *lw)
        ks_l.append(kn)
        vs_l.append(vn)
        ss_l.append(ss)
    y_prompt = rmsnorm(hp, g_final)
    y_sample = rmsnorm(hs, g_final)
    return (y_prompt, y_sample, jnp.stack(kp_l), jnp.stack(vp_l), jnp.stack(sp_l), jnp.stack(mk_l), jnp.stack(mv_l),
            jnp.stack(ks_l), jnp.stack(vs_l), jnp.stack(ss_l))
```

```python
from contextlib import ExitStack
import numpy as np
import concourse.bass as bass
import concourse.mybir as mybir
from concourse.bass_utils import run_bass_kernel_spmd

F32 = mybir.dt.float32
BF16 = mybir.dt.bfloat16
I32 = mybir.dt.int32
U32 = mybir.dt.uint32
AF = mybir.ActivationFunctionType
ALU = mybir.AluOpType
AX = mybir.AxisListType

ENGS = ("pe", "act", "dve", "pool", "sp")
SEM_LIMIT = 30000
DMA_RING = 8


class Tok:
    __slots__ = ("w", "rs", "name", "excl")

    def __init__(self, name="", excl=False):
        self.w = None
        self.rs = []
        self.name = name
        self.excl = excl


class Op:
    __slots__ = ("eng", "fn", "dma", "deps", "signal", "sem", "val", "gate")

    def __init__(self, eng, fn, dma):
        self.eng = eng
        self.fn = fn
        self.dma = dma
        self.deps = []
        self.signal = dma
        self.sem = None
        self.val = None
        self.gate = None


class Sched:
    def __init__(self, nc):
        self.nc = nc
        self.ops = {e: [] for e in ENGS}
        self.dma_ops = {e: [] for e in ENGS}
        self.out_dmas = []
        import os
        self.cut = int(os.environ.get("KCUT", "0")) or None
        self.total = 0

    def add(self, eng, fn, reads=(), writes=(), dma=False, out=False):
        self.total += 1
        if self.cut is not None and self.total > self.cut:
            return None
        op = Op(eng, fn, dma)
        deps = []
        ex = [t for t in reads if t.excl]
        if ex:
            reads = [t for t in reads if not t.excl]
            writes = list(writes) + [t for t in ex if t not in writes]
        for t in reads:
            if t.w is not None:
                deps.append(t.w)
        for t in writes:
            deps.extend(t.rs)
            if t.w is not None:
                deps.append(t.w)
        seen = set()
        flat = []
        for d in deps:
            if d.fn is None:
                flat.extend(d.deps)
            else:
                flat.append(d)
        for d in flat:
            if id(d) in seen:
                continue
            seen.add(id(d))
            if fn is None or d.dma or d.eng != eng or eng != "pe":
                op.deps.append(d)
                d.signal = True
        for t in reads:
            t.rs.append(op)
        for t in writes:
            t.w = op
            t.rs = []
        if dma:
            lst = self.dma_ops[eng]
            if len(lst) >= DMA_RING:
                op.gate = lst[len(lst) - DMA_RING]
            lst.append(op)
            if out:
                self.out_dmas.append(op)
        if fn is not None:
            self.ops[eng].append(op)
        return op

    def finish(self):
        op = Op("sp", None, False)
        op.deps = list(self.out_dmas)
        self.ops["sp"].append(op)

    def emit(self):
        nc = self.nc
        with ExitStack() as st:
            nsig = {e: sum(1 for o in self.ops[e] if o.signal and not o.dma) for e in ENGS}
            esems = {}
            for e in ENGS:
                k = nsig[e] // SEM_LIMIT + 1
                esems[e] = [st.enter_context(nc.semaphore(f"s_{e}_{i}")) for i in range(k)]
            dsems = {}
            for e in ENGS:
                if self.dma_ops[e]:
                    dsems[e] = [st.enter_context(nc.semaphore(f"d_{e}_{i}")) for i in range(DMA_RING)]
            for e in ENGS:
                c = 0
                for o in self.ops[e]:
                    if o.dma:
                        continue
                    if o.signal:
                        o.sem = esems[e][c // SEM_LIMIT]
                        o.val = c % SEM_LIMIT + 1
                        c += 1
                for n, o in enumerate(self.dma_ops[e]):
                    o.sem = dsems[e][n % DMA_RING]
                    o.val = 16 * (n // DMA_RING + 1)
            block = st.enter_context(nc.Block())

            def run(e, name):
                waited = {}
                for o in self.ops[name]:
                    ds = list(o.deps)
                    if o.gate is not None:
                        ds.append(o.gate)
                    for d in ds:
                        k = id(d.sem)
                        if waited.get(k, 0) >= d.val:
                            continue
                        waited[k] = d.val
                        e.wait_ge(d.sem, d.val)
                    if o.fn is None:
                        continue
                    ins = o.fn(e)
                    if o.signal:
                        ins.then_inc(o.sem, 16 if o.dma else 1)

            @block.tensor
            def _(e):
                run(e, "pe")

            @block.scalar
            def _(e):
                run(e, "act")

            @block.vector
            def _(e):
                run(e, "dve")

            @block.gpsimd
            def _(e):
                run(e, "pool")

            @block.sync
            def _(e):
                run(e, "sp")


class Buf:
    def __init__(self, t, name=""):
        self.t = t
        self.k = Tok(name)


class Ctx:
    def __init__(self, nc):
        self.nc = nc
        self.st = ExitStack()
        self.S = Sched(nc)
        self.n = 0

    def sb(self, shape, dt, name=None):
        self.n += 1
        name = name or f"sb{self.n}"
        return Buf(self.st.enter_context(self.nc.sbuf_tensor(name, list(shape), dt)), name)

    def ps(self, shape, dt, name=None):
        self.n += 1
        name = name or f"ps{self.n}"
        b = Buf(self.st.enter_context(self.nc.psum_tensor(name, list(shape), dt)), name)
        b.k.excl = True
        return b

    def dma(self, q, out, in_, reads=(), writes=(), out_dram=False, **kw):
        return self.S.add(q, lambda e: e.dma_start(out=out, in_=in_, **kw), reads, writes, dma=True, out=out_dram)

    def mm(self, out, lhsT, rhs, start, stop, reads, writes):
        return self.S.add("pe", lambda e: e.matmul(out, lhsT=lhsT, rhs=rhs, start=start, stop=stop), reads, writes)

    def tr(self, out, in_, ident, reads, writes):
        return self.S.add("pe", lambda e: e.transpose(out, in_, ident), reads, writes)

    def act(self, out, in_, func, reads, writes, **kw):
        return self.S.add("act", lambda e: e.activation(out=out, in_=in_, func=func, **kw), reads, writes)

    def copy(self, eng, out, in_, reads, writes):
        if eng == "act":
            return self.S.add("act", lambda e: e.copy(out=out, in_=in_), reads, writes)
        return self.S.add(eng, lambda e: e.tensor_copy(out=out, in_=in_), reads, writes)

    def tt(self, eng, out, in0, in1, op, reads, writes):
        return self.S.add(eng, lambda e: e.tensor_tensor(out=out, in0=in0, in1=in1, op=op), reads, writes)

    def ts(self, eng, out, in0, s1, s2, op0, op1, reads, writes, **kw):
        if s2 is None:
            return self.S.add(eng, lambda e: e.tensor_scalar(out=out, in0=in0, scalar1=s1, scalar2=None, op0=op0, **kw), reads, writes)
        return self.S.add(eng, lambda e: e.tensor_scalar(out=out, in0=in0, scalar1=s1, scalar2=s2, op0=op0, op1=op1, **kw), reads, writes)

    def stt(self, eng, out, in0, scalar, in1, op0, op1, reads, writes):
        return self.S.add(eng, lambda e: e.scalar_tensor_tensor(out=out, in0=in0, scalar=scalar, in1=in1, op0=op0, op1=op1), reads, writes)

    def close(self):
        self.S.finish()
        self.S.emit()
        self.st.close()


D = 1024
DIN = 3088
NT_FULL = 32
NSEQ = 2
NSS = 16
NR = 18
EPS = 1e-6
C_QA, C_KA, C_VA, C_GK, C_RA, C_QB, C_KB, C_VB = 0, 256, 512, 1024, 1040, 1552, 2064, 2576


def host_consts():
    t = np.arange(128)
    ch = t // 64
    same = ch[:, None] == ch[None, :]
    ucum = np.where(same & (t[:, None] <= t[None, :]), -1.0 / 16, 0.0).astype(np.float32)
    lrev = np.where(same & (t[:, None] > t[None, :]), -1.0 / 16, 0.0).astype(np.float32)
    amask = np.where(same & (t[:, None] <= t[None, :]), 1.0, 0.0).astype(np.float32)
    dm = np.zeros((17, 128, 128), np.float32)
    for db in range(17):
        dist = db * 128 + t[None, :] - t[:, None]
        m = ((dist >= 0) & (dist <= 128)).astype(np.float32)
        m += ((dist >= 0) & (dist <= 512) & (dist % 4 == 0))
        m += ((dist >= 0) & (dist <= 2048) & (dist % 16 == 0))
        dm[db] = m
    slopes = np.array([2.0 ** (-8.0 * (h + 1) / 8) for h in range(8)], np.float64)
    ab = np.zeros((128, 8, 17), np.float32)
    for h in range(8):
        for db in range(17):
            ab[:, h, db] = slopes[h] * (t - 64 - 128 * db)
    dmf = np.zeros((128, 8, 17, 128), np.float32)
    for db in range(17):
        dist = db * 128 + t[None, :] - t[:, None]
        for h in range(8):
            dmf[:, h, 16 - db, :] = dm[db] * np.exp(-slopes[h] * np.maximum(dist, 0))
    return {
        "c_ident": np.eye(128, dtype=np.float32),
        "c_ucum": ucum, "c_lrev": lrev, "c_amask": amask,
        "c_dmf": dmf.reshape(128, 8 * 17 * 128),
    }


def build(NT=NT_FULL, nseq=NSEQ, debug=False, phases="AB"):
    nc = bass.Bass("TRN2", target_bir_lowering=False)

    def din(name, shape, dt=F32):
        return nc.dram_tensor(name, list(shape), dt, kind="ExternalInput").ap()

    def dout(name, shape, dt=F32):
        return nc.dram_tensor(name, list(shape), dt, kind="ExternalOutput").ap()

    T = NT * 128
    c_ident = din("c_ident", [128, 128])
    samp = "S" in phases
    if "A" in phases:
        h1_d = dout("h1", [nseq, T, D]) if debug else nc.dram_tensor("h1", [nseq, T, D], F32).ap()
        phase_a(nc, din, dout, NT, nseq, h1_d, c_ident)
    else:
        h1_d = din("h1", [nseq, T, D])
    h1s_d = None
    if samp:
        h1s_d = dout("h1s", [128, D]) if debug else nc.dram_tensor("h1s", [128, D], F32).ap()
        phase_as(nc, din, dout, h1s_d, c_ident)
    NTOT = nseq * NT + (1 if samp else 0)
    if "B" in phases:
        h2_d, xn_d, route_d, cnt_d = phase_b(nc, din, dout, NT, nseq, h1_d, c_ident, debug, h1s_d)
    elif "C" in phases:
        h2_d = din("h2", [NTOT * 128, D])
        xn_d = din("xn", [NTOT * 128, D], BF16)
        route_d = din("route", [128, NTOT, 8])
        cnt_d = din("cnt", [128, 32])
    if "C" in phases:
        y_d = dout("y", [NTOT * 128, D])
        phase_c(nc, din, dout, NTOT, h2_d, xn_d, route_d, cnt_d, c_ident, y_d)
    return nc


def phase_a(nc, din, dout, NT, nseq, h1_d, c_ident):
    T = NT * 128
    KEEP = min(2048, T)
    KT0 = NT - KEEP // 128
    xp = din("xp", [nseq, T, D])
    g1 = nc_input(nc, din, "g_norm1", [1, D])
    w_in = nc_input(nc, din, "w_in", [D, DIN])
    w_gk2 = nc_input(nc, din, "w_gk2", [16, 256])
    b_gk = nc_input(nc, din, "b_gk", [1, 256])
    g_gla = nc_input(nc, din, "g_gla_out", [1, 128])
    w_out = nc_input(nc, din, "w_out", [D, D])
    c_ucum = din("c_ucum", [128, 128])
    c_lrev = din("c_lrev", [128, 128])
    c_amask = din("c_amask", [128, 128])
    c_dmf = din("c_dmf", [128, 8 * 17 * 128])

    o_swak = dout("o_swak", [nseq, KEEP, 512])
    o_swav = dout("o_swav", [nseq, KEEP, 512])
    o_glas = dout("o_glas", [nseq, 4, 64, 128])
    cx = Ctx(nc)
    S = cx.S
    ident_b = cx.sb([128, 128], BF16, "ident_b")
    ucum = cx.sb([128, 128], F32, "ucum")
    lrev = cx.sb([128, 128], F32, "lrev")
    amask = cx.sb([128, 128], F32, "amask")
    dmf = cx.sb([128, 8, 17, 128], BF16, "dmf")
    g1b = cx.sb([128, D], F32, "g1b")
    gglab = cx.sb([128, 128], F32, "gglab")
    win = cx.sb([128, 8, DIN], BF16, "win")
    wout = cx.sb([128, 8, D], BF16, "wout")
    wgk = cx.sb([32, 256], BF16, "wgk")
    kTr = cx.sb([128, 4, NR * 128], BF16, "kTr")
    Vr = cx.sb([128, NR, 8, 65], BF16, "Vr")
    gklrT = cx.sb([32, 128], BF16, "gklrT")
    Sst = cx.sb([128, 2, 128], F32, "Sst")
    Sb = [cx.sb([128, 2, 128], BF16, f"Sb{i}") for i in range(4)]

    cx.dma("pool", ident_b.t[:], c_ident, writes=[ident_b.k])
    cx.dma("sp", ucum.t[:], c_ucum, writes=[ucum.k])
    cx.dma("sp", lrev.t[:], c_lrev, writes=[lrev.k])
    cx.dma("sp", amask.t[:], c_amask, writes=[amask.k])
    c_dmf_v = c_dmf.rearrange("p (h r q) -> p h r q", h=8, r=17)
    for h in range(8):
        cx.dma("pool", dmf.t[:, h, 0:9, :], c_dmf_v[:, h, 0:9, :], writes=[dmf.k])
        cx.dma("pool", dmf.t[:, h, 9:17, :], c_dmf_v[:, h, 9:17, :], writes=[dmf.k])
    cx.dma("sp", g1b.t[:], g1.partition_broadcast(128), writes=[g1b.k])
    cx.dma("sp", gglab.t[:], g_gla.partition_broadcast(128), writes=[gglab.k])
    w_in_v = w_in.rearrange("(k p) f -> p k f", p=128)
    for k in range(8):
        for hh in range(2):
            c0 = hh * 1544
            cx.dma("pool", win.t[:, k, c0:c0 + 1544], w_in_v[:, k, c0:c0 + 1544], writes=[win.k])
    w_out_v = w_out.rearrange("(k p) f -> p k f", p=128)
    for k in range(8):
        cx.dma("pool", wout.t[:, k, :], w_out_v[:, k, :], writes=[wout.k])
    cx.dma("pool", wgk.t[0:16, :], w_gk2, writes=[wgk.k])
    cx.dma("pool", wgk.t[16:17, :], b_gk, writes=[wgk.k])
    S.add("pool", lambda e: e.memset(gklrT.t[:], 1.0), (), [gklrT.k])
    S.add("pool", lambda e: e.memset(Vr.t[:], 1.0), (), [Vr.k])
    S.add("pool", lambda e: e.memset(kTr.t[:], 0.0), (), [kTr.k])

    kT_k = [Tok(f"kT{i}") for i in range(NR)]
    V_k = [Tok(f"V{i}") for i in range(NR)]
    psT = cx.ps([128, 1024], BF16, "psT")
    psR = [cx.ps([128, 512], F32, f"psR{i}") for i in range(2)]
    psSb = [cx.ps([128, 512], F32, f"psS{i}") for i in range(2)]
    psD = [cx.ps([128, 512], F32, f"psD{i}") for i in range(2)]
    psGO = cx.ps([128, 4, 128], F32, "psGO")
    rr = [0]

    def next_ps():
        b = psR[rr[0] % 2]
        rr[0] += 1
        return b

    def rot(n, shape, dt, name):
        return [cx.sb(shape, dt, f"{name}{i}") for i in range(n)]

    x_t = rot(2, [128, D], F32, "x")
    junk = cx.sb([128, D], BF16, "junk")
    st_t = rot(2, [128, 8], F32, "stat")
    n_t = rot(2, [128, D], BF16, "n")
    nT_t = rot(2, [128, 8, 128], BF16, "nT")
    qkTa = rot(2, [128, 4, 128], BF16, "qkTa")
    qTb = rot(2, [128, 4, 2, 128], BF16, "qTb")
    va_t = rot(2, [128, 512], BF16, "va")
    ka_t = rot(2, [128, 256], F32, "ka")
    sr_t = rot(2, [128, 512], BF16, "sr")
    kbf = rot(1, [128, 512], F32, "kbf") * 2
    vbf = rot(1, [128, 512], F32, "vbf") * 2
    e1_t = rot(1, [128, 256], F32, "e1") * 2
    g_t = rot(2, [128, 256], F32, "g")
    ebT = rot(2, [128, 2, 128], F32, "ebT")
    enbT = rot(2, [128, 2, 128], F32, "enbT")
    ed_t = rot(2, [128, 256], F32, "ed")
    qtT = rot(2, [128, 2, 2, 128], BF16, "qtT")
    ktT = rot(2, [128, 2, 128], BF16, "ktT")
    khat = rot(2, [128, 2, 256], BF16, "khat")
    Am = rot(4, [128, 128], BF16, "Am")
    gst = rot(2, [128, 8], F32, "gst")
    gtmp = rot(2, [128, 128], F32, "gtmp")
    ocat = rot(2, [128, D], BF16, "ocat")
    pe_t = rot(2, [128, 512], BF16, "pe")
    pm_t = rot(2, [128, 512], BF16, "pm")
    rden = rot(4, [128, 1], F32, "rden")
    oT_t = rot(2, [128, 8, 128], BF16, "oT")
    cnt = {"s": 0, "a": 0, "p": 0, "r": 0}
    for b_ in qTb + qtT + khat:
        S.add("pool", lambda e, b_=b_: e.memset(b_.t[:], 0.0), (), [b_.k])

    def rmsnorm_to_bf16(xb, gb, nb, stb):
        cx.act(junk.t[:], xb.t[:], AF.Square, [xb.k], [junk.k, stb.k], accum_out=stb.t[:, 0:1])
        cx.act(stb.t[:, 1:2], stb.t[:, 0:1], AF.Ln, [stb.k], [stb.k], scale=1.0 / D, bias=EPS)
        cx.act(stb.t[:, 2:3], stb.t[:, 1:2], AF.Exp, [stb.k], [stb.k], scale=-0.5)
        cx.stt("dve", nb.t[:], xb.t[:], stb.t[:, 2:3], gb.t[:], ALU.mult, ALU.mult, [xb.k, stb.k, gb.k], [nb.k])

    def transpose8(src, dst):
        for k in range(8):
            cx.tr(psT.t[:, k * 128:(k + 1) * 128], src.t[:, k * 128:(k + 1) * 128], ident_b.t[:], [src.k, ident_b.k], [psT.k])
        cx.copy("act", dst.t[:].rearrange("p k t -> p (k t)"), psT.t[:], [psT.k], [dst.k])

    def proj_fm(nT, col0, nchunk, ps, m=128):
        for j in range(nchunk):
            for k in range(8):
                cx.mm(ps.t[0:m, j * 128:(j + 1) * 128], win.t[:, k, col0 + j * 128: col0 + j * 128 + m], nT.t[:, k, :],
                      k == 0, k == 7, [win.k, nT.k], [ps.k])

    def proj_tm(nT, col0, ncol, ps):
        for k in range(8):
            cx.mm(ps.t[:, 0:ncol], nT.t[:, k, :], win.t[:, k, col0:col0 + ncol], k == 0, k == 7, [win.k, nT.k], [ps.k])

    for s in range(nseq):
        S.add("dve", lambda e: e.memset(Sst.t[:], 0.0), (), [Sst.k])
        sbi = [0]
        S.add("pool", lambda e: e.memset(Sb[0].t[:], 0.0), (), [Sb[0].k])
        for qb in range(NT):
            i2 = qb % 2
            xb, nb, nT, stb = x_t[i2], n_t[i2], nT_t[i2], st_t[i2]
            cx.dma("sp", xb.t[:], xp[s, qb * 128:(qb + 1) * 128, :], writes=[xb.k])
            rmsnorm_to_bf16(xb, g1b, nb, stb)
            transpose8(nb, nT)
            slot = qb % NR
            ps = next_ps()
            proj_fm(nT, C_QA, 4, ps)
            cx.copy("act", qkTa[i2].t[:].rearrange("p k t -> p (k t)"), ps.t[:], [ps.k], [qkTa[i2].k])
            ps = next_ps()
            proj_fm(nT, C_QB, 4, ps)
            psv = ps.t[:].rearrange("p (k t) -> p k t", k=4)
            cx.copy("act", qTb[i2].t[0:64, :, 0, :], psv[0:64], [ps.k], [qTb[i2].k])
            cx.copy("act", qTb[i2].t[64:128, :, 1, :], psv[64:128], [ps.k], [qTb[i2].k])
            ps = next_ps()
            proj_fm(nT, C_KB, 4, ps)
            cx.copy("dve", kTr.t[:, :, slot * 128:(slot + 1) * 128], ps.t[:].rearrange("p (k t) -> p k t", k=4), [ps.k, kTr.k], [kT_k[slot]])
            ps = next_ps()
            proj_fm(nT, C_GK, 1, ps, m=16)
            cx.copy("dve", gklrT.t[0:16, :], ps.t[0:16, 0:128], [ps.k], [gklrT.k])
            ps = next_ps()
            proj_tm(nT, C_VA, 512, ps)
            cx.copy("act", va_t[i2].t[:], ps.t[:], [ps.k], [va_t[i2].k])
            ps = next_ps()
            proj_tm(nT, C_KA, 256, ps)
            cx.copy("dve", ka_t[i2].t[:], ps.t[:, 0:256], [ps.k], [ka_t[i2].k])
            ps = next_ps()
            proj_tm(nT, C_RA, 512, ps)
            cx.act(sr_t[i2].t[:], ps.t[:], AF.Silu, [ps.k], [sr_t[i2].k])
            if qb >= KT0:
                ps = next_ps()
                proj_tm(nT, C_KB, 512, ps)
                cx.copy("act", kbf[i2].t[:], ps.t[:], [ps.k], [kbf[i2].k])
                cx.dma("sp", o_swak[s, (qb - KT0) * 128:(qb - KT0 + 1) * 128, :], kbf[i2].t[:], reads=[kbf[i2].k], out_dram=True)
            ps = next_ps()
            proj_tm(nT, C_VB, 512, ps)
            cx.copy("dve", Vr.t[:, slot, :, 0:64], ps.t[:].rearrange("p (h d) -> p h d", h=8), [ps.k, Vr.k], [V_k[slot]])
            if qb >= KT0:
                cx.copy("act", vbf[i2].t[:], ps.t[:], [ps.k], [vbf[i2].k])
                cx.dma("sp", o_swav[s, (qb - KT0) * 128:(qb - KT0 + 1) * 128, :], vbf[i2].t[:], reads=[vbf[i2].k], out_dram=True)

            ps = next_ps()
            cx.mm(ps.t[:, 0:256], gklrT.t[0:17, :], wgk.t[0:17, :], True, True, [gklrT.k, wgk.k], [ps.k])
            cx.act(e1_t[i2].t[:], ps.t[:, 0:256], AF.Exp, [ps.k], [e1_t[i2].k], scale=-1.0)
            cx.act(g_t[i2].t[:], e1_t[i2].t[:], AF.Ln, [e1_t[i2].k], [g_t[i2].k], bias=1.0, scale=1.0)
            gb_ = g_t[i2]
            ps = next_ps()
            for p in range(2):
                cx.mm(ps.t[:, p * 128:(p + 1) * 128], gb_.t[:, p * 128:(p + 1) * 128], ucum.t[:], True, True, [gb_.k, ucum.k], [ps.k])
            cx.mm(ps.t[:, 256:512], lrev.t[:], gb_.t[:], True, True, [gb_.k, lrev.k], [ps.k])
            cx.act(ebT[i2].t[:].rearrange("p k t -> p (k t)"), ps.t[:, 0:256], AF.Exp, [ps.k], [ebT[i2].k])
            cx.act(enbT[i2].t[:].rearrange("p k t -> p (k t)"), ps.t[:, 0:256], AF.Exp, [ps.k], [enbT[i2].k], scale=-1.0)
            cx.act(ed_t[i2].t[:], ps.t[:, 256:512], AF.Exp, [ps.k], [ed_t[i2].k])
            for hh in range(2):
                pr = slice(hh * 64, hh * 64 + 64)
                cx.stt("dve", qtT[i2].t[pr, :, hh, :], qkTa[i2].t[pr, 0:2, :], 0.125, ebT[i2].t[pr], ALU.mult, ALU.mult,
                       [qkTa[i2].k, ebT[i2].k], [qtT[i2].k])
            cx.tt("dve", ktT[i2].t[:], qkTa[i2].t[:, 2:4, :], enbT[i2].t[:], ALU.mult, [qkTa[i2].k, enbT[i2].k], [ktT[i2].k])
            for c in range(2):
                pr = slice(c * 64, c * 64 + 64)
                cx.tt("dve", khat[i2].t[pr, c, :], ka_t[i2].t[pr], ed_t[i2].t[pr], ALU.mult, [ka_t[i2].k, ed_t[i2].k], [khat[i2].k])
            ams = []
            for h in range(4):
                p, base = h // 2, (h % 2) * 64
                pss = psSb[cnt["s"] % 2]
                cnt["s"] += 1
                cx.mm(pss.t[:, 0:128], ktT[i2].t[:, p, :], qtT[i2].t[:, p, h % 2, :], True, True,
                      [ktT[i2].k, qtT[i2].k], [pss.k])
                am = Am[cnt["a"] % 4]
                cnt["a"] += 1
                cx.tt("dve", am.t[:], pss.t[:, 0:128], amask.t[:], ALU.mult, [pss.k, amask.k], [am.k])
                ams.append(am)
            psU = next_ps()
            for c in range(2):
                for h in range(4):
                    p, base = h // 2, (h % 2) * 64
                    r0 = c * 64
                    col = (c * 2 + p) * 128
                    cx.mm(psU.t[base:base + 64, col:col + 128], khat[i2].t[:, c, h * 64:(h + 1) * 64],
                          va_t[i2].t[:, h * 128:(h + 1) * 128], True, True, [khat[i2].k, va_t[i2].k], [psU.k])
            sbs = [Sb[sbi[0] % 4]]
            for c in range(2):
                for p in range(2):
                    col = (c * 2 + p) * 128
                    cx.stt("dve", Sst.t[:, p, :], Sst.t[:, p, :], ebT[i2].t[:, p, c * 64 + 63:c * 64 + 64], psU.t[:, col:col + 128],
                           ALU.mult, ALU.add, [Sst.k, ebT[i2].k, psU.k], [Sst.k])
                sbi[0] += 1
                nsb = Sb[sbi[0] % 4]
                cx.copy("act", nsb.t[:], Sst.t[:], [Sst.k], [nsb.k])
                sbs.append(nsb)
            for h in range(4):
                p, base = h // 2, (h % 2) * 64
                cx.mm(psGO.t[:, h, :], ams[h].t[:], va_t[i2].t[:, h * 128:(h + 1) * 128], True, False, [ams[h].k, va_t[i2].k], [psGO.k])
                cx.mm(psGO.t[0:64, h, :], qtT[i2].t[:, p, h % 2, 0:64], sbs[0].t[:, p, :], False, True,
                      [qtT[i2].k, sbs[0].k], [psGO.k])
                cx.mm(psGO.t[64:128, h, :], qtT[i2].t[:, p, h % 2, 64:128], sbs[1].t[:, p, :], False, True,
                      [qtT[i2].k, sbs[1].k], [psGO.k])
            gs = gst[i2]
            for h in range(4):
                cx.act(junk.t[:, 0:128], psGO.t[:, h, :], AF.Square, [psGO.k], [junk.k, gs.k], accum_out=gs.t[:, h:h + 1])
            cx.act(gs.t[:, 4:8], gs.t[:, 0:4], AF.Ln, [gs.k], [gs.k], scale=1.0 / 128, bias=EPS)
            cx.act(gs.t[:, 0:4], gs.t[:, 4:8], AF.Exp, [gs.k], [gs.k], scale=-0.5)
            oc = ocat[i2]
            for h in range(4):
                gt = gtmp[h % 2]
                cx.stt("dve", gt.t[:], psGO.t[:, h, :], gs.t[:, h:h + 1], gglab.t[:], ALU.mult, ALU.mult, [psGO.k, gs.k, gglab.k], [gt.k])
                cx.tt("dve", oc.t[:, h * 128:(h + 1) * 128], gt.t[:], sr_t[i2].t[:, h * 128:(h + 1) * 128], ALU.mult,
                      [gt.k, sr_t[i2].k], [oc.k])
            if qb == NT - 1:
                cx.dma("sp", o_glas[s].rearrange("(p h) k v -> (h k) p v", h=2), Sst.t[:], reads=[Sst.k], out_dram=True)

            for h in range(8):
                c4, base = h // 2, (h % 2) * 64
                pd = psD[h % 2]
                kbs = list(range(max(0, qb - 16), qb + 1))
                groups = [kbs[i:i + 4] for i in range(0, len(kbs), 4)]
                for gi, grp in enumerate(groups):
                    ng = len(grp)
                    pss = psSb[cnt["s"] % 2]
                    cnt["s"] += 1
                    for j, kb in enumerate(grp):
                        ks = kb % NR
                        cx.mm(pss.t[:, j * 128:(j + 1) * 128], kTr.t[:, c4, ks * 128:(ks + 1) * 128], qTb[i2].t[:, c4, h % 2, :],
                              True, True, [kT_k[ks], qTb[i2].k], [pss.k])
                    pe_ = pe_t[cnt["p"] % 2]
                    pm_ = pm_t[cnt["p"] % 2]
                    cnt["p"] += 1
                    cx.act(pe_.t[:, 0:ng * 128], pss.t[:, 0:ng * 128], AF.Exp, [pss.k], [pe_.k], scale=0.125)
                    r0 = 16 - (qb - grp[0])
                    cx.tt("dve", pm_.t[:, 0:ng * 128], pe_.t[:, 0:ng * 128], dmf.t[:, h, r0:r0 + ng, :].rearrange("p r q -> p (r q)"), ALU.mult,
                          [pe_.k, dmf.k], [pm_.k])
                    for j, kb in enumerate(grp):
                        ks = kb % NR
                        first = gi == 0 and j == 0
                        last = gi == len(groups) - 1 and j == ng - 1
                        cx.mm(pd.t[:, 0:65], pm_.t[:, j * 128:(j + 1) * 128], Vr.t[:, ks, h, :], first, last, [pm_.k, V_k[ks]], [pd.k])
                rd = rden[cnt["r"] % 4]
                cnt["r"] += 1
                S.add("dve", lambda e, rd=rd, pd=pd, h=h: e.reciprocal(out=rd.t[:], in_=pd.t[:, 64:65]), [pd.k], [rd.k])
                cx.ts("dve", oc.t[:, 512 + h * 64:512 + (h + 1) * 64], pd.t[:, 0:64], rd.t[:], None, ALU.mult, None,
                      [pd.k, rd.k], [oc.k])

            transpose8(oc, oT_t[i2])
            hb = xb
            for half in range(2):
                ps = next_ps()
                for k in range(8):
                    cx.mm(ps.t[:], oT_t[i2].t[:, k, :], wout.t[:, k, half * 512:(half + 1) * 512], k == 0, k == 7,
                          [oT_t[i2].k, wout.k], [ps.k])
                cx.tt("dve", hb.t[:, half * 512:(half + 1) * 512], ps.t[:], xb.t[:, half * 512:(half + 1) * 512], ALU.add,
                      [ps.k, xb.k], [hb.k])
            cx.dma("sp", h1_d[s, qb * 128:(qb + 1) * 128, :], hb.t[:], reads=[hb.k], out_dram=True)
    cx.close()


def host_consts_b():
    t = np.arange(128)
    return {
        "c_tri": (t[:, None] < t[None, :]).astype(np.float32),
        "c_iota32": np.broadcast_to(np.arange(32, dtype=np.float32), (128, 32)).copy(),
        "c_iota4": np.broadcast_to(np.arange(4, dtype=np.float32), (128, 4)).copy(),
    }


def phase_b(nc, din, dout, NT, nseq, h1_d, c_ident, debug, h1s_d=None):
    T = NT * 128
    NTOT = nseq * NT + (1 if h1s_d is not None else 0)
    if h1s_d is not None:
        cmk = din("cmk", [NSS, 256, D])
        cmv = din("cmv", [NSS, 256, D])
        c_sel = nc_input(nc, din, "c_sel", [8, 2048])
    memp = din("memp", [nseq, 256, D])
    g2 = din("g_norm2", [1, D])
    gm = din("g_mem", [1, D])
    g3 = din("g_norm3", [1, D])
    w_cq = din("w_cq", [D, D])
    w_mk = din("w_mk", [D, D])
    w_mv = din("w_mv", [D, D])
    w_co = din("w_co", [D, D])
    w_r = din("w_r", [D, 36])
    b_r = din("b_r", [1, 36])
    c_tri = din("c_tri", [128, 128])
    c_iota32 = nc_input(nc, din, "c_iota32", [128, 32])
    c_iota4 = din("c_iota4", [128, 4])
    o_memk = dout("o_memk", [nseq, 256, D])
    o_memv = dout("o_memv", [nseq, 256, D])
    mk_out = dout if debug else (lambda n, sh, dt=F32: nc.dram_tensor(n, list(sh), dt).ap())
    h2_d = mk_out("h2", [NTOT * 128, D])
    xn_d = mk_out("xn", [NTOT * 128, D], BF16)
    route_d = mk_out("route", [128, NTOT, 8])
    cnt_d = mk_out("cnt", [128, 32])

    cx = Ctx(nc)
    S = cx.S
    ident_b = cx.sb([128, 128], BF16, "b_ident_b")
    ident_f = cx.sb([128, 128], F32, "b_ident_f")
    tri_b = cx.sb([128, 128], BF16, "b_tri")
    ones_b = cx.sb([128, 128], BF16, "b_ones")
    iota32 = cx.sb([128, 32], F32, "b_iota32")
    iota4 = cx.sb([128, 4], F32, "b_iota4")
    g2b = cx.sb([128, D], F32, "g2b")
    g3b = cx.sb([128, D], F32, "g3b")
    gmb = cx.sb([128, D], F32, "gmb")
    brb = cx.sb([128, 36], F32, "brb")
    wcq = cx.sb([128, 8, D], BF16, "wcq")
    wco = cx.sb([128, 8, D], BF16, "wco")
    wmk = cx.sb([128, 8, D], BF16, "wmk")
    wmv = cx.sb([128, 8, D], BF16, "wmv")
    wr = cx.sb([128, 8, 36], F32, "wr")
    mT = cx.sb([128, 8, 256], BF16, "mT")
    mkT = cx.sb([128, 8, 256], BF16, "mkT")
    mva = cx.sb([128, 2, 4, 257], BF16, "mva")
    route = cx.sb([128, NTOT, 8], F32, "route_sb")
    cntb = cx.sb([128, 32], F32, "cntb")

    if h1s_d is not None:
        sel = cx.sb([8, 16, 128], BF16, "b_sel")
        cx.dma("pool", sel.t[:].rearrange("p c t -> p (c t)"), c_sel, writes=[sel.k])
        stgk = [cx.sb([128, 2, D], BF16, f"b_stgk{i}") for i in range(2)]
        mkTs = [cx.sb([128, 8, 256], BF16, f"b_mkTs{i}") for i in range(2)]
        mvas = [cx.sb([128, 2, 4, 257], BF16, f"b_mvas{i}") for i in range(2)]
        PTs = [cx.sb([128, 8, 8], BF16, f"b_PTs{i}") for i in range(2)]
        ocs = [cx.sb([8, D], BF16, f"b_ocs{i}") for i in range(2)]
        rdens = [cx.sb([8, 1], F32, f"b_rdens{i}") for i in range(2)]
        for v_ in mvas:
            S.add("pool", lambda e, v_=v_: e.memset(v_.t[:], 1.0), (), [v_.k])
    cx.dma("pool", ident_b.t[:], c_ident, writes=[ident_b.k])
    cx.dma("sp", ident_f.t[:], c_ident, writes=[ident_f.k])
    cx.dma("pool", tri_b.t[:], c_tri, writes=[tri_b.k])
    cx.dma("sp", iota32.t[:], c_iota32, writes=[iota32.k])
    cx.dma("sp", iota4.t[:], c_iota4, writes=[iota4.k])
    cx.dma("sp", g2b.t[:], g2.partition_broadcast(128), writes=[g2b.k])
    cx.dma("sp", g3b.t[:], g3.partition_broadcast(128), writes=[g3b.k])
    cx.dma("sp", gmb.t[:], gm.partition_broadcast(128), writes=[gmb.k])
    cx.dma("sp", brb.t[:], b_r.partition_broadcast(128), writes=[brb.k])
    cx.dma("sp", wr.t[:], w_r.rearrange("(k p) f -> p k f", p=128), writes=[wr.k])
    for wsb, wd in ((wmk, w_mk), (wmv, w_mv), (wcq, w_cq), (wco, w_co)):
        v = wd.rearrange("(k p) f -> p k f", p=128)
        for k in range(8):
            cx.dma("pool", wsb.t[:, k, :], v[:, k, :], writes=[wsb.k])
    S.add("pool", lambda e: e.memset(ones_b.t[:], 1.0), (), [ones_b.k])
    S.add("pool", lambda e: e.memset(mva.t[:], 1.0), (), [mva.k])
    S.add("dve", lambda e: e.memset(cntb.t[:], 0.0), (), [cntb.k])
    S.add("dve", lambda e: e.memset(route.t[:], 0.0), (), [route.k])

    psT = cx.ps([128, 1024], BF16, "b_psT")
    psF = [cx.ps([128, 512], F32, f"b_psF{i}") for i in range(2)]
    psR = [cx.ps([128, 512], F32, f"b_psR{i}") for i in range(2)]
    psD = [cx.ps([128, 512], F32, f"b_psD{i}") for i in range(2)]
    psX = cx.ps([128, 512], F32, "b_psX")
    rr = [0]

    def next_ps():
        b = psR[rr[0] % 2]
        rr[0] += 1
        return b

    def rot(n, shape, dt, name):
        return [cx.sb(shape, dt, f"b_{name}{i}") for i in range(n)]

    h_t = rot(2, [128, D], F32, "h")
    junk = cx.sb([128, D], BF16, "b_junk")
    st_t = rot(2, [128, 8], F32, "stat")
    n_t = rot(2, [128, D], BF16, "n")
    nT_t = rot(2, [128, 8, 128], BF16, "nT")
    qT_t = rot(2, [128, 8, 128], BF16, "qT")
    PT_t = rot(2, [128, 8, 128], BF16, "PT")
    oc_t = rot(2, [128, D], BF16, "oc")
    oT_t = rot(2, [128, 8, 128], BF16, "oT")
    rden = rot(4, [128, 1], F32, "rden")
    mo_t = rot(2, [128, 512], F32, "mo")
    xf_t = rot(2, [128, D], F32, "xf")
    xb_t = rot(2, [128, D], BF16, "xb")
    xfT = cx.sb([128, 8, 128], F32, "b_xfT")
    rt_t = rot(2, [128, 64], F32, "rt")
    mx8 = rot(2, [128, 8], F32, "mx8")
    ix8 = rot(2, [128, 8], U32, "ix8")
    ixf = rot(2, [128, 8], F32, "ixf")
    O0_t = rot(2, [128, 32], F32, "O0")
    O1_t = rot(2, [128, 32], F32, "O1")
    Ob_t = rot(2, [128, 32], BF16, "Ob")
    rk_t = rot(2, [128, 32], F32, "rk")
    t32 = rot(2, [128, 32], F32, "t32")
    cnt = {"r": 0}

    def rmsnorm(xb, gb, out, stb, eng="dve"):
        cx.act(junk.t[:], xb.t[:], AF.Square, [xb.k], [junk.k, stb.k], accum_out=stb.t[:, 0:1])
        cx.act(stb.t[:, 1:2], stb.t[:, 0:1], AF.Ln, [stb.k], [stb.k], scale=1.0 / D, bias=EPS)
        cx.act(stb.t[:, 2:3], stb.t[:, 1:2], AF.Exp, [stb.k], [stb.k], scale=-0.5)
        cx.stt(eng, out.t[:], xb.t[:], stb.t[:, 2:3], gb.t[:], ALU.mult, ALU.mult, [xb.k, stb.k, gb.k], [out.k])

    def transpose8(src, dst_ap, dst_k):
        for k in range(8):
            cx.tr(psT.t[:, k * 128:(k + 1) * 128], src.t[:, k * 128:(k + 1) * 128], ident_b.t[:], [src.k, ident_b.k], [psT.k])
        cx.copy("act", dst_ap, psT.t[:].rearrange("p (k t) -> p k t", k=8), [psT.k], [dst_k])

    gt = 0
    units = [(s_, False) for s_ in range(nseq)] + ([(0, True)] if h1s_d is not None else [])
    for s, is_samp in units:
      if not is_samp:
          for mt in range(2):
              hb, nb, stb = h_t[mt], n_t[mt], st_t[mt]
              cx.dma("sp", hb.t[:], memp[s, mt * 128:(mt + 1) * 128, :], writes=[hb.k])
              rmsnorm(hb, gmb, nb, stb)
              transpose8(nb, mT.t[:, :, mt * 128:(mt + 1) * 128], mT.k)
          for mt in range(2):
              for wsb, od, isv in ((wmk, o_memk, False), (wmv, o_memv, True)):
                  for half in range(2):
                      ps = next_ps()
                      for k in range(8):
                          cx.mm(ps.t[:], mT.t[:, k, mt * 128:(mt + 1) * 128], wsb.t[:, k, half * 512:(half + 1) * 512], k == 0, k == 7,
                                [mT.k, wsb.k], [ps.k])
                      mo = mo_t[rr[0] % 2]
                      cx.copy("act", mo.t[:], ps.t[:], [ps.k], [mo.k])
                      if isv:
                          cx.copy("dve", mva.t[:, mt, 2 * half:2 * half + 2, 0:256], ps.t[:].rearrange("p (h d) -> p h d", h=2), [ps.k], [mva.k])
                      cx.dma("sp", od[s, mt * 128:(mt + 1) * 128, half * 512:(half + 1) * 512], mo.t[:], reads=[mo.k], out_dram=True)
          for c2 in range(4):
              ps = next_ps()
              for j in range(2):
                  c8 = c2 * 2 + j
                  for k in range(8):
                      cx.mm(ps.t[:, j * 256:(j + 1) * 256], wmk.t[:, k, c8 * 128:(c8 + 1) * 128], mT.t[:, k, :], k == 0, k == 7,
                            [wmk.k, mT.k], [ps.k])
              cx.copy("act", mkT.t[:, c2 * 2:c2 * 2 + 2, :], ps.t[:].rearrange("p (j m) -> p j m", j=2), [ps.k], [mkT.k])

      for qb in range(1 if is_samp else NT):
            i2 = gt % 2
            hb, nb, nT, stb = h_t[i2], n_t[i2], nT_t[i2], st_t[i2]
            cx.dma("sp", hb.t[:], h1s_d if is_samp else h1_d[s, qb * 128:(qb + 1) * 128, :], writes=[hb.k])
            rmsnorm(hb, g2b, nb, stb)
            transpose8(nb, nT.t[:], nT.k)
            qT = qT_t[i2]
            for half in range(2):
                ps = next_ps()
                for j in range(4):
                    c8 = half * 4 + j
                    for k in range(8):
                        cx.mm(ps.t[:, j * 128:(j + 1) * 128], wcq.t[:, k, c8 * 128:(c8 + 1) * 128], nT.t[:, k, :], k == 0, k == 7,
                              [wcq.k, nT.k], [ps.k])
                cx.copy("act", qT.t[:, half * 4:half * 4 + 4, :], ps.t[:].rearrange("p (j t) -> p j t", j=4), [ps.k], [qT.k])
            if is_samp:
                oc = oc_t[i2]
                for c in range(NSS):
                    sk, mkT_, mva_, PT_, oc_, rd_ = stgk[c % 2], mkTs[c % 2], mvas[c % 2], PTs[c % 2], ocs[c % 2], rdens[c % 2]
                    cx.dma("pool", sk.t[:], cmk[c].rearrange("(m p) f -> p m f", p=128), writes=[sk.k])
                    for mb in range(2):
                        cx.dma("pool", mva_.t[:, mb, :, 0:256], cmv[c, mb * 128:(mb + 1) * 128, :].rearrange("p (h d) -> p h d", h=4), writes=[mva_.k])
                    for mb in range(2):
                        for c8 in range(8):
                            cx.tr(psT.t[:, c8 * 128:(c8 + 1) * 128], sk.t[:, mb, c8 * 128:(c8 + 1) * 128], ident_b.t[:], [sk.k, ident_b.k], [psT.k])
                        cx.copy("act", mkT_.t[:, :, mb * 128:(mb + 1) * 128], psT.t[:].rearrange("p (k t) -> p k t", k=8), [psT.k], [mkT_.k])
                    ps = next_ps()
                    for h in range(4):
                        for mb in range(2):
                            idx = h * 2 + mb
                            for j in range(2):
                                cx.mm(ps.t[:, idx * 8:(idx + 1) * 8], mkT_.t[:, 2 * h + j, mb * 128:(mb + 1) * 128], qT.t[:, 2 * h + j, 8 * c:8 * c + 8],
                                      j == 0, j == 1, [mkT_.k, qT.k], [ps.k])
                    cx.act(PT_.t[:].rearrange("p a b -> p (a b)"), ps.t[:, 0:64], AF.Exp, [ps.k], [PT_.k], scale=1.0 / 16)
                    for h in range(4):
                        pd = psD[h % 2]
                        for mb in range(2):
                            cx.mm(pd.t[0:8, 0:257], PT_.t[:, h * 2 + mb, :], mva_.t[:, mb, h, :], mb == 0, mb == 1, [PT_.k, mva_.k], [pd.k])
                        S.add("dve", lambda e, rd_=rd_, pd=pd: e.reciprocal(out=rd_.t[:], in_=pd.t[0:8, 256:257]), [pd.k], [rd_.k])
                        cx.ts("dve", oc_.t[:, h * 256:(h + 1) * 256], pd.t[0:8, 0:256], rd_.t[:], None, ALU.mult, None, [pd.k, rd_.k], [oc_.k])
                    for half in range(2):
                        cx.mm(psF[half].t[:], sel.t[:, c, :], oc_.t[:, half * 512:(half + 1) * 512], c == 0, c == NSS - 1, [sel.k, oc_.k], [psF[half].k])
                for half in range(2):
                    cx.copy("act", oc.t[:, half * 512:(half + 1) * 512], psF[half].t[:], [psF[half].k], [oc.k])
            else:
                PT = PT_t[i2]
                for hp in range(2):
                    ps = next_ps()
                    for hh in range(2):
                        h = hp * 2 + hh
                        for mb in range(2):
                            idx = hh * 2 + mb
                            for j in range(2):
                                cx.mm(ps.t[:, idx * 128:(idx + 1) * 128], mkT.t[:, 2 * h + j, mb * 128:(mb + 1) * 128], qT.t[:, 2 * h + j, :],
                                      j == 0, j == 1, [mkT.k, qT.k], [ps.k])
                    cx.act(PT.t[:, hp * 4:hp * 4 + 4, :], ps.t[:].rearrange("p (j t) -> p j t", j=4), AF.Exp, [ps.k], [PT.k], scale=1.0 / 16)
                oc = oc_t[i2]
                for h in range(4):
                    pd = psD[h % 2]
                    for mb in range(2):
                        cx.mm(pd.t[:, 0:257], PT.t[:, h * 2 + mb, :], mva.t[:, mb, h, :], mb == 0, mb == 1, [PT.k, mva.k], [pd.k])
                    rd = rden[cnt["r"] % 4]
                    cnt["r"] += 1
                    S.add("dve", lambda e, rd=rd, pd=pd: e.reciprocal(out=rd.t[:], in_=pd.t[:, 256:257]), [pd.k], [rd.k])
                    cx.ts("dve", oc.t[:, h * 256:(h + 1) * 256], pd.t[:, 0:256], rd.t[:], None, ALU.mult, None, [pd.k, rd.k], [oc.k])
            oT = oT_t[i2]
            transpose8(oc, oT.t[:], oT.k)
            for half in range(2):
                ps = next_ps()
                for k in range(8):
                    cx.mm(ps.t[:], oT.t[:, k, :], wco.t[:, k, half * 512:(half + 1) * 512], k == 0, k == 7, [oT.k, wco.k], [ps.k])
                cx.tt("dve", hb.t[:, half * 512:(half + 1) * 512], ps.t[:], hb.t[:, half * 512:(half + 1) * 512], ALU.add, [ps.k, hb.k], [hb.k])
            cx.dma("sp", h2_d[gt * 128:(gt + 1) * 128, :], hb.t[:], reads=[hb.k], out_dram=True)
            xf, xb = xf_t[i2], xb_t[i2]
            rmsnorm(hb, g3b, xf, stb)
            cx.copy("act", xb.t[:], xf.t[:], [xf.k], [xb.k])
            cx.dma("sp", xn_d[gt * 128:(gt + 1) * 128, :], xb.t[:], reads=[xb.k], out_dram=True)
            for half in range(2):
                for j in range(4):
                    k = half * 4 + j
                    cx.tr(psF[half].t[:, j * 128:(j + 1) * 128], xf.t[:, k * 128:(k + 1) * 128], ident_f.t[:], [xf.k, ident_f.k], [psF[half].k])
                cx.copy("act", xfT.t[:, half * 4:half * 4 + 4, :], psF[half].t[:].rearrange("p (j t) -> p j t", j=4), [psF[half].k], [xfT.k])
            for k in range(8):
                cx.mm(psX.t[:, 0:36], xfT.t[:, k, :], wr.t[:, k, :], k == 0, k == 7, [xfT.k, wr.k], [psX.k])
            rt = rt_t[i2]
            R_ = [rt.k]
            c = lambda a, b=None: rt.t[:, a:(a + 1 if b is None else b)]
            cx.tt("dve", c(0, 36), psX.t[:, 0:36], brb.t[:], ALU.add, [psX.k, brb.k], R_)
            S.add("dve", lambda e, rt=rt: e.tensor_reduce(out=rt.t[:, 36:37], in_=rt.t[:, 0:4], axis=AX.X, op=ALU.max), R_, R_)
            cx.ts("dve", c(37), c(36), -1.0, None, ALU.mult, None, R_, R_)
            cx.act(c(45, 49), c(0, 4), AF.Exp, R_, R_, bias=c(37), scale=1.0, accum_out=c(38))
            S.add("dve", lambda e, rt=rt: e.reciprocal(out=rt.t[:, 39:40], in_=rt.t[:, 38:39]), R_, R_)
            cx.ts("dve", c(40, 44), c(0, 4), c(36), None, ALU.is_equal, None, R_, R_)
            cx.tt("dve", c(45, 49), c(40, 44), iota4.t[:], ALU.mult, R_ + [iota4.k], R_)
            S.add("dve", lambda e, rt=rt: e.tensor_reduce(out=rt.t[:, 44:45], in_=rt.t[:, 45:49], axis=AX.X, op=ALU.add), R_, R_)
            cx.ts("dve", c(49, 57), c(4, 12), c(40), None, ALU.mult, None, R_, R_)
            for g in range(1, 4):
                cx.stt("dve", c(49, 57), c(4 + 8 * g, 12 + 8 * g), c(40 + g), c(49, 57), ALU.mult, ALU.add, R_, R_)
            m8, i8, i8f = mx8[i2], ix8[i2], ixf[i2]
            S.add("dve", lambda e, rt=rt, m8=m8: e.max(out=m8.t[:], in_=rt.t[:, 49:57]), R_, [m8.k])
            S.add("dve", lambda e, rt=rt, m8=m8, i8=i8: e.max_index(out=i8.t[:], in_max=m8.t[:], in_values=rt.t[:, 49:57]), R_ + [m8.k], [i8.k])
            cx.copy("dve", i8f.t[:], i8.t[:], [i8.k], [i8f.k])
            cx.tt("dve", c(57), m8.t[:, 1:2], m8.t[:, 0:1], ALU.subtract, [m8.k], R_)
            cx.act(c(58), c(57), AF.Exp, R_, R_)
            cx.ts("dve", c(59), c(58), 1.0, None, ALU.add, None, R_, R_)
            S.add("dve", lambda e, rt=rt: e.reciprocal(out=rt.t[:, 59:60], in_=rt.t[:, 59:60]), R_, R_)
            cx.tt("dve", c(60), c(59), c(39), ALU.mult, R_, R_)
            cx.tt("dve", c(61), c(60), c(58), ALU.mult, R_, R_)
            cx.stt("dve", c(62), c(44), 8.0, i8f.t[:, 0:1], ALU.mult, ALU.add, R_ + [i8f.k], R_)
            cx.stt("dve", c(63), c(44), 8.0, i8f.t[:, 1:2], ALU.mult, ALU.add, R_ + [i8f.k], R_)
            O0, O1, Ob, rk, tt32 = O0_t[i2], O1_t[i2], Ob_t[i2], rk_t[i2], t32[i2]
            cx.ts("dve", O0.t[:], iota32.t[:], c(62), None, ALU.is_equal, None, R_ + [iota32.k], [O0.k])
            cx.ts("dve", O1.t[:], iota32.t[:], c(63), None, ALU.is_equal, None, R_ + [iota32.k], [O1.k])
            cx.tt("dve", Ob.t[:], O0.t[:], O1.t[:], ALU.add, [O0.k, O1.k], [Ob.k])
            cx.mm(psX.t[:, 64:96], tri_b.t[:], Ob.t[:], True, True, [tri_b.k, Ob.k], [psX.k])
            cx.mm(psX.t[:, 96:128], ones_b.t[:], Ob.t[:], True, True, [ones_b.k, Ob.k], [psX.k])
            cx.tt("dve", rk.t[:], psX.t[:, 64:96], cntb.t[:], ALU.add, [psX.k, cntb.k], [rk.k])
            cx.tt("dve", cntb.t[:], psX.t[:, 96:128], cntb.t[:], ALU.add, [psX.k, cntb.k], [cntb.k])
            cx.tt("dve", tt32.t[:], O0.t[:], rk.t[:], ALU.mult, [O0.k, rk.k], [tt32.k])
            S.add("dve", lambda e, tt32=tt32, gt=gt: e.tensor_reduce(out=route.t[:, gt, 2:3], in_=tt32.t[:], axis=AX.X, op=ALU.add), [tt32.k], [route.k])
            cx.tt("dve", tt32.t[:], O1.t[:], rk.t[:], ALU.mult, [O1.k, rk.k], [tt32.k])
            S.add("dve", lambda e, tt32=tt32, gt=gt: e.tensor_reduce(out=route.t[:, gt, 3:4], in_=tt32.t[:], axis=AX.X, op=ALU.add), [tt32.k], [route.k])
            cx.copy("dve", route.t[:, gt, 0:2], c(62, 64), R_, [route.k])
            cx.copy("dve", route.t[:, gt, 4:6], c(60, 62), R_, [route.k])
            gt += 1
    cx.dma("sp", route_d, route.t[:], reads=[route.k], out_dram=True)
    cx.dma("sp", cnt_d, cntb.t[:], reads=[cntb.k], out_dram=True)
    cx.close()
    return h2_d, xn_d, route_d, cnt_d


BLK = 256


def host_consts_c():
    p = np.arange(128, dtype=np.float32)
    return {
        "c_thr": (p * BLK).reshape(128, 1).astype(np.float32),
        "c_kp": (np.arange(8, dtype=np.float32)[None, :] * 128 + p[:, None]).astype(np.float32),
    }


def phase_c(nc, din, dout, NTOT, h2_d, xn_d, route_d, cnt_d, c_ident, y_d):
    NTOK = NTOT * 128
    NB = -(-2 * NTOK // BLK) + 32
    assert NB <= 128
    R = NB * BLK
    w_e1 = din("w_e1", [32 * D, 512])
    w_e3 = din("w_e3", [32 * D, 512])
    w_e2 = din("w_e2", [32 * 512, D])
    gf = din("g_final", [1, D])
    c_iota32 = nc_input(nc, din, "c_iota32", [128, 32])
    c_thr = din("c_thr", [128, 1])
    c_kp = din("c_kp", [128, 8])
    Xs = nc.dram_tensor("moe_xs", [R, D], BF16).ap()
    Ys = nc.dram_tensor("moe_ys", [R, D], F32).ap()
    Xs_k, Ys_k = Tok("Xs"), Tok("Ys")
    xs_parts, ys_parts = [], []

    cx = Ctx(nc)
    S = cx.S
    ident_b = cx.sb([128, 128], BF16, "c_ident_b")
    ident_f = cx.sb([128, 128], F32, "c_ident_f")
    ones_f = cx.sb([128, 128], F32, "c_ones_f")
    iota32 = cx.sb([128, 32], F32, "c_iota32s")
    thr = cx.sb([128, 1], F32, "c_thrs")
    kp = cx.sb([128, 8], F32, "c_kps")
    gfb = cx.sb([128, D], F32, "gfb")
    route = cx.sb([128, NTOT, 8], F32, "c_route")
    cntb = cx.sb([128, 32], F32, "c_cnt")
    sm = cx.sb([128, 8, 32], F32, "c_sm")
    z32 = cx.sb([128, 32], F32, "c_z32")
    becol = cx.sb([128, 2], F32, "c_becol")
    dgb = cx.sb([128, 128], F32, "c_dgb")
    bebc = cx.sb([128, 128], F32, "c_bebc")
    idf = cx.sb([128, NB, 12], F32, "c_idf")
    idi = cx.sb([128, NB, 12], I32, "c_idi")
    dstf = cx.sb([128, NTOT, 2], F32, "c_dstf")
    dsti = cx.sb([128, NTOT, 2], I32, "c_dsti")
    psT = cx.ps([128, 1024], BF16, "c_psT")
    psH1 = [cx.ps([128, 512], F32, f"c_psH1{i}") for i in range(2)]
    psH3 = [cx.ps([128, 512], F32, f"c_psH3{i}") for i in range(2)]
    psY = [cx.ps([128, 512], F32, f"c_psY{i}") for i in range(2)]
    psX = cx.ps([128, 512], F32, "c_psX")

    cx.dma("pool", ident_b.t[:], c_ident, writes=[ident_b.k])
    cx.dma("sp", ident_f.t[:], c_ident, writes=[ident_f.k])
    cx.dma("sp", iota32.t[:], c_iota32, writes=[iota32.k])
    cx.dma("sp", thr.t[:], c_thr, writes=[thr.k])
    cx.dma("sp", kp.t[:], c_kp, writes=[kp.k])
    cx.dma("sp", gfb.t[:], gf.partition_broadcast(128), writes=[gfb.k])
    cx.dma("sp", route.t[:], route_d, writes=[route.k])
    cx.dma("sp", cntb.t[:], cnt_d, writes=[cntb.k])
    S.add("dve", lambda e: e.memset(ones_f.t[:], 1.0), (), [ones_f.k])
    S.add("dve", lambda e: e.memset(z32.t[:], 0.0), (), [z32.k])
    K_ = [sm.k]
    padded, pend, pstart, cmp_ = sm.t[:, 0, :], sm.t[:, 1, :], sm.t[:, 2, :], sm.t[:, 3, :]
    tmpa, tmpb = sm.t[:, 4, :], sm.t[:, 5, :]
    cx.ts("dve", tmpa, cntb.t[:], 0.0, None, ALU.is_gt, None, [cntb.k], K_)
    for j in range(1, NTOK // BLK + 1):
        cx.stt("dve", tmpa, cntb.t[:], float(BLK * j), tmpa, ALU.is_gt, ALU.add, [cntb.k] + K_, K_)
    cx.ts("dve", padded, tmpa, float(BLK), None, ALU.mult, None, K_, K_)
    S.add("dve", lambda e: e.tensor_tensor_scan(out=pend, data0=padded, data1=z32.t[:], initial=0.0, op0=ALU.add, op1=ALU.add), K_ + [z32.k], K_)
    cx.tt("dve", pstart, pend, padded, ALU.subtract, K_, K_)
    cx.ts("dve", cmp_, pend, thr.t[:], None, ALU.is_le, None, K_ + [thr.k], K_)
    S.add("dve", lambda e: e.tensor_reduce(out=becol.t[:, 0:1], in_=cmp_, axis=AX.X, op=ALU.add), K_, [becol.k])
    cx.ts("dve", becol.t[:, 1:2], becol.t[:, 0:1], 31.0, None, ALU.min, None, [becol.k], [becol.k])
    cx.ts("dve", dgb.t[:], ident_f.t[:], becol.t[:, 1:2], None, ALU.mult, None, [ident_f.k, becol.k], [dgb.k])
    cx.mm(psX.t[:, 0:128], ones_f.t[:], dgb.t[:], True, True, [ones_f.k, dgb.k], [psX.k])
    cx.copy("dve", bebc.t[:], psX.t[:, 0:128], [psX.k], [bebc.k])
    for k in range(8):
        cx.ts("dve", idf.t[:, :, k], bebc.t[:, 0:NB], 1024.0, kp.t[:, k:k + 1], ALU.mult, ALU.add, [bebc.k, kp.k], [idf.k])
    for k in range(4):
        cx.ts("dve", idf.t[:, :, 8 + k], bebc.t[:, 0:NB], 512.0, kp.t[:, k:k + 1], ALU.mult, ALU.add, [bebc.k, kp.k], [idf.k])
    cx.copy("dve", idi.t[:], idf.t[:], [idf.k], [idi.k])

    def rot(n, shape, dt, name):
        return [cx.sb(shape, dt, f"c_{name}{i}") for i in range(n)]

    zt = cx.sb([128, 8, D], BF16, "c_zero")
    S.add("pool", lambda e: e.memset(zt.t[:], 0.0), (), [zt.k])
    r0 = 0
    while r0 < R:
        nr = min(1024, R - r0)
        zk = Tok("xz")
        cx.dma("sp", Xs[r0:r0 + nr, :].rearrange("(s p) f -> p s f", p=128), zt.t[:, 0:nr // 128, :], reads=[zt.k], writes=[zk], out_dram=True)
        xs_parts.append(zk)
        r0 += nr
    S.add("pool", None, xs_parts, [Xs_k])
    xs_parts = []
    xn_t = rot(3, [128, D], BF16, "xn")
    o_t = rot(2, [128, 32], F32, "o32")
    for gt in range(NTOT):
        xb = xn_t[gt % 3]
        cx.dma("sp", xb.t[:], xn_d[gt * 128:(gt + 1) * 128, :], writes=[xb.k])
        for sl in range(2):
            o32 = o_t[sl]
            cx.ts("dve", o32.t[:], iota32.t[:], route.t[:, gt, sl:sl + 1], None, ALU.is_equal, None, [iota32.k, route.k], [o32.k])
            cx.tt("dve", o32.t[:], o32.t[:], pstart, ALU.mult, [o32.k] + K_, [o32.k])
            S.add("dve", lambda e, o32=o32, gt=gt, sl=sl: e.tensor_reduce(out=dstf.t[:, gt, sl:sl + 1], in_=o32.t[:], axis=AX.X, op=ALU.add),
                  [o32.k], [dstf.k])
        cx.tt("dve", dstf.t[:, gt, :], dstf.t[:, gt, :], route.t[:, gt, 2:4], ALU.add, [dstf.k, route.k], [dstf.k])
        cx.copy("dve", dsti.t[:, gt, :], dstf.t[:, gt, :], [dstf.k], [dsti.k])
        for sl in range(2):
            S.add("pool", lambda e, xb=xb, gt=gt, sl=sl: e.indirect_dma_start(
                out=Xs, out_offset=bass.IndirectOffsetOnAxis(ap=dsti.t[:, gt, sl:sl + 1], axis=0), in_=xb.t[:, :], in_offset=None),
                [xb.k, dsti.k, Xs_k], [xs_parts.append(Tok("xsc")) or xs_parts[-1]], dma=True, out=True)

    S.add("sp", None, xs_parts, [Xs_k])
    xs_t = rot(2, [128, 2, D], BF16, "xs")
    xsT_t = rot(2, [128, 8, 256], BF16, "xsT")
    w1_t = rot(2, [128, 8, 512], BF16, "w1")
    w3_t = rot(2, [128, 8, 512], BF16, "w3")
    w2_t = rot(2, [128, 4, D], BF16, "w2")
    sg_t = rot(2, [128, 512], F32, "sg")
    hT_t = rot(2, [128, 4, 256], BF16, "hT")
    yo_t = rot(2, [128, D], F32, "yo")
    for b in range(NB):
        i2 = b % 2
        xs, xsT, w1, w3, w2, hT = xs_t[i2], xsT_t[i2], w1_t[i2], w3_t[i2], w2_t[i2], hT_t[i2]
        for k in range(8):
            S.add("pool", lambda e, w1=w1, k=k, b=b: e.indirect_dma_start(
                out=w1.t[:, k, :], out_offset=None, in_=w_e1, in_offset=bass.IndirectOffsetOnAxis(ap=idi.t[:, b, k:k + 1], axis=0)),
                [idi.k], [w1.k], dma=True)
            S.add("pool", lambda e, w3=w3, k=k, b=b: e.indirect_dma_start(
                out=w3.t[:, k, :], out_offset=None, in_=w_e3, in_offset=bass.IndirectOffsetOnAxis(ap=idi.t[:, b, k:k + 1], axis=0)),
                [idi.k], [w3.k], dma=True)
        for k in range(4):
            S.add("pool", lambda e, w2=w2, k=k, b=b: e.indirect_dma_start(
                out=w2.t[:, k, :], out_offset=None, in_=w_e2, in_offset=bass.IndirectOffsetOnAxis(ap=idi.t[:, b, 8 + k:9 + k], axis=0)),
                [idi.k], [w2.k], dma=True)
        cx.dma("sp", xs.t[:], Xs[b * BLK:(b + 1) * BLK, :].rearrange("(s p) f -> p s f", p=128), reads=[Xs_k], writes=[xs.k])
        for sub in range(2):
            for k in range(8):
                cx.tr(psT.t[:, k * 128:(k + 1) * 128], xs.t[:, sub, k * 128:(k + 1) * 128], ident_b.t[:], [xs.k, ident_b.k], [psT.k])
            cx.copy("act", xsT.t[:, :, sub * 128:(sub + 1) * 128], psT.t[:].rearrange("p (k t) -> p k t", k=8), [psT.k], [xsT.k])
        for pr in range(2):
            for wt, pst in ((w1, psH1[pr]), (w3, psH3[pr])):
                for j in range(2):
                    fc = pr * 2 + j
                    for k in range(8):
                        cx.mm(pst.t[:, j * 256:(j + 1) * 256], wt.t[:, k, fc * 128:(fc + 1) * 128], xsT.t[:, k, :], k == 0, k == 7,
                              [wt.k, xsT.k], [pst.k])
            sg = sg_t[pr]
            cx.act(sg.t[:], psH1[pr].t[:], AF.Silu, [psH1[pr].k], [sg.k])
            cx.tt("dve", hT.t[:, pr * 2:pr * 2 + 2, :], sg.t[:].rearrange("p (j t) -> p j t", j=2),
                  psH3[pr].t[:].rearrange("p (j t) -> p j t", j=2), ALU.mult, [sg.k, psH3[pr].k], [hT.k])
        for sub in range(2):
            yo = yo_t[sub]
            for half in range(2):
                py = psY[half]
                for fk in range(4):
                    cx.mm(py.t[:], hT.t[:, fk, sub * 128:(sub + 1) * 128], w2.t[:, fk, half * 512:(half + 1) * 512], fk == 0, fk == 3,
                          [hT.k, w2.k], [py.k])
                cx.copy("act" if half else "dve", yo.t[:, half * 512:(half + 1) * 512], py.t[:], [py.k], [yo.k])
            cx.dma("sp", Ys[b * BLK + sub * 128:b * BLK + (sub + 1) * 128, :], yo.t[:], reads=[yo.k], writes=[ys_parts.append(Tok("ysp")) or ys_parts[-1]], out_dram=True)

    S.add("pool", None, ys_parts, [Ys_k])
    h_t = rot(2, [128, D], F32, "h")
    y0_t = rot(2, [128, D], F32, "y0")
    y1_t = rot(2, [128, D], F32, "y1")
    st_t = rot(2, [128, 8], F32, "stat")
    junk = cx.sb([128, D], BF16, "c_junk")
    for gt in range(NTOT):
        i2 = gt % 2
        hb, y0, y1, stb = h_t[i2], y0_t[i2], y1_t[i2], st_t[i2]
        cx.dma("sp", hb.t[:], h2_d[gt * 128:(gt + 1) * 128, :], writes=[hb.k])
        for sl, yy in ((0, y0), (1, y1)):
            S.add("pool", lambda e, yy=yy, gt=gt, sl=sl: e.indirect_dma_start(
                out=yy.t[:, :], out_offset=None, in_=Ys, in_offset=bass.IndirectOffsetOnAxis(ap=dsti.t[:, gt, sl:sl + 1], axis=0)),
                [Ys_k, dsti.k], [yy.k], dma=True)
        cx.stt("dve", hb.t[:], y0.t[:], route.t[:, gt, 4:5], hb.t[:], ALU.mult, ALU.add, [y0.k, route.k, hb.k], [hb.k])
        cx.stt("dve", hb.t[:], y1.t[:], route.t[:, gt, 5:6], hb.t[:], ALU.mult, ALU.add, [y1.k, route.k, hb.k], [hb.k])
        cx.act(junk.t[:], hb.t[:], AF.Square, [hb.k], [junk.k, stb.k], accum_out=stb.t[:, 0:1])
        cx.act(stb.t[:, 1:2], stb.t[:, 0:1], AF.Ln, [stb.k], [stb.k], scale=1.0 / D, bias=EPS)
        cx.act(stb.t[:, 2:3], stb.t[:, 1:2], AF.Exp, [stb.k], [stb.k], scale=-0.5)
        cx.stt("dve", y0.t[:], hb.t[:], stb.t[:, 2:3], gfb.t[:], ALU.mult, ALU.mult, [hb.k, stb.k, gfb.k], [y0.k])
        cx.dma("sp", y_d[gt * 128:(gt + 1) * 128, :], y0.t[:], reads=[y0.k], out_dram=True)
    cx.close()


_DIN_CACHE = {}


def nc_input(nc, din, name, shape):
    key = (id(nc), name)
    if key not in _DIN_CACHE:
        _DIN_CACHE[key] = din(name, shape)
    return _DIN_CACHE[key]


def host_consts_s():
    t = np.arange(128)
    ch = t // 8
    same = ch[:, None] == ch[None, :]
    ucum = np.where(same & (t[:, None] <= t[None, :]), -1.0 / 16, 0.0).astype(np.float32)
    lrev = np.where(same & (t[:, None] > t[None, :]), -1.0 / 16, 0.0).astype(np.float32)
    amask = np.where(same & (t[:, None] <= t[None, :]), 1.0, 0.0).astype(np.float32)
    rowmask = (ch[:, None] == np.arange(16)[None, :]).astype(np.float32)
    slopes = np.array([2.0 ** (-8.0 * (h + 1) / 8) for h in range(8)], np.float64)

    def mfun(dist):
        m = ((dist >= 0) & (dist <= 128)).astype(np.float64)
        m += ((dist >= 0) & (dist <= 512) & (dist % 4 == 0))
        m += ((dist >= 0) & (dist <= 2048) & (dist % 16 == 0))
        return m
    ki = t[:, None, None, None]
    kb = np.arange(16)[None, None, :, None]
    j = np.arange(8)[None, None, None, :]
    dist = 2048 + j - (kb * 128 + ki)
    ms = mfun(dist) * np.exp(-slopes[None, :, None, None] * dist)
    c = np.arange(16)[None, None, :, None]
    jp = ki - 8 * c
    dist2 = j - jp
    mn = np.where((jp >= 0) & (jp < 8), mfun(dist2) * np.exp(-slopes[None, :, None, None] * np.maximum(dist2, 0)), 0.0)
    sel = np.zeros((8, 16, 128), np.float32)
    for cc in range(16):
        for jj in range(8):
            sel[jj, cc, 8 * cc + jj] = 1.0
    return {"c_ucum8": ucum, "c_lrev8": lrev, "c_amask8": amask, "c_rowmask": rowmask,
            "c_ms": ms.astype(np.float32).reshape(128, 8 * 16 * 8), "c_mn": mn.astype(np.float32).reshape(128, 8 * 16 * 8),
            "c_sel": sel.reshape(8, 16 * 128)}


def phase_as(nc, din, dout, h1s_d, c_ident):
    xs = din("xs", [128, D])
    ck = din("ck", [NSS, 2048, 512])
    cv = din("cv", [NSS, 2048, 512])
    sg = din("sg", [NSS, 4, 64, 128])
    g1 = nc_input(nc, din, "g_norm1", [1, D])
    w_in = nc_input(nc, din, "w_in", [D, DIN])
    w_gk2 = nc_input(nc, din, "w_gk2", [16, 256])
    b_gk = nc_input(nc, din, "b_gk", [1, 256])
    g_gla = nc_input(nc, din, "g_gla_out", [1, 128])
    w_out = nc_input(nc, din, "w_out", [D, D])
    c_ucum = din("c_ucum8", [128, 128])
    c_lrev = din("c_lrev8", [128, 128])
    c_amask = din("c_amask8", [128, 128])
    c_rowmask = din("c_rowmask", [128, 16])
    c_ms = din("c_ms", [128, 1024])
    c_mn = din("c_mn", [128, 1024])
    c_sel = nc_input(nc, din, "c_sel", [8, 2048])
    o_swak = dout("o_swak_s", [128, 512])
    o_swav = dout("o_swav_s", [128, 512])
    o_glas = dout("o_glas_s", [NSS, 4, 64, 128])

    cx = Ctx(nc)
    S = cx.S
    ident_b = cx.sb([128, 128], BF16, "s_ident_b")
    ident_f = cx.sb([128, 128], F32, "s_ident_f")
    ucum = cx.sb([128, 128], F32, "s_ucum")
    lrev = cx.sb([128, 128], F32, "s_lrev")
    amask = cx.sb([128, 128], F32, "s_amask")
    rowmask = cx.sb([128, 16], F32, "s_rowmask")
    ms = cx.sb([128, 8, 128], BF16, "s_ms")
    mn = cx.sb([128, 8, 16, 8], BF16, "s_mn")
    sel = cx.sb([8, 16, 128], BF16, "s_sel")
    g1b = cx.sb([128, D], F32, "s_g1b")
    gglab = cx.sb([128, 128], F32, "s_gglab")
    win = cx.sb([128, 8, DIN], BF16, "s_win")
    wout = cx.sb([128, 8, D], BF16, "s_wout")
    wgk = cx.sb([32, 256], BF16, "s_wgk")
    gklrT = cx.sb([32, 128], BF16, "s_gklrT")
    S0f = cx.sb([128, NSS, 2, 128], F32, "s_S0f")
    S0b = cx.sb([128, NSS, 2, 128], BF16, "s_S0b")
    stg = cx.sb([128, 16, 512], BF16, "s_stg")
    kcT = [cx.sb([128, 4, 2048], BF16, f"s_kcT{i}") for i in range(1)]
    vca = [cx.sb([128, 16, 8, 65], BF16, f"s_vca{i}") for i in range(1)]
    Vs = cx.sb([128, 8, 65], BF16, "s_Vs")

    cx.dma("pool", ident_b.t[:], c_ident, writes=[ident_b.k])
    cx.dma("sp", ident_f.t[:], c_ident, writes=[ident_f.k])
    cx.dma("sp", ucum.t[:], c_ucum, writes=[ucum.k])
    cx.dma("sp", lrev.t[:], c_lrev, writes=[lrev.k])
    cx.dma("sp", amask.t[:], c_amask, writes=[amask.k])
    cx.dma("sp", rowmask.t[:], c_rowmask, writes=[rowmask.k])
    cx.dma("pool", ms.t[:].rearrange("p h x -> p (h x)"), c_ms, writes=[ms.k])
    cx.dma("pool", mn.t[:].rearrange("p h c j -> p (h c j)"), c_mn, writes=[mn.k])
    cx.dma("pool", sel.t[:].rearrange("p c t -> p (c t)"), c_sel, writes=[sel.k])
    cx.dma("sp", g1b.t[:], g1.partition_broadcast(128), writes=[g1b.k])
    cx.dma("sp", gglab.t[:], g_gla.partition_broadcast(128), writes=[gglab.k])
    w_in_v = w_in.rearrange("(k p) f -> p k f", p=128)
    for k in range(8):
        for hh in range(2):
            c0 = hh * 1544
            cx.dma("pool", win.t[:, k, c0:c0 + 1544], w_in_v[:, k, c0:c0 + 1544], writes=[win.k])
    w_out_v = w_out.rearrange("(k p) f -> p k f", p=128)
    for k in range(8):
        cx.dma("pool", wout.t[:, k, :], w_out_v[:, k, :], writes=[wout.k])
    cx.dma("pool", wgk.t[0:16, :], w_gk2, writes=[wgk.k])
    cx.dma("pool", wgk.t[16:17, :], b_gk, writes=[wgk.k])
    S.add("pool", lambda e: e.memset(gklrT.t[:], 1.0), (), [gklrT.k])
    S.add("pool", lambda e: e.memset(Vs.t[:], 1.0), (), [Vs.k])
    for v_ in vca:
        S.add("pool", lambda e, v_=v_: e.memset(v_.t[:], 1.0), (), [v_.k])
    cx.dma("sp", S0f.t[:], sg.rearrange("c (p h) k v -> (h k) c p v", h=2), writes=[S0f.k])
    cx.copy("act", S0b.t[:], S0f.t[:], [S0f.k], [S0b.k])

    psT = cx.ps([128, 1024], BF16, "s_psT")
    psR = [cx.ps([128, 512], F32, f"s_psR{i}") for i in range(2)]
    psSb = [cx.ps([128, 512], F32, f"s_psS{i}") for i in range(2)]
    psD = [cx.ps([128, 512], F32, f"s_psD{i}") for i in range(2)]
    psSel = cx.ps([128, 512], F32, "s_psSel")
    rr = [0]

    def next_ps():
        b = psR[rr[0] % 2]
        rr[0] += 1
        return b

    xb = cx.sb([128, D], F32, "s_x")
    junk = cx.sb([128, D], BF16, "s_junk")
    stb = cx.sb([128, 8], F32, "s_stat")
    nb = cx.sb([128, D], BF16, "s_n")
    nT = cx.sb([128, 8, 128], BF16, "s_nT")
    qkTa = cx.sb([128, 4, 128], BF16, "s_qkTa")
    qTb = cx.sb([128, 4, 2, 128], BF16, "s_qTb")
    kTs = cx.sb([128, 4, 128], BF16, "s_kTs")
    va = cx.sb([128, 512], BF16, "s_va")
    ka = cx.sb([128, 256], F32, "s_ka")
    sr = cx.sb([128, 512], F32, "s_sr")
    kbf = cx.sb([128, 512], F32, "s_kbf")
    vbf = cx.sb([128, 512], F32, "s_vbf")
    e1 = cx.sb([128, 256], F32, "s_e1")
    gg = cx.sb([128, 256], F32, "s_g")
    ebT = cx.sb([128, 2, 128], F32, "s_ebT")
    enbT = cx.sb([128, 2, 128], F32, "s_enbT")
    ed = cx.sb([128, 256], F32, "s_ed")
    qtT = cx.sb([128, 2, 2, 128], BF16, "s_qtT")
    ktT = cx.sb([128, 2, 128], BF16, "s_ktT")
    khat = cx.sb([128, 256], BF16, "s_khat")
    khm = [cx.sb([128, 256], BF16, f"s_khm{i}") for i in range(2)]
    Am = [cx.sb([128, 128], BF16, f"s_Am{i}") for i in range(4)]
    oTf = cx.sb([128, 4, 128], F32, "s_oTf")
    gs = cx.sb([128, 8], F32, "s_gs")
    gtmp = [cx.sb([128, 128], F32, f"s_gtmp{i}") for i in range(2)]
    oc = cx.sb([128, D], BF16, "s_ocat")
    Sn = [cx.sb([128, 2, 128], F32, f"s_Sn{i}") for i in range(2)]
    pe_t = [cx.sb([128, 136], BF16, f"s_pe{i}") for i in range(2)]
    pm_t = [cx.sb([128, 136], BF16, f"s_pm{i}") for i in range(2)]
    rden = [cx.sb([8, 1], F32, f"s_rden{i}") for i in range(2)]
    occ = [cx.sb([8, 512], BF16, f"s_occ{i}") for i in range(2)]
    oT = cx.sb([128, 8, 128], BF16, "s_oT")
    for b_ in (qTb, qtT):
        S.add("pool", lambda e, b_=b_: e.memset(b_.t[:], 0.0), (), [b_.k])

    def transpose8(src, dst):
        for k in range(8):
            cx.tr(psT.t[:, k * 128:(k + 1) * 128], src.t[:, k * 128:(k + 1) * 128], ident_b.t[:], [src.k, ident_b.k], [psT.k])
        cx.copy("act", dst.t[:].rearrange("p k t -> p (k t)"), psT.t[:], [psT.k], [dst.k])

    def proj_fm(col0, nchunk, ps, m=128):
        for j in range(nchunk):
            for k in range(8):
                cx.mm(ps.t[0:m, j * 128:(j + 1) * 128], win.t[:, k, col0 + j * 128: col0 + j * 128 + m], nT.t[:, k, :],
                      k == 0, k == 7, [win.k, nT.k], [ps.k])

    def proj_tm(col0, ncol, ps):
        for k in range(8):
            cx.mm(ps.t[:, 0:ncol], nT.t[:, k, :], win.t[:, k, col0:col0 + ncol], k == 0, k == 7, [win.k, nT.k], [ps.k])

    cx.dma("sp", xb.t[:], xs, writes=[xb.k])
    cx.act(junk.t[:], xb.t[:], AF.Square, [xb.k], [junk.k, stb.k], accum_out=stb.t[:, 0:1])
    cx.act(stb.t[:, 1:2], stb.t[:, 0:1], AF.Ln, [stb.k], [stb.k], scale=1.0 / D, bias=EPS)
    cx.act(stb.t[:, 2:3], stb.t[:, 1:2], AF.Exp, [stb.k], [stb.k], scale=-0.5)
    cx.stt("dve", nb.t[:], xb.t[:], stb.t[:, 2:3], g1b.t[:], ALU.mult, ALU.mult, [xb.k, stb.k, g1b.k], [nb.k])
    transpose8(nb, nT)
    ps = next_ps()
    proj_fm(C_QA, 4, ps)
    cx.copy("act", qkTa.t[:].rearrange("p k t -> p (k t)"), ps.t[:], [ps.k], [qkTa.k])
    ps = next_ps()
    proj_fm(C_QB, 4, ps)
    psv = ps.t[:].rearrange("p (k t) -> p k t", k=4)
    cx.copy("act", qTb.t[0:64, :, 0, :], psv[0:64], [ps.k], [qTb.k])
    cx.copy("act", qTb.t[64:128, :, 1, :], psv[64:128], [ps.k], [qTb.k])
    ps = next_ps()
    proj_fm(C_KB, 4, ps)
    cx.copy("dve", kTs.t[:].rearrange("p k t -> p (k t)"), ps.t[:], [ps.k], [kTs.k])
    ps = next_ps()
    proj_fm(C_GK, 1, ps, m=16)
    cx.copy("dve", gklrT.t[0:16, :], ps.t[0:16, 0:128], [ps.k], [gklrT.k])
    ps = next_ps()
    proj_tm(C_VA, 512, ps)
    cx.copy("act", va.t[:], ps.t[:], [ps.k], [va.k])
    ps = next_ps()
    proj_tm(C_KA, 256, ps)
    cx.copy("dve", ka.t[:], ps.t[:, 0:256], [ps.k], [ka.k])
    ps = next_ps()
    proj_tm(C_RA, 512, ps)
    cx.act(sr.t[:], ps.t[:], AF.Silu, [ps.k], [sr.k])
    ps = next_ps()
    proj_tm(C_KB, 512, ps)
    cx.copy("act", kbf.t[:], ps.t[:], [ps.k], [kbf.k])
    cx.dma("sp", o_swak, kbf.t[:], reads=[kbf.k], out_dram=True)
    ps = next_ps()
    proj_tm(C_VB, 512, ps)
    cx.copy("dve", Vs.t[:, :, 0:64], ps.t[:].rearrange("p (h d) -> p h d", h=8), [ps.k], [Vs.k])
    cx.copy("act", vbf.t[:], ps.t[:], [ps.k], [vbf.k])
    cx.dma("sp", o_swav, vbf.t[:], reads=[vbf.k], out_dram=True)

    ps = next_ps()
    cx.mm(ps.t[:, 0:256], gklrT.t[0:17, :], wgk.t[0:17, :], True, True, [gklrT.k, wgk.k], [ps.k])
    cx.act(e1.t[:], ps.t[:, 0:256], AF.Exp, [ps.k], [e1.k], scale=-1.0)
    cx.act(gg.t[:], e1.t[:], AF.Ln, [e1.k], [gg.k], bias=1.0, scale=1.0)
    ps = next_ps()
    for p in range(2):
        cx.mm(ps.t[:, p * 128:(p + 1) * 128], gg.t[:, p * 128:(p + 1) * 128], ucum.t[:], True, True, [gg.k, ucum.k], [ps.k])
    cx.mm(ps.t[:, 256:512], lrev.t[:], gg.t[:], True, True, [gg.k, lrev.k], [ps.k])
    cx.act(ebT.t[:].rearrange("p k t -> p (k t)"), ps.t[:, 0:256], AF.Exp, [ps.k], [ebT.k])
    cx.act(enbT.t[:].rearrange("p k t -> p (k t)"), ps.t[:, 0:256], AF.Exp, [ps.k], [enbT.k], scale=-1.0)
    cx.act(ed.t[:], ps.t[:, 256:512], AF.Exp, [ps.k], [ed.k])
    for hh in range(2):
        pr = slice(hh * 64, hh * 64 + 64)
        cx.stt("dve", qtT.t[pr, :, hh, :], qkTa.t[pr, 0:2, :], 0.125, ebT.t[pr], ALU.mult, ALU.mult, [qkTa.k, ebT.k], [qtT.k])
    cx.tt("dve", ktT.t[:], qkTa.t[:, 2:4, :], enbT.t[:], ALU.mult, [qkTa.k, enbT.k], [ktT.k])
    cx.tt("dve", khat.t[:], ka.t[:], ed.t[:], ALU.mult, [ka.k, ed.k], [khat.k])
    for h in range(4):
        p = h // 2
        pss = psSb[h % 2]
        cx.mm(pss.t[:, 0:128], ktT.t[:, p, :], qtT.t[:, p, h % 2, :], True, True, [ktT.k, qtT.k], [pss.k])
        cx.tt("dve", Am[h].t[:], pss.t[:, 0:128], amask.t[:], ALU.mult, [pss.k, amask.k], [Am[h].k])
    psO = psD[0]
    for h in range(4):
        p = h // 2
        cx.mm(psO.t[:, h * 128:(h + 1) * 128], va.t[:, h * 128:(h + 1) * 128], Am[h].t[:], True, False, [va.k, Am[h].k], [psO.k])
        for c in range(NSS):
            cx.mm(psO.t[:, h * 128 + 8 * c:h * 128 + 8 * c + 8], S0b.t[:, c, p, :], qtT.t[:, p, h % 2, 8 * c:8 * c + 8], False, c == NSS - 1,
                  [S0b.k, qtT.k], [psO.k])
    cx.copy("act", oTf.t[:].rearrange("p h t -> p (h t)"), psO.t[:], [psO.k], [oTf.k])
    psGO = psD[1]
    for h in range(4):
        cx.tr(psGO.t[:, h * 128:(h + 1) * 128], oTf.t[:, h, :], ident_f.t[:], [oTf.k, ident_f.k], [psGO.k])
    for h in range(4):
        cx.act(junk.t[:, 0:128], psGO.t[:, h * 128:(h + 1) * 128], AF.Square, [psGO.k], [junk.k, gs.k], accum_out=gs.t[:, h:h + 1])
    cx.act(gs.t[:, 4:8], gs.t[:, 0:4], AF.Ln, [gs.k], [gs.k], scale=1.0 / 128, bias=EPS)
    cx.act(gs.t[:, 0:4], gs.t[:, 4:8], AF.Exp, [gs.k], [gs.k], scale=-0.5)
    for h in range(4):
        gt_ = gtmp[h % 2]
        cx.stt("dve", gt_.t[:], psGO.t[:, h * 128:(h + 1) * 128], gs.t[:, h:h + 1], gglab.t[:], ALU.mult, ALU.mult, [psGO.k, gs.k, gglab.k], [gt_.k])
        cx.tt("dve", oc.t[:, h * 128:(h + 1) * 128], gt_.t[:], sr.t[:, h * 128:(h + 1) * 128], ALU.mult, [gt_.k, sr.k], [oc.k])
    for c in range(NSS):
        km = khm[c % 2]
        cx.ts("dve", km.t[:], khat.t[:], rowmask.t[:, c:c + 1], None, ALU.mult, None, [khat.k, rowmask.k], [km.k])
        psU = next_ps()
        for h in range(4):
            p, base = h // 2, (h % 2) * 64
            cx.mm(psU.t[base:base + 64, p * 128:(p + 1) * 128], km.t[:, h * 64:(h + 1) * 64], va.t[:, h * 128:(h + 1) * 128], True, True,
                  [km.k, va.k], [psU.k])
        sn = Sn[c % 2]
        for p in range(2):
            cx.stt("dve", sn.t[:, p, :], S0f.t[:, c, p, :], ebT.t[:, p, 8 * c + 7:8 * c + 8], psU.t[:, p * 128:(p + 1) * 128],
                   ALU.mult, ALU.add, [S0f.k, ebT.k, psU.k], [sn.k])
        cx.dma("sp", o_glas[c].rearrange("(p h) k v -> (h k) p v", h=2), sn.t[:], reads=[sn.k], out_dram=True)

    it = 0
    for c in range(NSS):
        kT_, va_ = kcT[0], vca[0]
        cx.dma("pool", stg.t[:], ck[c].rearrange("(b p) f -> p b f", p=128), writes=[stg.k])
        for kb in range(16):
            for c4 in range(4):
                cx.tr(psT.t[:, c4 * 128:(c4 + 1) * 128], stg.t[:, kb, c4 * 128:(c4 + 1) * 128], ident_b.t[:], [stg.k, ident_b.k], [psT.k])
            cx.copy("act" if kb % 2 else "dve", kT_.t[:, :, kb * 128:(kb + 1) * 128], psT.t[:, 0:512].rearrange("p (k t) -> p k t", k=4), [psT.k], [kT_.k])
        cx.dma("pool", stg.t[:], cv[c].rearrange("(b p) f -> p b f", p=128), writes=[stg.k])
        cx.copy("act", va_.t[:, :, :, 0:64], stg.t[:].rearrange("p b (h d) -> p b h d", h=8), [stg.k], [va_.k])
        ocb = occ[c % 2]
        for h in range(8):
            c4, par = h // 2, h % 2
            pss, pd = psSb[it % 2], psD[it % 2]
            pe_, pm_, rd = pe_t[it % 2], pm_t[it % 2], rden[it % 2]
            it += 1
            rhs = qTb.t[:, c4, par, 8 * c:8 * c + 8]
            for kb in range(16):
                cx.mm(pss.t[:, kb * 8:(kb + 1) * 8], kT_.t[:, c4, kb * 128:(kb + 1) * 128], rhs, True, True, [kT_.k, qTb.k], [pss.k])
            cx.mm(pss.t[:, 128:136], kTs.t[:, c4, :], rhs, True, True, [kTs.k, qTb.k], [pss.k])
            cx.act(pe_.t[:], pss.t[:, 0:136], AF.Exp, [pss.k], [pe_.k], scale=0.125)
            cx.tt("dve", pm_.t[:, 0:128], pe_.t[:, 0:128], ms.t[:, h, :], ALU.mult, [pe_.k, ms.k], [pm_.k])
            cx.tt("dve", pm_.t[:, 128:136], pe_.t[:, 128:136], mn.t[:, h, c, :], ALU.mult, [pe_.k, mn.k], [pm_.k])
            for kb in range(16):
                cx.mm(pd.t[0:8, 0:65], pm_.t[:, kb * 8:(kb + 1) * 8], va_.t[:, kb, h, :], kb == 0, False, [pm_.k, va_.k], [pd.k])
            cx.mm(pd.t[0:8, 0:65], pm_.t[:, 128:136], Vs.t[:, h, :], False, True, [pm_.k, Vs.k], [pd.k])
            S.add("dve", lambda e, rd=rd, pd=pd: e.reciprocal(out=rd.t[:], in_=pd.t[0:8, 64:65]), [pd.k], [rd.k])
            cx.ts("dve", ocb.t[:, h * 64:(h + 1) * 64], pd.t[0:8, 0:64], rd.t[:], None, ALU.mult, None, [pd.k, rd.k], [ocb.k])
        cx.mm(psSel.t[:], sel.t[:, c, :], ocb.t[:], c == 0, c == NSS - 1, [sel.k, ocb.k], [psSel.k])
    cx.copy("act", oc.t[:, 512:1024], psSel.t[:], [psSel.k], [oc.k])

    transpose8(oc, oT)
    for half in range(2):
        ps = next_ps()
        for k in range(8):
            cx.mm(ps.t[:], oT.t[:, k, :], wout.t[:, k, half * 512:(half + 1) * 512], k == 0, k == 7, [oT.k, wout.k], [ps.k])
        cx.tt("dve", xb.t[:, half * 512:(half + 1) * 512], ps.t[:], xb.t[:, half * 512:(half + 1) * 512], ALU.add, [ps.k, xb.k], [xb.k])
    cx.dma("sp", h1s_d, xb.t[:], reads=[xb.k], out_dram=True)
    cx.close()


_NC_CACHE = {}


def kernel(**inputs):
    n = 8
    if "nc" not in _NC_CACHE:
        _NC_CACHE["nc"] = build(NT=NT_FULL, nseq=NSEQ, debug=False, phases="ASBC")
    nc = _NC_CACHE["nc"]
    f32 = lambda a: np.ascontiguousarray(np.asarray(a, dtype=np.float32))
    I = {k: np.asarray(v) for k, v in inputs.items()}
    shared = {
        "g_norm1": f32(I["g_norm1"]), "w_in": f32(I["w_in"][0]), "w_gk2": f32(I["w_gk2"][0]),
        "b_gk": f32(I["b_gk"]), "g_gla_out": f32(I["g_gla_out"]), "w_out": f32(I["w_out"][0]),
        "g_norm2": f32(I["g_norm2"]), "g_mem": f32(I["g_mem"]), "g_norm3": f32(I["g_norm3"]),
        "w_cq": f32(I["w_cq"][0]), "w_mk": f32(I["w_mk"][0]), "w_mv": f32(I["w_mv"][0]), "w_co": f32(I["w_co"][0]),
        "w_r": f32(np.concatenate([I["w_gr"][0], I["w_er"][0]], axis=1)),
        "b_r": f32(np.concatenate([I["b_gr"], I["b_er"]], axis=1)),
        "w_e1": f32(I["w_e1"][0]).reshape(32 * D, 512), "w_e3": f32(I["w_e3"][0]).reshape(32 * D, 512),
        "w_e2": f32(I["w_e2"][0]).reshape(32 * 512, D), "g_final": f32(I["g_final"]).reshape(1, D),
    }
    shared.update(host_consts())
    shared.update(host_consts_b())
    shared.update(host_consts_c())
    shared.update(host_consts_s())
    in_maps = []
    for i in range(n):
        m = dict(shared)
        ps, ss = slice(NSEQ * i, NSEQ * (i + 1)), slice(NSS * i, NSS * (i + 1))
        m["xp"] = f32(I["x_prompt"][ps])
        m["memp"] = f32(I["mem_prompt"][ps])
        m["xs"] = f32(I["x_sample"][ss]).reshape(128, D)
        m["ck"] = f32(I["cache_swa_k"][0, ss]).reshape(NSS, 2048, 512)
        m["cv"] = f32(I["cache_swa_v"][0, ss]).reshape(NSS, 2048, 512)
        m["sg"] = f32(I["state_gla"][0, ss])
        m["cmk"] = f32(I["cache_mem_k"][0, ss]).reshape(NSS, 256, D)
        m["cmv"] = f32(I["cache_mem_v"][0, ss]).reshape(NSS, 256, D)
        in_maps.append(m)
    res = run_bass_kernel_spmd(nc, in_maps, core_ids=list(range(n)))
    rs = res.results
    npr = NSEQ * NT_FULL * 128
    cat = lambda key: np.concatenate([np.asarray(r[key], dtype=np.float32) for r in rs], axis=0)
    y_prompt = np.concatenate([np.asarray(r["y"][:npr], dtype=np.float32).reshape(NSEQ, NT_FULL * 128, D) for r in rs], axis=0)
    y_sample = np.concatenate([np.asarray(r["y"][npr:npr + 128], dtype=np.float32).reshape(NSS, 8, D) for r in rs], axis=0)
    return (y_prompt, y_sample,
            cat("o_swak").reshape(1, 16, 2048, 8, 64), cat("o_swav").reshape(1, 16, 2048, 8, 64),
            cat("o_glas").reshape(1, 16, 4, 64, 128),
            cat("o_memk").reshape(1, 16, 256, 4, 256), cat("o_memv").reshape(1, 16, 256, 4, 256),
            cat("o_swak_s").reshape(1, 128, 8, 8, 64), cat("o_swav_s").reshape(1, 128, 8, 8, 64),
            cat("o_glas_s").reshape(1, 128, 4, 64, 128))
```

```python
from contextlib import ExitStack
import numpy as np
import concourse.bass as bass
import concourse.mybir as mybir
from concourse.bass_utils import run_bass_kernel_spmd

F32 = mybir.dt.float32
BF16 = mybir.dt.bfloat16
I32 = mybir.dt.int32
U32 = mybir.dt.uint32
AF = mybir.ActivationFunctionType
ALU = mybir.AluOpType
AX = mybir.AxisListType

ENGS = ("pe", "act", "dve", "pool", "sp")
SEM_LIMIT = 30000
DMA_RING = 8


class Tok:
    __slots__ = ("w", "rs", "name", "excl")

    def __init__(self, name="", excl=False):
        self.w = None
        self.rs = []
        self.name = name
        self.excl = excl


class Op:
    __slots__ = ("eng", "fn", "dma", "deps", "signal", "sem", "val", "gate")

    def __init__(self, eng, fn, dma):
        self.eng = eng
        self.fn = fn
        self.dma = dma
        self.deps = []
        self.signal = dma
        self.sem = None
        self.val = None
        self.gate = None


class Sched:
    def __init__(self, nc):
        self.nc = nc
        self.ops = {e: [] for e in ENGS}
        self.dma_ops = {e: [] for e in ENGS}
        self.out_dmas = []
        import os
        self.cut = int(os.environ.get("KCUT", "0")) or None
        self.total = 0

    def add(self, eng, fn, reads=(), writes=(), dma=False, out=False):
        self.total += 1
        if self.cut is not None and self.total > self.cut:
            return None
        op = Op(eng, fn, dma)
        deps = []
        ex = [t for t in reads if t.excl]
        if ex:
            reads = [t for t in reads if not t.excl]
            writes = list(writes) + [t for t in ex if t not in writes]
        for t in reads:
            if t.w is not None:
                deps.append(t.w)
        for t in writes:
            deps.extend(t.rs)
            if t.w is not None:
                deps.append(t.w)
        seen = set()
        flat = []
        for d in deps:
            if d.fn is None:
                flat.extend(d.deps)
            else:
                flat.append(d)
        for d in flat:
            if id(d) in seen:
                continue
            seen.add(id(d))
            if fn is None or d.dma or d.eng != eng or eng != "pe":
                op.deps.append(d)
                d.signal = True
        for t in reads:
            t.rs.append(op)
        for t in writes:
            t.w = op
            t.rs = []
        if dma:
            lst = self.dma_ops[eng]
            if len(lst) >= DMA_RING:
                op.gate = lst[len(lst) - DMA_RING]
            lst.append(op)
            if out:
                self.out_dmas.append(op)
        if fn is not None:
            self.ops[eng].append(op)
        return op

    def finish(self):
        op = Op("sp", None, False)
        op.deps = list(self.out_dmas)
        self.ops["sp"].append(op)

    def emit(self):
        nc = self.nc
        with ExitStack() as st:
            nsig = {e: sum(1 for o in self.ops[e] if o.signal and not o.dma) for e in ENGS}
            esems = {}
            for e in ENGS:
                k = nsig[e] // SEM_LIMIT + 1
                esems[e] = [st.enter_context(nc.semaphore(f"s_{e}_{i}")) for i in range(k)]
            dsems = {}
            for e in ENGS:
                if self.dma_ops[e]:
                    dsems[e] = [st.enter_context(nc.semaphore(f"d_{e}_{i}")) for i in range(DMA_RING)]
            for e in ENGS:
                c = 0
                for o in self.ops[e]:
                    if o.dma:
                        continue
                    if o.signal:
                        o.sem = esems[e][c // SEM_LIMIT]
                        o.val = c % SEM_LIMIT + 1
                        c += 1
                for n, o in enumerate(self.dma_ops[e]):
                    o.sem = dsems[e][n % DMA_RING]
                    o.val = 16 * (n // DMA_RING + 1)
            block = st.enter_context(nc.Block())

            def run(e, name):
                waited = {}
                for o in self.ops[name]:
                    ds = list(o.deps)
                    if o.gate is not None:
                        ds.append(o.gate)
                    for d in ds:
                        k = id(d.sem)
                        if waited.get(k, 0) >= d.val:
                            continue
                        waited[k] = d.val
                        e.wait_ge(d.sem, d.val)
                    if o.fn is None:
                        continue
                    ins = o.fn(e)
                    if o.signal:
                        ins.then_inc(o.sem, 16 if o.dma else 1)

            @block.tensor
            def _(e):
                run(e, "pe")

            @block.scalar
            def _(e):
                run(e, "act")

            @block.vector
            def _(e):
                run(e, "dve")

            @block.gpsimd
            def _(e):
                run(e, "pool")

            @block.sync
            def _(e):
                run(e, "sp")


class Buf:
    def __init__(self, t, name=""):
        self.t = t
        self.k = Tok(name)


class Ctx:
    def __init__(self, nc):
        self.nc = nc
        self.st = ExitStack()
        self.S = Sched(nc)
        self.n = 0

    def sb(self, shape, dt, name=None):
        self.n += 1
        name = name or f"sb{self.n}"
        return Buf(self.st.enter_context(self.nc.sbuf_tensor(name, list(shape), dt)), name)

    def ps(self, shape, dt, name=None):
        self.n += 1
        name = name or f"ps{self.n}"
        b = Buf(self.st.enter_context(self.nc.psum_tensor(name, list(shape), dt)), name)
        b.k.excl = True
        return b

    def dma(self, q, out, in_, reads=(), writes=(), out_dram=False, **kw):
        return self.S.add(q, lambda e: e.dma_start(out=out, in_=in_, **kw), reads, writes, dma=True, out=out_dram)

    def mm(self, out, lhsT, rhs, start, stop, reads, writes):
        return self.S.add("pe", lambda e: e.matmul(out, lhsT=lhsT, rhs=rhs, start=start, stop=stop), reads, writes)

    def tr(self, out, in_, ident, reads, writes):
        return self.S.add("pe", lambda e: e.transpose(out, in_, ident), reads, writes)

    def act(self, out, in_, func, reads, writes, **kw):
        return self.S.add("act", lambda e: e.activation(out=out, in_=in_, func=func, **kw), reads, writes)

    def copy(self, eng, out, in_, reads, writes):
        if eng == "act":
            return self.S.add("act", lambda e: e.copy(out=out, in_=in_), reads, writes)
        return self.S.add(eng, lambda e: e.tensor_copy(out=out, in_=in_), reads, writes)

    def tt(self, eng, out, in0, in1, op, reads, writes):
        return self.S.add(eng, lambda e: e.tensor_tensor(out=out, in0=in0, in1=in1, op=op), reads, writes)

    def ts(self, eng, out, in0, s1, s2, op0, op1, reads, writes, **kw):
        if s2 is None:
            return self.S.add(eng, lambda e: e.tensor_scalar(out=out, in0=in0, scalar1=s1, scalar2=None, op0=op0, **kw), reads, writes)
        return self.S.add(eng, lambda e: e.tensor_scalar(out=out, in0=in0, scalar1=s1, scalar2=s2, op0=op0, op1=op1, **kw), reads, writes)

    def stt(self, eng, out, in0, scalar, in1, op0, op1, reads, writes):
        return self.S.add(eng, lambda e: e.scalar_tensor_tensor(out=out, in0=in0, scalar=scalar, in1=in1, op0=op0, op1=op1), reads, writes)

    def close(self):
        self.S.finish()
        self.S.emit()
        self.st.close()


def interleave(gens, width=2):
    active = []
    for g in gens:
        active.append(g)
        while len(active) >= width:
            for a in list(active):
                try:
                    next(a)
                except StopIteration:
                    active.remove(a)
    while active:
        for a in list(active):
            try:
                next(a)
            except StopIteration:
                active.remove(a)

D = 1024
DIN = 3088
NT_FULL = 32
NSEQ = 2
NSS = 16
NR = 18
EPS = 1e-6
C_QA, C_KA, C_VA, C_GK, C_RA, C_QB, C_KB, C_VB = 0, 256, 512, 1024, 1040, 1552, 2064, 2576


def host_consts():
    t = np.arange(128)
    ch = t // 64
    same = ch[:, None] == ch[None, :]
    ucum = np.where(same & (t[:, None] <= t[None, :]), -1.0 / 16, 0.0).astype(np.float32)
    lrev = np.where(same & (t[:, None] > t[None, :]), -1.0 / 16, 0.0).astype(np.float32)
    amask = np.where(same & (t[:, None] <= t[None, :]), 1.0, 0.0).astype(np.float32)
    dm = np.zeros((17, 128, 128), np.float32)
    for db in range(17):
        dist = db * 128 + t[None, :] - t[:, None]
        m = ((dist >= 0) & (dist <= 128)).astype(np.float32)
        m += ((dist >= 0) & (dist <= 512) & (dist % 4 == 0))
        m += ((dist >= 0) & (dist <= 2048) & (dist % 16 == 0))
        dm[db] = m
    slopes = np.array([2.0 ** (-8.0 * (h + 1) / 8) for h in range(8)], np.float64)
    ab = np.zeros((128, 8, 17), np.float32)
    for h in range(8):
        for db in range(17):
            ab[:, h, db] = slopes[h] * (t - 64 - 128 * db)
    dmf = np.zeros((128, 8, 17, 128), np.float32)
    for db in range(17):
        dist = db * 128 + t[None, :] - t[:, None]
        for h in range(8):
            dmf[:, h, 16 - db, :] = dm[db] * np.exp(-slopes[h] * np.maximum(dist, 0))
    return {
        "c_ident": np.eye(128, dtype=np.float32),
        "c_ucum": ucum, "c_lrev": lrev, "c_amask": amask,
        "c_dmf": dmf.reshape(128, 8 * 17 * 128),
    }


def build(NT=NT_FULL, nseq=NSEQ, debug=False, phases="AB"):
    nc = bass.Bass("TRN2", target_bir_lowering=False)

    def din(name, shape, dt=F32):
        return nc.dram_tensor(name, list(shape), dt, kind="ExternalInput").ap()

    def dout(name, shape, dt=F32):
        return nc.dram_tensor(name, list(shape), dt, kind="ExternalOutput").ap()

    T = NT * 128
    c_ident = din("c_ident", [128, 128])
    samp = "S" in phases
    if "A" in phases:
        h1_d = dout("h1", [nseq, T, D]) if debug else nc.dram_tensor("h1", [nseq, T, D], F32).ap()
        phase_a(nc, din, dout, NT, nseq, h1_d, c_ident)
    else:
        h1_d = din("h1", [nseq, T, D])
    h1s_d = None
    if samp:
        h1s_d = dout("h1s", [128, D]) if debug else nc.dram_tensor("h1s", [128, D], F32).ap()
        phase_as(nc, din, dout, h1s_d, c_ident)
    NTOT = nseq * NT + (1 if samp else 0)
    if "B" in phases:
        h2_d, xn_d, route_d, cnt_d = phase_b(nc, din, dout, NT, nseq, h1_d, c_ident, debug, h1s_d)
    elif "C" in phases:
        h2_d = din("h2", [NTOT * 128, D])
        xn_d = din("xn", [NTOT * 128, D], BF16)
        route_d = din("route", [128, NTOT, 8])
        cnt_d = din("cnt", [128, 32])
    if "C" in phases:
        y_d = dout("y", [NTOT * 128, D])
        phase_c(nc, din, dout, NTOT, h2_d, xn_d, route_d, cnt_d, c_ident, y_d)
    return nc


def phase_a(nc, din, dout, NT, nseq, h1_d, c_ident):
    T = NT * 128
    KEEP = min(2048, T)
    KT0 = NT - KEEP // 128
    xp = din("xp", [nseq, T, D])
    g1 = nc_input(nc, din, "g_norm1", [1, D])
    w_in = nc_input(nc, din, "w_in", [D, DIN])
    w_gk2 = nc_input(nc, din, "w_gk2", [16, 256])
    b_gk = nc_input(nc, din, "b_gk", [1, 256])
    g_gla = nc_input(nc, din, "g_gla_out", [1, 128])
    w_out = nc_input(nc, din, "w_out", [D, D])
    c_ucum = din("c_ucum", [128, 128])
    c_lrev = din("c_lrev", [128, 128])
    c_amask = din("c_amask", [128, 128])
    c_dmf = din("c_dmf", [128, 8 * 17 * 128])

    o_swak = dout("o_swak", [nseq, KEEP, 512])
    o_swav = dout("o_swav", [nseq, KEEP, 512])
    o_glas = dout("o_glas", [nseq, 4, 64, 128])
    cx = Ctx(nc)
    S = cx.S
    ident_b = cx.sb([128, 128], BF16, "ident_b")
    ucum = cx.sb([128, 128], F32, "ucum")
    lrev = cx.sb([128, 128], F32, "lrev")
    amask = cx.sb([128, 128], F32, "amask")
    dmf = cx.sb([128, 8, 17, 128], BF16, "dmf")
    g1b = cx.sb([128, D], F32, "g1b")
    gglab = cx.sb([128, 128], F32, "gglab")
    win = cx.sb([128, 8, DIN], BF16, "win")
    wout = cx.sb([128, 8, D], BF16, "wout")
    wgk = cx.sb([32, 256], BF16, "wgk")
    kTr = cx.sb([128, 4, NR * 128], BF16, "kTr")
    Vr = cx.sb([128, NR, 8, 65], BF16, "Vr")
    gklrT2 = [cx.sb([32, 128], BF16, f"gklrT{i}") for i in range(2)]
    Sst = cx.sb([128, 2, 128], F32, "Sst")
    Sb = [cx.sb([128, 2, 128], BF16, f"Sb{i}") for i in range(8)]

    cx.dma("pool", ident_b.t[:], c_ident, writes=[ident_b.k])
    cx.dma("sp", ucum.t[:], c_ucum, writes=[ucum.k])
    cx.dma("sp", lrev.t[:], c_lrev, writes=[lrev.k])
    cx.dma("sp", amask.t[:], c_amask, writes=[amask.k])
    c_dmf_v = c_dmf.rearrange("p (h r q) -> p h r q", h=8, r=17)
    for h in range(8):
        cx.dma("pool", dmf.t[:, h, 0:9, :], c_dmf_v[:, h, 0:9, :], writes=[dmf.k])
        cx.dma("pool", dmf.t[:, h, 9:17, :], c_dmf_v[:, h, 9:17, :], writes=[dmf.k])
    cx.dma("sp", g1b.t[:], g1.partition_broadcast(128), writes=[g1b.k])
    cx.dma("sp", gglab.t[:], g_gla.partition_broadcast(128), writes=[gglab.k])
    w_in_v = w_in.rearrange("(k p) f -> p k f", p=128)
    for k in range(8):
        for hh in range(2):
            c0 = hh * 1544
            cx.dma("pool", win.t[:, k, c0:c0 + 1544], w_in_v[:, k, c0:c0 + 1544], writes=[win.k])
    w_out_v = w_out.rearrange("(k p) f -> p k f", p=128)
    for k in range(8):
        cx.dma("pool", wout.t[:, k, :], w_out_v[:, k, :], writes=[wout.k])
    cx.dma("pool", wgk.t[0:16, :], w_gk2, writes=[wgk.k])
    cx.dma("pool", wgk.t[16:17, :], b_gk, writes=[wgk.k])
    for g_ in gklrT2:
        S.add("pool", lambda e, g_=g_: e.memset(g_.t[:], 1.0), (), [g_.k])
    S.add("pool", lambda e: e.memset(Vr.t[:], 1.0), (), [Vr.k])
    S.add("pool", lambda e: e.memset(kTr.t[:], 0.0), (), [kTr.k])

    kT_k = [Tok(f"kT{i}") for i in range(NR)]
    V_k = [Tok(f"V{i}") for i in range(NR)]
    psT = cx.ps([128, 1024], BF16, "psT")
    psR = [cx.ps([128, 512], F32, f"psR{i}") for i in range(2)]
    psSb = [cx.ps([128, 512], F32, f"psS{i}") for i in range(2)]
    psD = [cx.ps([128, 512], F32, f"psD{i}") for i in range(2)]
    psGO = cx.ps([128, 4, 128], F32, "psGO")
    rr = [0]

    def next_ps():
        b = psR[rr[0] % 2]
        rr[0] += 1
        return b

    def rot(n, shape, dt, name):
        return [cx.sb(shape, dt, f"{name}{i}") for i in range(n)]

    x_t = rot(2, [128, D], F32, "x")
    junk = cx.sb([128, D], BF16, "junk")
    st_t = rot(2, [128, 8], F32, "stat")
    n_t = rot(1, [128, D], BF16, "n") * 2
    nT_t = rot(2, [128, 8, 128], BF16, "nT")
    qkTa = rot(2, [128, 4, 128], BF16, "qkTa")
    qTb = rot(2, [128, 4, 2, 128], BF16, "qTb")
    va_t = rot(2, [128, 512], BF16, "va")
    ka_t = rot(2, [128, 256], F32, "ka")
    sr_t = rot(2, [128, 512], BF16, "sr")
    kbf = rot(1, [128, 512], F32, "kbf") * 2
    vbf = rot(1, [128, 512], F32, "vbf") * 2
    e1_t = rot(1, [128, 256], F32, "e1") * 2
    g_t = rot(2, [128, 256], F32, "g")
    ebT = rot(2, [128, 2, 128], F32, "ebT")
    enbT = rot(2, [128, 2, 128], F32, "enbT")
    ed_t = rot(2, [128, 256], F32, "ed")
    qtT = rot(2, [128, 2, 2, 128], BF16, "qtT")
    ktT = rot(2, [128, 2, 128], BF16, "ktT")
    khat = rot(2, [128, 2, 256], BF16, "khat")
    Am = rot(8, [128, 128], BF16, "Am")
    gst = rot(2, [128, 8], F32, "gst")
    gtmp = rot(2, [128, 128], F32, "gtmp")
    ocat = rot(2, [128, D], BF16, "ocat")
    pe_t = rot(2, [128, 512], BF16, "pe")
    pm_t = rot(2, [128, 512], BF16, "pm")
    rden = rot(4, [128, 1], F32, "rden")
    oT_t = rot(1, [128, 8, 128], BF16, "oT") * 2
    cnt = {"s": 0, "a": 0, "p": 0, "r": 0}
    for b_ in qTb + qtT + khat:
        S.add("pool", lambda e, b_=b_: e.memset(b_.t[:], 0.0), (), [b_.k])

    def rmsnorm_to_bf16(xb, gb, nb, stb):
        cx.act(junk.t[:], xb.t[:], AF.Square, [xb.k], [junk.k, stb.k], accum_out=stb.t[:, 0:1])
        cx.act(stb.t[:, 1:2], stb.t[:, 0:1], AF.Ln, [stb.k], [stb.k], scale=1.0 / D, bias=EPS)
        cx.act(stb.t[:, 2:3], stb.t[:, 1:2], AF.Exp, [stb.k], [stb.k], scale=-0.5)
        cx.stt("dve", nb.t[:], xb.t[:], stb.t[:, 2:3], gb.t[:], ALU.mult, ALU.mult, [xb.k, stb.k, gb.k], [nb.k])

    def transpose8(src, dst):
        for k in range(8):
            cx.tr(psT.t[:, k * 128:(k + 1) * 128], src.t[:, k * 128:(k + 1) * 128], ident_b.t[:], [src.k, ident_b.k], [psT.k])
        cx.copy("act", dst.t[:].rearrange("p k t -> p (k t)"), psT.t[:], [psT.k], [dst.k])

    def proj_fm(nT, col0, nchunk, ps, m=128):
        for j in range(nchunk):
            for k in range(8):
                cx.mm(ps.t[0:m, j * 128:(j + 1) * 128], win.t[:, k, col0 + j * 128: col0 + j * 128 + m], nT.t[:, k, :],
                      k == 0, k == 7, [win.k, nT.k], [ps.k])

    def proj_tm(nT, col0, ncol, ps):
        for k in range(8):
            cx.mm(ps.t[:, 0:ncol], nT.t[:, k, :], win.t[:, k, col0:col0 + ncol], k == 0, k == 7, [win.k, nT.k], [ps.k])

    sbi = [0]

    def tile_gen(s, qb):
        i2 = qb % 2
        gklrT = gklrT2[i2]
        xb, nb, nT, stb = x_t[i2], n_t[i2], nT_t[i2], st_t[i2]
        cx.dma("sp", xb.t[:], xp[s, qb * 128:(qb + 1) * 128, :], writes=[xb.k])
        rmsnorm_to_bf16(xb, g1b, nb, stb)
        transpose8(nb, nT)
        slot = qb % NR
        ps = next_ps()
        proj_fm(nT, C_QA, 4, ps)
        cx.copy("act", qkTa[i2].t[:].rearrange("p k t -> p (k t)"), ps.t[:], [ps.k], [qkTa[i2].k])
        ps = next_ps()
        proj_fm(nT, C_QB, 4, ps)
        psv = ps.t[:].rearrange("p (k t) -> p k t", k=4)
        cx.copy("act", qTb[i2].t[0:64, :, 0, :], psv[0:64], [ps.k], [qTb[i2].k])
        cx.copy("act", qTb[i2].t[64:128, :, 1, :], psv[64:128], [ps.k], [qTb[i2].k])
        ps = next_ps()
        proj_fm(nT, C_KB, 4, ps)
        cx.copy("dve", kTr.t[:, :, slot * 128:(slot + 1) * 128], ps.t[:].rearrange("p (k t) -> p k t", k=4), [ps.k, kTr.k], [kT_k[slot]])
        ps = next_ps()
        proj_fm(nT, C_GK, 1, ps, m=16)
        cx.copy("dve", gklrT.t[0:16, :], ps.t[0:16, 0:128], [ps.k], [gklrT.k])
        yield
        ps = next_ps()
        proj_tm(nT, C_VA, 512, ps)
        cx.copy("act", va_t[i2].t[:], ps.t[:], [ps.k], [va_t[i2].k])
        ps = next_ps()
        proj_tm(nT, C_KA, 256, ps)
        cx.copy("dve", ka_t[i2].t[:], ps.t[:, 0:256], [ps.k], [ka_t[i2].k])
        ps = next_ps()
        proj_tm(nT, C_RA, 512, ps)
        cx.act(sr_t[i2].t[:], ps.t[:], AF.Silu, [ps.k], [sr_t[i2].k])
        if qb >= KT0:
            ps = next_ps()
            proj_tm(nT, C_KB, 512, ps)
            cx.copy("act", kbf[i2].t[:], ps.t[:], [ps.k], [kbf[i2].k])
            cx.dma("sp", o_swak[s, (qb - KT0) * 128:(qb - KT0 + 1) * 128, :], kbf[i2].t[:], reads=[kbf[i2].k], out_dram=True)
        ps = next_ps()
        proj_tm(nT, C_VB, 512, ps)
        cx.copy("dve", Vr.t[:, slot, :, 0:64], ps.t[:].rearrange("p (h d) -> p h d", h=8), [ps.k, Vr.k], [V_k[slot]])
        if qb >= KT0:
            cx.copy("act", vbf[i2].t[:], ps.t[:], [ps.k], [vbf[i2].k])
            cx.dma("sp", o_swav[s, (qb - KT0) * 128:(qb - KT0 + 1) * 128, :], vbf[i2].t[:], reads=[vbf[i2].k], out_dram=True)

        yield
        if qb == 0:
            S.add("dve", lambda e: e.memset(Sst.t[:], 0.0), (), [Sst.k])
            sbi[0] = 0
            S.add("pool", lambda e: e.memset(Sb[0].t[:], 0.0), (), [Sb[0].k])
        ps = next_ps()
        cx.mm(ps.t[:, 0:256], gklrT.t[0:17, :], wgk.t[0:17, :], True, True, [gklrT.k, wgk.k], [ps.k])
        cx.act(e1_t[i2].t[:], ps.t[:, 0:256], AF.Exp, [ps.k], [e1_t[i2].k], scale=-1.0)
        cx.act(g_t[i2].t[:], e1_t[i2].t[:], AF.Ln, [e1_t[i2].k], [g_t[i2].k], bias=1.0, scale=1.0)
        gb_ = g_t[i2]
        ps = next_ps()
        for p in range(2):
            cx.mm(ps.t[:, p * 128:(p + 1) * 128], gb_.t[:, p * 128:(p + 1) * 128], ucum.t[:], True, True, [gb_.k, ucum.k], [ps.k])
        cx.mm(ps.t[:, 256:512], lrev.t[:], gb_.t[:], True, True, [gb_.k, lrev.k], [ps.k])
        cx.act(ebT[i2].t[:].rearrange("p k t -> p (k t)"), ps.t[:, 0:256], AF.Exp, [ps.k], [ebT[i2].k])
        cx.act(enbT[i2].t[:].rearrange("p k t -> p (k t)"), ps.t[:, 0:256], AF.Exp, [ps.k], [enbT[i2].k], scale=-1.0)
        cx.act(ed_t[i2].t[:], ps.t[:, 256:512], AF.Exp, [ps.k], [ed_t[i2].k])
        for hh in range(2):
            pr = slice(hh * 64, hh * 64 + 64)
            cx.stt("dve", qtT[i2].t[pr, :, hh, :], qkTa[i2].t[pr, 0:2, :], 0.125, ebT[i2].t[pr], ALU.mult, ALU.mult,
                   [qkTa[i2].k, ebT[i2].k], [qtT[i2].k])
        cx.tt("dve", ktT[i2].t[:], qkTa[i2].t[:, 2:4, :], enbT[i2].t[:], ALU.mult, [qkTa[i2].k, enbT[i2].k], [ktT[i2].k])
        for c in range(2):
            pr = slice(c * 64, c * 64 + 64)
            cx.tt("dve", khat[i2].t[pr, c, :], ka_t[i2].t[pr], ed_t[i2].t[pr], ALU.mult, [ka_t[i2].k, ed_t[i2].k], [khat[i2].k])
        yield
        ams = []
        for h in range(4):
            p, base = h // 2, (h % 2) * 64
            pss = psSb[cnt["s"] % 2]
            cnt["s"] += 1
            cx.mm(pss.t[:, 0:128], ktT[i2].t[:, p, :], qtT[i2].t[:, p, h % 2, :], True, True,
                  [ktT[i2].k, qtT[i2].k], [pss.k])
            am = Am[i2 * 4 + h]
            cx.tt("dve", am.t[:], pss.t[:, 0:128], amask.t[:], ALU.mult, [pss.k, amask.k], [am.k])
            ams.append(am)
        psU = next_ps()
        for c in range(2):
            for h in range(4):
                p, base = h // 2, (h % 2) * 64
                r0 = c * 64
                col = (c * 2 + p) * 128
                cx.mm(psU.t[base:base + 64, col:col + 128], khat[i2].t[:, c, h * 64:(h + 1) * 64],
                      va_t[i2].t[:, h * 128:(h + 1) * 128], True, True, [khat[i2].k, va_t[i2].k], [psU.k])
        sbs = [Sb[sbi[0] % 8]]
        for c in range(2):
            for p in range(2):
                col = (c * 2 + p) * 128
                cx.stt("dve", Sst.t[:, p, :], Sst.t[:, p, :], ebT[i2].t[:, p, c * 64 + 63:c * 64 + 64], psU.t[:, col:col + 128],
                       ALU.mult, ALU.add, [Sst.k, ebT[i2].k, psU.k], [Sst.k])
            sbi[0] += 1
            nsb = Sb[sbi[0] % 8]
            cx.copy("act", nsb.t[:], Sst.t[:], [Sst.k], [nsb.k])
            sbs.append(nsb)
        yield
        for h in range(4):
            p, base = h // 2, (h % 2) * 64
            cx.mm(psGO.t[:, h, :], ams[h].t[:], va_t[i2].t[:, h * 128:(h + 1) * 128], True, False, [ams[h].k, va_t[i2].k], [psGO.k])
            cx.mm(psGO.t[0:64, h, :], qtT[i2].t[:, p, h % 2, 0:64], sbs[0].t[:, p, :], False, True,
                  [qtT[i2].k, sbs[0].k], [psGO.k])
            cx.mm(psGO.t[64:128, h, :], qtT[i2].t[:, p, h % 2, 64:128], sbs[1].t[:, p, :], False, True,
                  [qtT[i2].k, sbs[1].k], [psGO.k])
        gs = gst[i2]
        for h in range(4):
            cx.act(junk.t[:, 0:128], psGO.t[:, h, :], AF.Square, [psGO.k], [junk.k, gs.k], accum_out=gs.t[:, h:h + 1])
        cx.act(gs.t[:, 4:8], gs.t[:, 0:4], AF.Ln, [gs.k], [gs.k], scale=1.0 / 128, bias=EPS)
        cx.act(gs.t[:, 0:4], gs.t[:, 4:8], AF.Exp, [gs.k], [gs.k], scale=-0.5)
        oc = ocat[i2]
        for h in range(4):
            gt = gtmp[h % 2]
            cx.stt("dve", gt.t[:], psGO.t[:, h, :], gs.t[:, h:h + 1], gglab.t[:], ALU.mult, ALU.mult, [psGO.k, gs.k, gglab.k], [gt.k])
            cx.tt("dve", oc.t[:, h * 128:(h + 1) * 128], gt.t[:], sr_t[i2].t[:, h * 128:(h + 1) * 128], ALU.mult,
                  [gt.k, sr_t[i2].k], [oc.k])
        if qb == NT - 1:
            cx.dma("sp", o_glas[s].rearrange("(p h) k v -> (h k) p v", h=2), Sst.t[:], reads=[Sst.k], out_dram=True)

        yield
        for h in range(8):
            c4, base = h // 2, (h % 2) * 64
            pd = psD[h % 2]
            kbs = list(range(max(0, qb - 16), qb + 1))
            groups = [kbs[i:i + 4] for i in range(0, len(kbs), 4)]
            for gi, grp in enumerate(groups):
                ng = len(grp)
                pss = psSb[cnt["s"] % 2]
                cnt["s"] += 1
                for j, kb in enumerate(grp):
                    ks = kb % NR
                    cx.mm(pss.t[:, j * 128:(j + 1) * 128], kTr.t[:, c4, ks * 128:(ks + 1) * 128], qTb[i2].t[:, c4, h % 2, :],
                          True, True, [kT_k[ks], qTb[i2].k], [pss.k])
                pe_ = pe_t[cnt["p"] % 2]
                pm_ = pm_t[cnt["p"] % 2]
                cnt["p"] += 1
                cx.act(pe_.t[:, 0:ng * 128], pss.t[:, 0:ng * 128], AF.Exp, [pss.k], [pe_.k], scale=0.125)
                r0 = 16 - (qb - grp[0])
                cx.tt("dve", pm_.t[:, 0:ng * 128], pe_.t[:, 0:ng * 128], dmf.t[:, h, r0:r0 + ng, :].rearrange("p r q -> p (r q)"), ALU.mult,
                      [pe_.k, dmf.k], [pm_.k])
                for j, kb in enumerate(grp):
                    ks = kb % NR
                    first = gi == 0 and j == 0
                    last = gi == len(groups) - 1 and j == ng - 1
                    cx.mm(pd.t[:, 0:65], pm_.t[:, j * 128:(j + 1) * 128], Vr.t[:, ks, h, :], first, last, [pm_.k, V_k[ks]], [pd.k])
            rd = rden[cnt["r"] % 4]
            cnt["r"] += 1
            S.add("dve", lambda e, rd=rd, pd=pd, h=h: e.reciprocal(out=rd.t[:], in_=pd.t[:, 64:65]), [pd.k], [rd.k])
            cx.ts("dve", oc.t[:, 512 + h * 64:512 + (h + 1) * 64], pd.t[:, 0:64], rd.t[:], None, ALU.mult, None,
                  [pd.k, rd.k], [oc.k])
            yield

        transpose8(oc, oT_t[i2])
        hb = xb
        for half in range(2):
            ps = next_ps()
            for k in range(8):
                cx.mm(ps.t[:], oT_t[i2].t[:, k, :], wout.t[:, k, half * 512:(half + 1) * 512], k == 0, k == 7,
                      [oT_t[i2].k, wout.k], [ps.k])
            cx.tt("dve", hb.t[:, half * 512:(half + 1) * 512], ps.t[:], xb.t[:, half * 512:(half + 1) * 512], ALU.add,
                  [ps.k, xb.k], [hb.k])
        cx.dma("sp", h1_d[s, qb * 128:(qb + 1) * 128, :], hb.t[:], reads=[hb.k], out_dram=True)
        yield

    interleave((tile_gen(s_, qb_) for s_ in range(nseq) for qb_ in range(NT)), width=2)
    cx.close()


def host_consts_b():
    t = np.arange(128)
    return {
        "c_tri": (t[:, None] < t[None, :]).astype(np.float32),
        "c_iota32": np.broadcast_to(np.arange(32, dtype=np.float32), (128, 32)).copy(),
        "c_iota4": np.broadcast_to(np.arange(4, dtype=np.float32), (128, 4)).copy(),
    }


def phase_b(nc, din, dout, NT, nseq, h1_d, c_ident, debug, h1s_d=None):
    T = NT * 128
    NTOT = nseq * NT + (1 if h1s_d is not None else 0)
    if h1s_d is not None:
        cmk = din("cmk", [NSS, 256, D])
        cmv = din("cmv", [NSS, 256, D])
        c_sel = nc_input(nc, din, "c_sel", [8, 2048])
    memp = din("memp", [nseq, 256, D])
    g2 = din("g_norm2", [1, D])
    gm = din("g_mem", [1, D])
    g3 = din("g_norm3", [1, D])
    w_cq = din("w_cq", [D, D])
    w_mk = din("w_mk", [D, D])
    w_mv = din("w_mv", [D, D])
    w_co = din("w_co", [D, D])
    w_r = din("w_r", [D, 36])
    b_r = din("b_r", [1, 36])
    c_tri = din("c_tri", [128, 128])
    c_iota32 = nc_input(nc, din, "c_iota32", [128, 32])
    c_iota4 = din("c_iota4", [128, 4])
    o_memk = dout("o_memk", [nseq, 256, D])
    o_memv = dout("o_memv", [nseq, 256, D])
    mk_out = dout if debug else (lambda n, sh, dt=F32: nc.dram_tensor(n, list(sh), dt).ap())
    h2_d = mk_out("h2", [NTOT * 128, D])
    xn_d = mk_out("xn", [NTOT * 128, D], BF16)
    route_d = mk_out("route", [128, NTOT, 8])
    cnt_d = mk_out("cnt", [128, 32])

    cx = Ctx(nc)
    S = cx.S
    ident_b = cx.sb([128, 128], BF16, "b_ident_b")
    ident_f = cx.sb([128, 128], F32, "b_ident_f")
    tri_b = cx.sb([128, 128], BF16, "b_tri")
    ones_b = cx.sb([128, 128], BF16, "b_ones")
    iota32 = cx.sb([128, 32], F32, "b_iota32")
    iota4 = cx.sb([128, 4], F32, "b_iota4")
    g2b = cx.sb([128, D], F32, "g2b")
    g3b = cx.sb([128, D], F32, "g3b")
    gmb = cx.sb([128, D], F32, "gmb")
    brb = cx.sb([128, 36], F32, "brb")
    wcq = cx.sb([128, 8, D], BF16, "wcq")
    wco = cx.sb([128, 8, D], BF16, "wco")
    wmk = cx.sb([128, 8, D], BF16, "wmk")
    wmv = cx.sb([128, 8, D], BF16, "wmv")
    wr = cx.sb([128, 8, 36], F32, "wr")
    mT = cx.sb([128, 8, 256], BF16, "mT")
    mkT = cx.sb([128, 8, 256], BF16, "mkT")
    mva = cx.sb([128, 2, 4, 257], BF16, "mva")
    route = cx.sb([128, NTOT, 8], F32, "route_sb")
    cntb = cx.sb([128, 32], F32, "cntb")

    if h1s_d is not None:
        sel = cx.sb([8, 16, 128], BF16, "b_sel")
        cx.dma("pool", sel.t[:].rearrange("p c t -> p (c t)"), c_sel, writes=[sel.k])
        stgk = [cx.sb([128, 2, D], BF16, f"b_stgk{i}") for i in range(2)]
        mkTs = [cx.sb([128, 8, 256], BF16, f"b_mkTs{i}") for i in range(2)]
        mvas = [cx.sb([128, 2, 4, 257], BF16, f"b_mvas{i}") for i in range(2)]
        PTs = [cx.sb([128, 8, 8], BF16, f"b_PTs{i}") for i in range(2)]
        ocs = [cx.sb([8, D], BF16, f"b_ocs{i}") for i in range(2)]
        rdens = [cx.sb([8, 1], F32, f"b_rdens{i}") for i in range(2)]
        for v_ in mvas:
            S.add("pool", lambda e, v_=v_: e.memset(v_.t[:], 1.0), (), [v_.k])
    cx.dma("pool", ident_b.t[:], c_ident, writes=[ident_b.k])
    cx.dma("sp", ident_f.t[:], c_ident, writes=[ident_f.k])
    cx.dma("pool", tri_b.t[:], c_tri, writes=[tri_b.k])
    cx.dma("sp", iota32.t[:], c_iota32, writes=[iota32.k])
    cx.dma("sp", iota4.t[:], c_iota4, writes=[iota4.k])
    cx.dma("sp", g2b.t[:], g2.partition_broadcast(128), writes=[g2b.k])
    cx.dma("sp", g3b.t[:], g3.partition_broadcast(128), writes=[g3b.k])
    cx.dma("sp", gmb.t[:], gm.partition_broadcast(128), writes=[gmb.k])
    cx.dma("sp", brb.t[:], b_r.partition_broadcast(128), writes=[brb.k])
    cx.dma("sp", wr.t[:], w_r.rearrange("(k p) f -> p k f", p=128), writes=[wr.k])
    for wsb, wd in ((wmk, w_mk), (wmv, w_mv), (wcq, w_cq), (wco, w_co)):
        v = wd.rearrange("(k p) f -> p k f", p=128)
        for k in range(8):
            cx.dma("pool", wsb.t[:, k, :], v[:, k, :], writes=[wsb.k])
    S.add("pool", lambda e: e.memset(ones_b.t[:], 1.0), (), [ones_b.k])
    S.add("pool", lambda e: e.memset(mva.t[:], 1.0), (), [mva.k])
    S.add("dve", lambda e: e.memset(cntb.t[:], 0.0), (), [cntb.k])
    S.add("dve", lambda e: e.memset(route.t[:], 0.0), (), [route.k])

    psT = cx.ps([128, 1024], BF16, "b_psT")
    psF = [cx.ps([128, 512], F32, f"b_psF{i}") for i in range(2)]
    psR = [cx.ps([128, 512], F32, f"b_psR{i}") for i in range(2)]
    psD = [cx.ps([128, 512], F32, f"b_psD{i}") for i in range(2)]
    psX = cx.ps([128, 512], F32, "b_psX")
    rr = [0]

    def next_ps():
        b = psR[rr[0] % 2]
        rr[0] += 1
        return b

    def rot(n, shape, dt, name):
        return [cx.sb(shape, dt, f"b_{name}{i}") for i in range(n)]

    h_t = rot(2, [128, D], F32, "h")
    junk = cx.sb([128, D], BF16, "b_junk")
    st_t = rot(2, [128, 8], F32, "stat")
    n_t = rot(2, [128, D], BF16, "n")
    nT_t = rot(2, [128, 8, 128], BF16, "nT")
    qT_t = rot(2, [128, 8, 128], BF16, "qT")
    PT_t = rot(2, [128, 8, 128], BF16, "PT")
    oc_t = rot(2, [128, D], BF16, "oc")
    oT_t = rot(2, [128, 8, 128], BF16, "oT")
    rden = rot(4, [128, 1], F32, "rden")
    mo_t = rot(2, [128, 512], F32, "mo")
    xf_t = rot(2, [128, D], F32, "xf")
    xb_t = rot(2, [128, D], BF16, "xb")
    xfT = cx.sb([128, 8, 128], F32, "b_xfT")
    rt_t = rot(2, [128, 64], F32, "rt")
    mx8 = rot(2, [128, 8], F32, "mx8")
    ix8 = rot(2, [128, 8], U32, "ix8")
    ixf = rot(2, [128, 8], F32, "ixf")
    O0_t = rot(2, [128, 32], F32, "O0")
    O1_t = rot(2, [128, 32], F32, "O1")
    Ob_t = rot(2, [128, 32], BF16, "Ob")
    rk_t = rot(2, [128, 32], F32, "rk")
    t32 = rot(2, [128, 32], F32, "t32")
    cnt = {"r": 0}

    def rmsnorm(xb, gb, out, stb, eng="dve"):
        cx.act(junk.t[:], xb.t[:], AF.Square, [xb.k], [junk.k, stb.k], accum_out=stb.t[:, 0:1])
        cx.act(stb.t[:, 1:2], stb.t[:, 0:1], AF.Ln, [stb.k], [stb.k], scale=1.0 / D, bias=EPS)
        cx.act(stb.t[:, 2:3], stb.t[:, 1:2], AF.Exp, [stb.k], [stb.k], scale=-0.5)
        cx.stt(eng, out.t[:], xb.t[:], stb.t[:, 2:3], gb.t[:], ALU.mult, ALU.mult, [xb.k, stb.k, gb.k], [out.k])

    def transpose8(src, dst_ap, dst_k):
        for k in range(8):
            cx.tr(psT.t[:, k * 128:(k + 1) * 128], src.t[:, k * 128:(k + 1) * 128], ident_b.t[:], [src.k, ident_b.k], [psT.k])
        cx.copy("act", dst_ap, psT.t[:].rearrange("p (k t) -> p k t", k=8), [psT.k], [dst_k])

    gt = 0
    units = [(s_, False) for s_ in range(nseq)] + ([(0, True)] if h1s_d is not None else [])
    for s, is_samp in units:
      if not is_samp:
          for mt in range(2):
              hb, nb, stb = h_t[mt], n_t[mt], st_t[mt]
              cx.dma("sp", hb.t[:], memp[s, mt * 128:(mt + 1) * 128, :], writes=[hb.k])
              rmsnorm(hb, gmb, nb, stb)
              transpose8(nb, mT.t[:, :, mt * 128:(mt + 1) * 128], mT.k)
          for mt in range(2):
              for wsb, od, isv in ((wmk, o_memk, False), (wmv, o_memv, True)):
                  for half in range(2):
                      ps = next_ps()
                      for k in range(8):
                          cx.mm(ps.t[:], mT.t[:, k, mt * 128:(mt + 1) * 128], wsb.t[:, k, half * 512:(half + 1) * 512], k == 0, k == 7,
                                [mT.k, wsb.k], [ps.k])
                      mo = mo_t[rr[0] % 2]
                      cx.copy("act", mo.t[:], ps.t[:], [ps.k], [mo.k])
                      if isv:
                          cx.copy("dve", mva.t[:, mt, 2 * half:2 * half + 2, 0:256], ps.t[:].rearrange("p (h d) -> p h d", h=2), [ps.k], [mva.k])
                      cx.dma("sp", od[s, mt * 128:(mt + 1) * 128, half * 512:(half + 1) * 512], mo.t[:], reads=[mo.k], out_dram=True)
          for c2 in range(4):
              ps = next_ps()
              for j in range(2):
                  c8 = c2 * 2 + j
                  for k in range(8):
                      cx.mm(ps.t[:, j * 256:(j + 1) * 256], wmk.t[:, k, c8 * 128:(c8 + 1) * 128], mT.t[:, k, :], k == 0, k == 7,
                            [wmk.k, mT.k], [ps.k])
              cx.copy("act", mkT.t[:, c2 * 2:c2 * 2 + 2, :], ps.t[:].rearrange("p (j m) -> p j m", j=2), [ps.k], [mkT.k])

      def tile_b(s, is_samp, qb, gt):
            i2 = gt % 2
            hb, nb, nT, stb = h_t[i2], n_t[i2], nT_t[i2], st_t[i2]
            cx.dma("sp", hb.t[:], h1s_d if is_samp else h1_d[s, qb * 128:(qb + 1) * 128, :], writes=[hb.k])
            rmsnorm(hb, g2b, nb, stb)
            transpose8(nb, nT.t[:], nT.k)
            qT = qT_t[i2]
            for half in range(2):
                ps = next_ps()
                for j in range(4):
                    c8 = half * 4 + j
                    for k in range(8):
                        cx.mm(ps.t[:, j * 128:(j + 1) * 128], wcq.t[:, k, c8 * 128:(c8 + 1) * 128], nT.t[:, k, :], k == 0, k == 7,
                              [wcq.k, nT.k], [ps.k])
                cx.copy("act", qT.t[:, half * 4:half * 4 + 4, :], ps.t[:].rearrange("p (j t) -> p j t", j=4), [ps.k], [qT.k])
            yield
            if is_samp:
                oc = oc_t[i2]
                for c in range(NSS):
                    sk, mkT_, mva_, PT_, oc_, rd_ = stgk[c % 2], mkTs[c % 2], mvas[c % 2], PTs[c % 2], ocs[c % 2], rdens[c % 2]
                    cx.dma("pool", sk.t[:], cmk[c].rearrange("(m p) f -> p m f", p=128), writes=[sk.k])
                    for mb in range(2):
                        cx.dma("pool", mva_.t[:, mb, :, 0:256], cmv[c, mb * 128:(mb + 1) * 128, :].rearrange("p (h d) -> p h d", h=4), writes=[mva_.k])
                    for mb in range(2):
                        for c8 in range(8):
                            cx.tr(psT.t[:, c8 * 128:(c8 + 1) * 128], sk.t[:, mb, c8 * 128:(c8 + 1) * 128], ident_b.t[:], [sk.k, ident_b.k], [psT.k])
                        cx.copy("act", mkT_.t[:, :, mb * 128:(mb + 1) * 128], psT.t[:].rearrange("p (k t) -> p k t", k=8), [psT.k], [mkT_.k])
                    ps = next_ps()
                    for h in range(4):
                        for mb in range(2):
                            idx = h * 2 + mb
                            for j in range(2):
                                cx.mm(ps.t[:, idx * 8:(idx + 1) * 8], mkT_.t[:, 2 * h + j, mb * 128:(mb + 1) * 128], qT.t[:, 2 * h + j, 8 * c:8 * c + 8],
                                      j == 0, j == 1, [mkT_.k, qT.k], [ps.k])
                    cx.act(PT_.t[:].rearrange("p a b -> p (a b)"), ps.t[:, 0:64], AF.Exp, [ps.k], [PT_.k], scale=1.0 / 16)
                    for h in range(4):
                        pd = psD[h % 2]
                        for mb in range(2):
                            cx.mm(pd.t[0:8, 0:257], PT_.t[:, h * 2 + mb, :], mva_.t[:, mb, h, :], mb == 0, mb == 1, [PT_.k, mva_.k], [pd.k])
                        S.add("dve", lambda e, rd_=rd_, pd=pd: e.reciprocal(out=rd_.t[:], in_=pd.t[0:8, 256:257]), [pd.k], [rd_.k])
                        cx.ts("dve", oc_.t[:, h * 256:(h + 1) * 256], pd.t[0:8, 0:256], rd_.t[:], None, ALU.mult, None, [pd.k, rd_.k], [oc_.k])
                    for half in range(2):
                        cx.mm(psF[half].t[:], sel.t[:, c, :], oc_.t[:, half * 512:(half + 1) * 512], c == 0, c == NSS - 1, [sel.k, oc_.k], [psF[half].k])
                for half in range(2):
                    cx.copy("act", oc.t[:, half * 512:(half + 1) * 512], psF[half].t[:], [psF[half].k], [oc.k])
            else:
                PT = PT_t[i2]
                for hp in range(2):
                    ps = next_ps()
                    for hh in range(2):
                        h = hp * 2 + hh
                        for mb in range(2):
                            idx = hh * 2 + mb
                            for j in range(2):
                                cx.mm(ps.t[:, idx * 128:(idx + 1) * 128], mkT.t[:, 2 * h + j, mb * 128:(mb + 1) * 128], qT.t[:, 2 * h + j, :],
                                      j == 0, j == 1, [mkT.k, qT.k], [ps.k])
                    cx.act(PT.t[:, hp * 4:hp * 4 + 4, :], ps.t[:].rearrange("p (j t) -> p j t", j=4), AF.Exp, [ps.k], [PT.k], scale=1.0 / 16)
                oc = oc_t[i2]
                for h in range(4):
                    pd = psD[h % 2]
                    for mb in range(2):
                        cx.mm(pd.t[:, 0:257], PT.t[:, h * 2 + mb, :], mva.t[:, mb, h, :], mb == 0, mb == 1, [PT.k, mva.k], [pd.k])
                    rd = rden[cnt["r"] % 4]
                    cnt["r"] += 1
                    S.add("dve", lambda e, rd=rd, pd=pd: e.reciprocal(out=rd.t[:], in_=pd.t[:, 256:257]), [pd.k], [rd.k])
                    cx.ts("dve", oc.t[:, h * 256:(h + 1) * 256], pd.t[:, 0:256], rd.t[:], None, ALU.mult, None, [pd.k, rd.k], [oc.k])
            yield
            oT = oT_t[i2]
            transpose8(oc, oT.t[:], oT.k)
            for half in range(2):
                ps = next_ps()
                for k in range(8):
                    cx.mm(ps.t[:], oT.t[:, k, :], wco.t[:, k, half * 512:(half + 1) * 512], k == 0, k == 7, [oT.k, wco.k], [ps.k])
                cx.tt("dve", hb.t[:, half * 512:(half + 1) * 512], ps.t[:], hb.t[:, half * 512:(half + 1) * 512], ALU.add, [ps.k, hb.k], [hb.k])
            cx.dma("sp", h2_d[gt * 128:(gt + 1) * 128, :], hb.t[:], reads=[hb.k], out_dram=True)
            xf, xb = xf_t[i2], xb_t[i2]
            rmsnorm(hb, g3b, xf, stb)
            cx.copy("act", xb.t[:], xf.t[:], [xf.k], [xb.k])
            cx.dma("sp", xn_d[gt * 128:(gt + 1) * 128, :], xb.t[:], reads=[xb.k], out_dram=True)
            yield
            for half in range(2):
                for j in range(4):
                    k = half * 4 + j
                    cx.tr(psF[half].t[:, j * 128:(j + 1) * 128], xf.t[:, k * 128:(k + 1) * 128], ident_f.t[:], [xf.k, ident_f.k], [psF[half].k])
                cx.copy("act", xfT.t[:, half * 4:half * 4 + 4, :], psF[half].t[:].rearrange("p (j t) -> p j t", j=4), [psF[half].k], [xfT.k])
            for k in range(8):
                cx.mm(psX.t[:, 0:36], xfT.t[:, k, :], wr.t[:, k, :], k == 0, k == 7, [xfT.k, wr.k], [psX.k])
            rt = rt_t[i2]
            R_ = [rt.k]
            c = lambda a, b=None: rt.t[:, a:(a + 1 if b is None else b)]
            cx.tt("dve", c(0, 36), psX.t[:, 0:36], brb.t[:], ALU.add, [psX.k, brb.k], R_)
            S.add("dve", lambda e, rt=rt: e.tensor_reduce(out=rt.t[:, 36:37], in_=rt.t[:, 0:4], axis=AX.X, op=ALU.max), R_, R_)
            cx.ts("dve", c(37), c(36), -1.0, None, ALU.mult, None, R_, R_)
            cx.act(c(45, 49), c(0, 4), AF.Exp, R_, R_, bias=c(37), scale=1.0, accum_out=c(38))
            S.add("dve", lambda e, rt=rt: e.reciprocal(out=rt.t[:, 39:40], in_=rt.t[:, 38:39]), R_, R_)
            cx.ts("dve", c(40, 44), c(0, 4), c(36), None, ALU.is_equal, None, R_, R_)
            cx.tt("dve", c(45, 49), c(40, 44), iota4.t[:], ALU.mult, R_ + [iota4.k], R_)
            S.add("dve", lambda e, rt=rt: e.tensor_reduce(out=rt.t[:, 44:45], in_=rt.t[:, 45:49], axis=AX.X, op=ALU.add), R_, R_)
            cx.ts("dve", c(49, 57), c(4, 12), c(40), None, ALU.mult, None, R_, R_)
            for g in range(1, 4):
                cx.stt("dve", c(49, 57), c(4 + 8 * g, 12 + 8 * g), c(40 + g), c(49, 57), ALU.mult, ALU.add, R_, R_)
            m8, i8, i8f = mx8[i2], ix8[i2], ixf[i2]
            S.add("dve", lambda e, rt=rt, m8=m8: e.max(out=m8.t[:], in_=rt.t[:, 49:57]), R_, [m8.k])
            S.add("dve", lambda e, rt=rt, m8=m8, i8=i8: e.max_index(out=i8.t[:], in_max=m8.t[:], in_values=rt.t[:, 49:57]), R_ + [m8.k], [i8.k])
            cx.copy("dve", i8f.t[:], i8.t[:], [i8.k], [i8f.k])
            cx.tt("dve", c(57), m8.t[:, 1:2], m8.t[:, 0:1], ALU.subtract, [m8.k], R_)
            cx.act(c(58), c(57), AF.Exp, R_, R_)
            cx.ts("dve", c(59), c(58), 1.0, None, ALU.add, None, R_, R_)
            S.add("dve", lambda e, rt=rt: e.reciprocal(out=rt.t[:, 59:60], in_=rt.t[:, 59:60]), R_, R_)
            cx.tt("dve", c(60), c(59), c(39), ALU.mult, R_, R_)
            cx.tt("dve", c(61), c(60), c(58), ALU.mult, R_, R_)
            cx.stt("dve", c(62), c(44), 8.0, i8f.t[:, 0:1], ALU.mult, ALU.add, R_ + [i8f.k], R_)
            cx.stt("dve", c(63), c(44), 8.0, i8f.t[:, 1:2], ALU.mult, ALU.add, R_ + [i8f.k], R_)
            O0, O1, Ob, rk, tt32 = O0_t[i2], O1_t[i2], Ob_t[i2], rk_t[i2], t32[i2]
            cx.ts("dve", O0.t[:], iota32.t[:], c(62), None, ALU.is_equal, None, R_ + [iota32.k], [O0.k])
            cx.ts("dve", O1.t[:], iota32.t[:], c(63), None, ALU.is_equal, None, R_ + [iota32.k], [O1.k])
            cx.tt("dve", Ob.t[:], O0.t[:], O1.t[:], ALU.add, [O0.k, O1.k], [Ob.k])
            cx.mm(psX.t[:, 64:96], tri_b.t[:], Ob.t[:], True, True, [tri_b.k, Ob.k], [psX.k])
            cx.mm(psX.t[:, 96:128], ones_b.t[:], Ob.t[:], True, True, [ones_b.k, Ob.k], [psX.k])
            cx.tt("dve", rk.t[:], psX.t[:, 64:96], cntb.t[:], ALU.add, [psX.k, cntb.k], [rk.k])
            cx.tt("dve", cntb.t[:], psX.t[:, 96:128], cntb.t[:], ALU.add, [psX.k, cntb.k], [cntb.k])
            cx.tt("dve", tt32.t[:], O0.t[:], rk.t[:], ALU.mult, [O0.k, rk.k], [tt32.k])
            S.add("dve", lambda e, tt32=tt32, gt=gt: e.tensor_reduce(out=route.t[:, gt, 2:3], in_=tt32.t[:], axis=AX.X, op=ALU.add), [tt32.k], [route.k])
            cx.tt("dve", tt32.t[:], O1.t[:], rk.t[:], ALU.mult, [O1.k, rk.k], [tt32.k])
            S.add("dve", lambda e, tt32=tt32, gt=gt: e.tensor_reduce(out=route.t[:, gt, 3:4], in_=tt32.t[:], axis=AX.X, op=ALU.add), [tt32.k], [route.k])
            cx.copy("dve", route.t[:, gt, 0:2], c(62, 64), R_, [route.k])
            cx.copy("dve", route.t[:, gt, 4:6], c(60, 62), R_, [route.k])
            yield

      ntl = 1 if is_samp else NT
      interleave((tile_b(s, is_samp, qb_, gt + qb_) for qb_ in range(ntl)), width=2)
      gt += ntl
    cx.dma("sp", route_d, route.t[:], reads=[route.k], out_dram=True)
    cx.dma("sp", cnt_d, cntb.t[:], reads=[cntb.k], out_dram=True)
    cx.close()
    return h2_d, xn_d, route_d, cnt_d


BLK = 256
_BREGS = {}


def _breg(e, val):
    key = (id(e), val)
    if key not in _BREGS:
        _BREGS[key] = e.to_reg(val)
    return _BREGS[key]


def host_consts_c():
    p = np.arange(128, dtype=np.float32)
    return {
        "c_thr": (p * BLK).reshape(128, 1).astype(np.float32),
        "c_kp": (np.arange(8, dtype=np.float32)[None, :] * 128 + p[:, None]).astype(np.float32),
    }


def phase_c(nc, din, dout, NTOT, h2_d, xn_d, route_d, cnt_d, c_ident, y_d):
    NTOK = NTOT * 128
    NB = -(-2 * NTOK // BLK) + 32
    assert NB <= 128
    R = NB * BLK
    w_e1 = din("w_e1", [32 * D, 512])
    w_e3 = din("w_e3", [32 * D, 512])
    w_e2 = din("w_e2", [32 * 512, D])
    gf = din("g_final", [1, D])
    c_iota32 = nc_input(nc, din, "c_iota32", [128, 32])
    c_thr = din("c_thr", [128, 1])
    c_kp = din("c_kp", [128, 8])
    Xs = nc.dram_tensor("moe_xs", [R, D], BF16).ap()
    Ys = nc.dram_tensor("moe_ys", [R, D], F32).ap()
    Xs_k, Ys_k = Tok("Xs"), Tok("Ys")
    xs_parts, ys_parts = [], []

    cx = Ctx(nc)
    S = cx.S
    ident_b = cx.sb([128, 128], BF16, "c_ident_b")
    ident_f = cx.sb([128, 128], F32, "c_ident_f")
    ones_f = cx.sb([128, 128], F32, "c_ones_f")
    iota32 = cx.sb([128, 32], F32, "c_iota32s")
    thr = cx.sb([128, 1], F32, "c_thrs")
    kp = cx.sb([128, 8], F32, "c_kps")
    gfb = cx.sb([128, D], F32, "gfb")
    route = cx.sb([128, NTOT, 8], F32, "c_route")
    cntb = cx.sb([128, 32], F32, "c_cnt")
    sm = cx.sb([128, 8, 32], F32, "c_sm")
    z32 = cx.sb([128, 32], F32, "c_z32")
    becol = cx.sb([128, 2], F32, "c_becol")
    dgb = cx.sb([128, 128], F32, "c_dgb")
    bebc = cx.sb([128, 128], F32, "c_bebc")
    idf = cx.sb([128, NB, 12], F32, "c_idf")
    idi = cx.sb([128, NB, 12], I32, "c_idi")
    dstf = cx.sb([128, NTOT, 2], F32, "c_dstf")
    dsti = cx.sb([128, NTOT, 2], I32, "c_dsti")
    psT = cx.ps([128, 1024], BF16, "c_psT")
    psH1 = [cx.ps([128, 512], F32, f"c_psH1{i}") for i in range(2)]
    psH3 = [cx.ps([128, 512], F32, f"c_psH3{i}") for i in range(2)]
    psY = [cx.ps([128, 512], F32, f"c_psY{i}") for i in range(2)]
    psX = cx.ps([128, 512], F32, "c_psX")

    cx.dma("pool", ident_b.t[:], c_ident, writes=[ident_b.k])
    cx.dma("sp", ident_f.t[:], c_ident, writes=[ident_f.k])
    cx.dma("sp", iota32.t[:], c_iota32, writes=[iota32.k])
    cx.dma("sp", thr.t[:], c_thr, writes=[thr.k])
    cx.dma("sp", kp.t[:], c_kp, writes=[kp.k])
    cx.dma("sp", gfb.t[:], gf.partition_broadcast(128), writes=[gfb.k])
    cx.dma("sp", route.t[:], route_d, writes=[route.k])
    cx.dma("sp", cntb.t[:], cnt_d, writes=[cntb.k])
    S.add("dve", lambda e: e.memset(ones_f.t[:], 1.0), (), [ones_f.k])
    S.add("dve", lambda e: e.memset(z32.t[:], 0.0), (), [z32.k])
    K_ = [sm.k]
    padded, pend, pstart, cmp_ = sm.t[:, 0, :], sm.t[:, 1, :], sm.t[:, 2, :], sm.t[:, 3, :]
    tmpa, tmpb = sm.t[:, 4, :], sm.t[:, 5, :]
    cx.ts("dve", tmpa, cntb.t[:], 0.0, None, ALU.is_gt, None, [cntb.k], K_)
    for j in range(1, NTOK // BLK + 1):
        cx.stt("dve", tmpa, cntb.t[:], float(BLK * j), tmpa, ALU.is_gt, ALU.add, [cntb.k] + K_, K_)
    cx.ts("dve", padded, tmpa, float(BLK), None, ALU.mult, None, K_, K_)
    S.add("dve", lambda e: e.tensor_tensor_scan(out=pend, data0=padded, data1=z32.t[:], initial=0.0, op0=ALU.add, op1=ALU.add), K_ + [z32.k], K_)
    cx.tt("dve", pstart, pend, padded, ALU.subtract, K_, K_)
    cx.ts("dve", cmp_, pend, thr.t[:], None, ALU.is_le, None, K_ + [thr.k], K_)
    S.add("dve", lambda e: e.tensor_reduce(out=becol.t[:, 0:1], in_=cmp_, axis=AX.X, op=ALU.add), K_, [becol.k])
    cx.ts("dve", becol.t[:, 1:2], becol.t[:, 0:1], 31.0, None, ALU.min, None, [becol.k], [becol.k])
    cx.ts("dve", dgb.t[:], ident_f.t[:], becol.t[:, 1:2], None, ALU.mult, None, [ident_f.k, becol.k], [dgb.k])
    cx.mm(psX.t[:, 0:128], ones_f.t[:], dgb.t[:], True, True, [ones_f.k, dgb.k], [psX.k])
    cx.copy("dve", bebc.t[:], psX.t[:, 0:128], [psX.k], [bebc.k])
    for k in range(8):
        cx.ts("dve", idf.t[:, :, k], bebc.t[:, 0:NB], 1024.0, kp.t[:, k:k + 1], ALU.mult, ALU.add, [bebc.k, kp.k], [idf.k])
    for k in range(4):
        cx.ts("dve", idf.t[:, :, 8 + k], bebc.t[:, 0:NB], 512.0, kp.t[:, k:k + 1], ALU.mult, ALU.add, [bebc.k, kp.k], [idf.k])
    same = cx.sb([128, 128], F32, "c_same")
    S.add("dve", lambda e: e.memset(same.t[:], 0.0), (), [same.k])
    cx.tt("dve", same.t[:, 2:NB], bebc.t[:, 2:NB], bebc.t[:, 0:NB - 2], ALU.is_equal, [bebc.k, same.k], [same.k])
    for k in range(12):
        cx.stt("dve", idf.t[:, :, k], same.t[:, 0:NB], 1.0e6, idf.t[:, :, k], ALU.mult, ALU.add, [same.k, idf.k], [idf.k])
    cx.copy("dve", idi.t[:], idf.t[:], [idf.k], [idi.k])

    def rot(n, shape, dt, name):
        return [cx.sb(shape, dt, f"c_{name}{i}") for i in range(n)]

    zt = cx.sb([128, 8, D], BF16, "c_zero")
    S.add("pool", lambda e: e.memset(zt.t[:], 0.0), (), [zt.k])
    r0 = 0
    while r0 < R:
        nr = min(1024, R - r0)
        zk = Tok("xz")
        cx.dma("sp", Xs[r0:r0 + nr, :].rearrange("(s p) f -> p s f", p=128), zt.t[:, 0:nr // 128, :], reads=[zt.k], writes=[zk], out_dram=True)
        xs_parts.append(zk)
        r0 += nr
    S.add("pool", None, xs_parts, [Xs_k])
    xs_parts = []
    xn_t = rot(3, [128, D], BF16, "xn")
    o_t = rot(2, [128, 32], F32, "o32")
    for gt in range(NTOT):
        xb = xn_t[gt % 3]
        cx.dma("sp", xb.t[:], xn_d[gt * 128:(gt + 1) * 128, :], writes=[xb.k])
        for sl in range(2):
            o32 = o_t[sl]
            cx.ts("dve", o32.t[:], iota32.t[:], route.t[:, gt, sl:sl + 1], None, ALU.is_equal, None, [iota32.k, route.k], [o32.k])
            cx.tt("dve", o32.t[:], o32.t[:], pstart, ALU.mult, [o32.k] + K_, [o32.k])
            S.add("dve", lambda e, o32=o32, gt=gt, sl=sl: e.tensor_reduce(out=dstf.t[:, gt, sl:sl + 1], in_=o32.t[:], axis=AX.X, op=ALU.add),
                  [o32.k], [dstf.k])
        cx.tt("dve", dstf.t[:, gt, :], dstf.t[:, gt, :], route.t[:, gt, 2:4], ALU.add, [dstf.k, route.k], [dstf.k])
        cx.copy("dve", dsti.t[:, gt, :], dstf.t[:, gt, :], [dstf.k], [dsti.k])
        for sl in range(2):
            S.add("pool", lambda e, xb=xb, gt=gt, sl=sl: e.indirect_dma_start(
                out=Xs, out_offset=bass.IndirectOffsetOnAxis(ap=dsti.t[:, gt, sl:sl + 1], axis=0), in_=xb.t[:, :], in_offset=None),
                [xb.k, dsti.k, Xs_k], [xs_parts.append(Tok("xsc")) or xs_parts[-1]], dma=True, out=True)

    S.add("sp", None, xs_parts, [Xs_k])
    xs_t = rot(2, [128, 2, D], BF16, "xs")
    xsT_t = rot(2, [128, 8, 256], BF16, "xsT")
    w1_t = rot(2, [128, 8, 512], BF16, "w1")
    w3_t = rot(2, [128, 8, 512], BF16, "w3")
    w2_t = rot(2, [128, 4, D], BF16, "w2")
    sg_t = rot(2, [128, 512], F32, "sg")
    hT_t = rot(2, [128, 4, 256], BF16, "hT")
    yo_t = rot(2, [128, D], F32, "yo")
    for b in range(NB):
        i2 = b % 2
        xs, xsT, w1, w3, w2, hT = xs_t[i2], xsT_t[i2], w1_t[i2], w3_t[i2], w2_t[i2], hT_t[i2]
        for k in range(8):
            S.add("pool", lambda e, w1=w1, k=k, b=b: e.indirect_dma_start(
                out=w1.t[:, k, :], out_offset=None, in_=w_e1, in_offset=bass.IndirectOffsetOnAxis(ap=idi.t[:, b, k:k + 1], axis=0),
                bounds_check=_breg(e, 32 * D - 1), oob_is_err=False),
                [idi.k], [w1.k], dma=True)
            S.add("pool", lambda e, w3=w3, k=k, b=b: e.indirect_dma_start(
                out=w3.t[:, k, :], out_offset=None, in_=w_e3, in_offset=bass.IndirectOffsetOnAxis(ap=idi.t[:, b, k:k + 1], axis=0),
                bounds_check=_breg(e, 32 * D - 1), oob_is_err=False),
                [idi.k], [w3.k], dma=True)
        for k in range(4):
            S.add("pool", lambda e, w2=w2, k=k, b=b: e.indirect_dma_start(
                out=w2.t[:, k, :], out_offset=None, in_=w_e2, in_offset=bass.IndirectOffsetOnAxis(ap=idi.t[:, b, 8 + k:9 + k], axis=0),
                bounds_check=_breg(e, 32 * 512 - 1), oob_is_err=False),
                [idi.k], [w2.k], dma=True)
        cx.dma("sp", xs.t[:], Xs[b * BLK:(b + 1) * BLK, :].rearrange("(s p) f -> p s f", p=128), reads=[Xs_k], writes=[xs.k])
        for sub in range(2):
            for k in range(8):
                cx.tr(psT.t[:, k * 128:(k + 1) * 128], xs.t[:, sub, k * 128:(k + 1) * 128], ident_b.t[:], [xs.k, ident_b.k], [psT.k])
            cx.copy("act", xsT.t[:, :, sub * 128:(sub + 1) * 128], psT.t[:].rearrange("p (k t) -> p k t", k=8), [psT.k], [xsT.k])
        for pr in range(2):
            for wt, pst in ((w1, psH1[pr]), (w3, psH3[pr])):
                for j in range(2):
                    fc = pr * 2 + j
                    for k in range(8):
                        cx.mm(pst.t[:, j * 256:(j + 1) * 256], wt.t[:, k, fc * 128:(fc + 1) * 128], xsT.t[:, k, :], k == 0, k == 7,
                              [wt.k, xsT.k], [pst.k])
            sg = sg_t[pr]
            cx.act(sg.t[:], psH1[pr].t[:], AF.Silu, [psH1[pr].k], [sg.k])
            cx.tt("dve", hT.t[:, pr * 2:pr * 2 + 2, :], sg.t[:].rearrange("p (j t) -> p j t", j=2),
                  psH3[pr].t[:].rearrange("p (j t) -> p j t", j=2), ALU.mult, [sg.k, psH3[pr].k], [hT.k])
        for sub in range(2):
            yo = yo_t[sub]
            for half in range(2):
                py = psY[half]
                for fk in range(4):
                    cx.mm(py.t[:], hT.t[:, fk, sub * 128:(sub + 1) * 128], w2.t[:, fk, half * 512:(half + 1) * 512], fk == 0, fk == 3,
                          [hT.k, w2.k], [py.k])
                cx.copy("act" if half else "dve", yo.t[:, half * 512:(half + 1) * 512], py.t[:], [py.k], [yo.k])
            cx.dma("sp", Ys[b * BLK + sub * 128:b * BLK + (sub + 1) * 128, :], yo.t[:], reads=[yo.k], writes=[ys_parts.append(Tok("ysp")) or ys_parts[-1]], out_dram=True)

    S.add("pool", None, ys_parts, [Ys_k])
    h_t = rot(2, [128, D], F32, "h")
    y0_t = rot(2, [128, D], F32, "y0")
    y1_t = rot(2, [128, D], F32, "y1")
    st_t = rot(2, [128, 8], F32, "stat")
    junk = cx.sb([128, D], BF16, "c_junk")
    for gt in range(NTOT):
        i2 = gt % 2
        hb, y0, y1, stb = h_t[i2], y0_t[i2], y1_t[i2], st_t[i2]
        cx.dma("sp", hb.t[:], h2_d[gt * 128:(gt + 1) * 128, :], writes=[hb.k])
        for sl, yy in ((0, y0), (1, y1)):
            S.add("pool", lambda e, yy=yy, gt=gt, sl=sl: e.indirect_dma_start(
                out=yy.t[:, :], out_offset=None, in_=Ys, in_offset=bass.IndirectOffsetOnAxis(ap=dsti.t[:, gt, sl:sl + 1], axis=0)),
                [Ys_k, dsti.k], [yy.k], dma=True)
        cx.stt("dve", hb.t[:], y0.t[:], route.t[:, gt, 4:5], hb.t[:], ALU.mult, ALU.add, [y0.k, route.k, hb.k], [hb.k])
        cx.stt("dve", hb.t[:], y1.t[:], route.t[:, gt, 5:6], hb.t[:], ALU.mult, ALU.add, [y1.k, route.k, hb.k], [hb.k])
        cx.act(junk.t[:], hb.t[:], AF.Square, [hb.k], [junk.k, stb.k], accum_out=stb.t[:, 0:1])
        cx.act(stb.t[:, 1:2], stb.t[:, 0:1], AF.Ln, [stb.k], [stb.k], scale=1.0 / D, bias=EPS)
        cx.act(stb.t[:, 2:3], stb.t[:, 1:2], AF.Exp, [stb.k], [stb.k], scale=-0.5)
        cx.stt("dve", y0.t[:], hb.t[:], stb.t[:, 2:3], gfb.t[:], ALU.mult, ALU.mult, [hb.k, stb.k, gfb.k], [y0.k])
        cx.dma("sp", y_d[gt * 128:(gt + 1) * 128, :], y0.t[:], reads=[y0.k], out_dram=True)
    cx.close()


_DIN_CACHE = {}


def nc_input(nc, din, name, shape):
    key = (id(nc), name)
    if key not in _DIN_CACHE:
        _DIN_CACHE[key] = din(name, shape)
    return _DIN_CACHE[key]


def host_consts_s():
    t = np.arange(128)
    ch = t // 8
    same = ch[:, None] == ch[None, :]
    ucum = np.where(same & (t[:, None] <= t[None, :]), -1.0 / 16, 0.0).astype(np.float32)
    lrev = np.where(same & (t[:, None] > t[None, :]), -1.0 / 16, 0.0).astype(np.float32)
    amask = np.where(same & (t[:, None] <= t[None, :]), 1.0, 0.0).astype(np.float32)
    rowmask = (ch[:, None] == np.arange(16)[None, :]).astype(np.float32)
    slopes = np.array([2.0 ** (-8.0 * (h + 1) / 8) for h in range(8)], np.float64)

    def mfun(dist):
        m = ((dist >= 0) & (dist <= 128)).astype(np.float64)
        m += ((dist >= 0) & (dist <= 512) & (dist % 4 == 0))
        m += ((dist >= 0) & (dist <= 2048) & (dist % 16 == 0))
        return m
    ki = t[:, None, None, None]
    kb = np.arange(16)[None, None, :, None]
    j = np.arange(8)[None, None, None, :]
    dist = 2048 + j - (kb * 128 + ki)
    ms = mfun(dist) * np.exp(-slopes[None, :, None, None] * dist)
    c = np.arange(16)[None, None, :, None]
    jp = ki - 8 * c
    dist2 = j - jp
    mn = np.where((jp >= 0) & (jp < 8), mfun(dist2) * np.exp(-slopes[None, :, None, None] * np.maximum(dist2, 0)), 0.0)
    sel = np.zeros((8, 16, 128), np.float32)
    for cc in range(16):
        for jj in range(8):
            sel[jj, cc, 8 * cc + jj] = 1.0
    return {"c_ucum8": ucum, "c_lrev8": lrev, "c_amask8": amask, "c_rowmask": rowmask,
            "c_ms": ms.astype(np.float32).reshape(128, 8 * 16 * 8), "c_mn": mn.astype(np.float32).reshape(128, 8 * 16 * 8),
            "c_sel": sel.reshape(8, 16 * 128)}


def phase_as(nc, din, dout, h1s_d, c_ident):
    xs = din("xs", [128, D])
    ck = din("ck", [NSS, 2048, 512])
    cv = din("cv", [NSS, 2048, 512])
    sg = din("sg", [NSS, 4, 64, 128])
    g1 = nc_input(nc, din, "g_norm1", [1, D])
    w_in = nc_input(nc, din, "w_in", [D, DIN])
    w_gk2 = nc_input(nc, din, "w_gk2", [16, 256])
    b_gk = nc_input(nc, din, "b_gk", [1, 256])
    g_gla = nc_input(nc, din, "g_gla_out", [1, 128])
    w_out = nc_input(nc, din, "w_out", [D, D])
    c_ucum = din("c_ucum8", [128, 128])
    c_lrev = din("c_lrev8", [128, 128])
    c_amask = din("c_amask8", [128, 128])
    c_rowmask = din("c_rowmask", [128, 16])
    c_ms = din("c_ms", [128, 1024])
    c_mn = din("c_mn", [128, 1024])
    c_sel = nc_input(nc, din, "c_sel", [8, 2048])
    o_swak = dout("o_swak_s", [128, 512])
    o_swav = dout("o_swav_s", [128, 512])
    o_glas = dout("o_glas_s", [NSS, 4, 64, 128])

    cx = Ctx(nc)
    S = cx.S
    ident_b = cx.sb([128, 128], BF16, "s_ident_b")
    ident_f = cx.sb([128, 128], F32, "s_ident_f")
    ucum = cx.sb([128, 128], F32, "s_ucum")
    lrev = cx.sb([128, 128], F32, "s_lrev")
    amask = cx.sb([128, 128], F32, "s_amask")
    rowmask = cx.sb([128, 16], F32, "s_rowmask")
    ms = cx.sb([128, 8, 128], BF16, "s_ms")
    mn = cx.sb([128, 8, 16, 8], BF16, "s_mn")
    sel = cx.sb([8, 16, 128], BF16, "s_sel")
    g1b = cx.sb([128, D], F32, "s_g1b")
    gglab = cx.sb([128, 128], F32, "s_gglab")
    win = cx.sb([128, 8, DIN], BF16, "s_win")
    wout = cx.sb([128, 8, D], BF16, "s_wout")
    wgk = cx.sb([32, 256], BF16, "s_wgk")
    gklrT = cx.sb([32, 128], BF16, "s_gklrT")
    S0f = cx.sb([128, NSS, 2, 128], F32, "s_S0f")
    S0b = cx.sb([128, NSS, 2, 128], BF16, "s_S0b")
    stg = cx.sb([128, 16, 512], BF16, "s_stg")
    kcT = [cx.sb([128, 4, 2048], BF16, f"s_kcT{i}") for i in range(1)]
    vca = [cx.sb([128, 16, 8, 65], BF16, f"s_vca{i}") for i in range(1)]
    Vs = cx.sb([128, 8, 65], BF16, "s_Vs")

    cx.dma("pool", ident_b.t[:], c_ident, writes=[ident_b.k])
    cx.dma("sp", ident_f.t[:], c_ident, writes=[ident_f.k])
    cx.dma("sp", ucum.t[:], c_ucum, writes=[ucum.k])
    cx.dma("sp", lrev.t[:], c_lrev, writes=[lrev.k])
    cx.dma("sp", amask.t[:], c_amask, writes=[amask.k])
    cx.dma("sp", rowmask.t[:], c_rowmask, writes=[rowmask.k])
    cx.dma("pool", ms.t[:].rearrange("p h x -> p (h x)"), c_ms, writes=[ms.k])
    cx.dma("pool", mn.t[:].rearrange("p h c j -> p (h c j)"), c_mn, writes=[mn.k])
    cx.dma("pool", sel.t[:].rearrange("p c t -> p (c t)"), c_sel, writes=[sel.k])
    cx.dma("sp", g1b.t[:], g1.partition_broadcast(128), writes=[g1b.k])
    cx.dma("sp", gglab.t[:], g_gla.partition_broadcast(128), writes=[gglab.k])
    w_in_v = w_in.rearrange("(k p) f -> p k f", p=128)
    for k in range(8):
        for hh in range(2):
            c0 = hh * 1544
            cx.dma("pool", win.t[:, k, c0:c0 + 1544], w_in_v[:, k, c0:c0 + 1544], writes=[win.k])
    w_out_v = w_out.rearrange("(k p) f -> p k f", p=128)
    for k in range(8):
        cx.dma("pool", wout.t[:, k, :], w_out_v[:, k, :], writes=[wout.k])
    cx.dma("pool", wgk.t[0:16, :], w_gk2, writes=[wgk.k])
    cx.dma("pool", wgk.t[16:17, :], b_gk, writes=[wgk.k])
    S.add("pool", lambda e: e.memset(gklrT.t[:], 1.0), (), [gklrT.k])
    S.add("pool", lambda e: e.memset(Vs.t[:], 1.0), (), [Vs.k])
    for v_ in vca:
        S.add("pool", lambda e, v_=v_: e.memset(v_.t[:], 1.0), (), [v_.k])
    cx.dma("sp", S0f.t[:], sg.rearrange("c (p h) k v -> (h k) c p v", h=2), writes=[S0f.k])
    cx.copy("act", S0b.t[:], S0f.t[:], [S0f.k], [S0b.k])

    psT = cx.ps([128, 1024], BF16, "s_psT")
    psR = [cx.ps([128, 512], F32, f"s_psR{i}") for i in range(2)]
    psSb = [cx.ps([128, 512], F32, f"s_psS{i}") for i in range(2)]
    psD = [cx.ps([128, 512], F32, f"s_psD{i}") for i in range(2)]
    psSel = cx.ps([128, 512], F32, "s_psSel")
    rr = [0]

    def next_ps():
        b = psR[rr[0] % 2]
        rr[0] += 1
        return b

    xb = cx.sb([128, D], F32, "s_x")
    junk = cx.sb([128, D], BF16, "s_junk")
    stb = cx.sb([128, 8], F32, "s_stat")
    nb = cx.sb([128, D], BF16, "s_n")
    nT = cx.sb([128, 8, 128], BF16, "s_nT")
    qkTa = cx.sb([128, 4, 128], BF16, "s_qkTa")
    qTb = cx.sb([128, 4, 2, 128], BF16, "s_qTb")
    kTs = cx.sb([128, 4, 128], BF16, "s_kTs")
    va = cx.sb([128, 512], BF16, "s_va")
    ka = cx.sb([128, 256], F32, "s_ka")
    sr = cx.sb([128, 512], F32, "s_sr")
    kbf = cx.sb([128, 512], F32, "s_kbf")
    vbf = cx.sb([128, 512], F32, "s_vbf")
    e1 = cx.sb([128, 256], F32, "s_e1")
    gg = cx.sb([128, 256], F32, "s_g")
    ebT = cx.sb([128, 2, 128], F32, "s_ebT")
    enbT = cx.sb([128, 2, 128], F32, "s_enbT")
    ed = cx.sb([128, 256], F32, "s_ed")
    qtT = cx.sb([128, 2, 2, 128], BF16, "s_qtT")
    ktT = cx.sb([128, 2, 128], BF16, "s_ktT")
    khat = cx.sb([128, 256], BF16, "s_khat")
    khm = [cx.sb([128, 256], BF16, f"s_khm{i}") for i in range(2)]
    Am = [cx.sb([128, 128], BF16, f"s_Am{i}") for i in range(4)]
    oTf = cx.sb([128, 4, 128], F32, "s_oTf")
    gs = cx.sb([128, 8], F32, "s_gs")
    gtmp = [cx.sb([128, 128], F32, f"s_gtmp{i}") for i in range(2)]
    oc = cx.sb([128, D], BF16, "s_ocat")
    Sn = [cx.sb([128, 2, 128], F32, f"s_Sn{i}") for i in range(2)]
    pe_t = [cx.sb([128, 136], BF16, f"s_pe{i}") for i in range(2)]
    pm_t = [cx.sb([128, 136], BF16, f"s_pm{i}") for i in range(2)]
    rden = [cx.sb([8, 1], F32, f"s_rden{i}") for i in range(2)]
    occ = [cx.sb([8, 512], BF16, f"s_occ{i}") for i in range(2)]
    oT = cx.sb([128, 8, 128], BF16, "s_oT")
    for b_ in (qTb, qtT):
        S.add("pool", lambda e, b_=b_: e.memset(b_.t[:], 0.0), (), [b_.k])

    def transpose8(src, dst):
        for k in range(8):
            cx.tr(psT.t[:, k * 128:(k + 1) * 128], src.t[:, k * 128:(k + 1) * 128], ident_b.t[:], [src.k, ident_b.k], [psT.k])
        cx.copy("act", dst.t[:].rearrange("p k t -> p (k t)"), psT.t[:], [psT.k], [dst.k])

    def proj_fm(col0, nchunk, ps, m=128):
        for j in range(nchunk):
            for k in range(8):
                cx.mm(ps.t[0:m, j * 128:(j + 1) * 128], win.t[:, k, col0 + j * 128: col0 + j * 128 + m], nT.t[:, k, :],
                      k == 0, k == 7, [win.k, nT.k], [ps.k])

    def proj_tm(col0, ncol, ps):
        for k in range(8):
            cx.mm(ps.t[:, 0:ncol], nT.t[:, k, :], win.t[:, k, col0:col0 + ncol], k == 0, k == 7, [win.k, nT.k], [ps.k])

    cx.dma("sp", xb.t[:], xs, writes=[xb.k])
    cx.act(junk.t[:], xb.t[:], AF.Square, [xb.k], [junk.k, stb.k], accum_out=stb.t[:, 0:1])
    cx.act(stb.t[:, 1:2], stb.t[:, 0:1], AF.Ln, [stb.k], [stb.k], scale=1.0 / D, bias=EPS)
    cx.act(stb.t[:, 2:3], stb.t[:, 1:2], AF.Exp, [stb.k], [stb.k], scale=-0.5)
    cx.stt("dve", nb.t[:], xb.t[:], stb.t[:, 2:3], g1b.t[:], ALU.mult, ALU.mult, [xb.k, stb.k, g1b.k], [nb.k])
    transpose8(nb, nT)
    ps = next_ps()
    proj_fm(C_QA, 4, ps)
    cx.copy("act", qkTa.t[:].rearrange("p k t -> p (k t)"), ps.t[:], [ps.k], [qkTa.k])
    ps = next_ps()
    proj_fm(C_QB, 4, ps)
    psv = ps.t[:].rearrange("p (k t) -> p k t", k=4)
    cx.copy("act", qTb.t[0:64, :, 0, :], psv[0:64], [ps.k], [qTb.k])
    cx.copy("act", qTb.t[64:128, :, 1, :], psv[64:128], [ps.k], [qTb.k])
    ps = next_ps()
    proj_fm(C_KB, 4, ps)
    cx.copy("dve", kTs.t[:].rearrange("p k t -> p (k t)"), ps.t[:], [ps.k], [kTs.k])
    ps = next_ps()
    proj_fm(C_GK, 1, ps, m=16)
    cx.copy("dve", gklrT.t[0:16, :], ps.t[0:16, 0:128], [ps.k], [gklrT.k])
    ps = next_ps()
    proj_tm(C_VA, 512, ps)
    cx.copy("act", va.t[:], ps.t[:], [ps.k], [va.k])
    ps = next_ps()
    proj_tm(C_KA, 256, ps)
    cx.copy("dve", ka.t[:], ps.t[:, 0:256], [ps.k], [ka.k])
    ps = next_ps()
    proj_tm(C_RA, 512, ps)
    cx.act(sr.t[:], ps.t[:], AF.Silu, [ps.k], [sr.k])
    ps = next_ps()
    proj_tm(C_KB, 512, ps)
    cx.copy("act", kbf.t[:], ps.t[:], [ps.k], [kbf.k])
    cx.dma("sp", o_swak, kbf.t[:], reads=[kbf.k], out_dram=True)
    ps = next_ps()
    proj_tm(C_VB, 512, ps)
    cx.copy("dve", Vs.t[:, :, 0:64], ps.t[:].rearrange("p (h d) -> p h d", h=8), [ps.k], [Vs.k])
    cx.copy("act", vbf.t[:], ps.t[:], [ps.k], [vbf.k])
    cx.dma("sp", o_swav, vbf.t[:], reads=[vbf.k], out_dram=True)

    ps = next_ps()
    cx.mm(ps.t[:, 0:256], gklrT.t[0:17, :], wgk.t[0:17, :], True, True, [gklrT.k, wgk.k], [ps.k])
    cx.act(e1.t[:], ps.t[:, 0:256], AF.Exp, [ps.k], [e1.k], scale=-1.0)
    cx.act(gg.t[:], e1.t[:], AF.Ln, [e1.k], [gg.k], bias=1.0, scale=1.0)
    ps = next_ps()
    for p in range(2):
        cx.mm(ps.t[:, p * 128:(p + 1) * 128], gg.t[:, p * 128:(p + 1) * 128], ucum.t[:], True, True, [gg.k, ucum.k], [ps.k])
    cx.mm(ps.t[:, 256:512], lrev.t[:], gg.t[:], True, True, [gg.k, lrev.k], [ps.k])
    cx.act(ebT.t[:].rearrange("p k t -> p (k t)"), ps.t[:, 0:256], AF.Exp, [ps.k], [ebT.k])
    cx.act(enbT.t[:].rearrange("p k t -> p (k t)"), ps.t[:, 0:256], AF.Exp, [ps.k], [enbT.k], scale=-1.0)
    cx.act(ed.t[:], ps.t[:, 256:512], AF.Exp, [ps.k], [ed.k])
    for hh in range(2):
        pr = slice(hh * 64, hh * 64 + 64)
        cx.stt("dve", qtT.t[pr, :, hh, :], qkTa.t[pr, 0:2, :], 0.125, ebT.t[pr], ALU.mult, ALU.mult, [qkTa.k, ebT.k], [qtT.k])
    cx.tt("dve", ktT.t[:], qkTa.t[:, 2:4, :], enbT.t[:], ALU.mult, [qkTa.k, enbT.k], [ktT.k])
    cx.tt("dve", khat.t[:], ka.t[:], ed.t[:], ALU.mult, [ka.k, ed.k], [khat.k])
    for h in range(4):
        p = h // 2
        pss = psSb[h % 2]
        cx.mm(pss.t[:, 0:128], ktT.t[:, p, :], qtT.t[:, p, h % 2, :], True, True, [ktT.k, qtT.k], [pss.k])
        cx.tt("dve", Am[h].t[:], pss.t[:, 0:128], amask.t[:], ALU.mult, [pss.k, amask.k], [Am[h].k])
    psO = psD[0]
    for h in range(4):
        p = h // 2
        cx.mm(psO.t[:, h * 128:(h + 1) * 128], va.t[:, h * 128:(h + 1) * 128], Am[h].t[:], True, False, [va.k, Am[h].k], [psO.k])
        for c in range(NSS):
            cx.mm(psO.t[:, h * 128 + 8 * c:h * 128 + 8 * c + 8], S0b.t[:, c, p, :], qtT.t[:, p, h % 2, 8 * c:8 * c + 8], False, c == NSS - 1,
                  [S0b.k, qtT.k], [psO.k])
    cx.copy("act", oTf.t[:].rearrange("p h t -> p (h t)"), psO.t[:], [psO.k], [oTf.k])
    psGO = psD[1]
    for h in range(4):
        cx.tr(psGO.t[:, h * 128:(h + 1) * 128], oTf.t[:, h, :], ident_f.t[:], [oTf.k, ident_f.k], [psGO.k])
    for h in range(4):
        cx.act(junk.t[:, 0:128], psGO.t[:, h * 128:(h + 1) * 128], AF.Square, [psGO.k], [junk.k, gs.k], accum_out=gs.t[:, h:h + 1])
    cx.act(gs.t[:, 4:8], gs.t[:, 0:4], AF.Ln, [gs.k], [gs.k], scale=1.0 / 128, bias=EPS)
    cx.act(gs.t[:, 0:4], gs.t[:, 4:8], AF.Exp, [gs.k], [gs.k], scale=-0.5)
    for h in range(4):
        gt_ = gtmp[h % 2]
        cx.stt("dve", gt_.t[:], psGO.t[:, h * 128:(h + 1) * 128], gs.t[:, h:h + 1], gglab.t[:], ALU.mult, ALU.mult, [psGO.k, gs.k, gglab.k], [gt_.k])
        cx.tt("dve", oc.t[:, h * 128:(h + 1) * 128], gt_.t[:], sr.t[:, h * 128:(h + 1) * 128], ALU.mult, [gt_.k, sr.k], [oc.k])
    for c in range(NSS):
        km = khm[c % 2]
        cx.ts("dve", km.t[:], khat.t[:], rowmask.t[:, c:c + 1], None, ALU.mult, None, [khat.k, rowmask.k], [km.k])
        psU = next_ps()
        for h in range(4):
            p, base = h // 2, (h % 2) * 64
            cx.mm(psU.t[base:base + 64, p * 128:(p + 1) * 128], km.t[:, h * 64:(h + 1) * 64], va.t[:, h * 128:(h + 1) * 128], True, True,
                  [km.k, va.k], [psU.k])
        sn = Sn[c % 2]
        for p in range(2):
            cx.stt("dve", sn.t[:, p, :], S0f.t[:, c, p, :], ebT.t[:, p, 8 * c + 7:8 * c + 8], psU.t[:, p * 128:(p + 1) * 128],
                   ALU.mult, ALU.add, [S0f.k, ebT.k, psU.k], [sn.k])
        cx.dma("sp", o_glas[c].rearrange("(p h) k v -> (h k) p v", h=2), sn.t[:], reads=[sn.k], out_dram=True)

    it = 0
    for c in range(NSS):
        kT_, va_ = kcT[0], vca[0]
        cx.dma("pool", stg.t[:], ck[c].rearrange("(b p) f -> p b f", p=128), writes=[stg.k])
        for kb in range(16):
            for c4 in range(4):
                cx.tr(psT.t[:, c4 * 128:(c4 + 1) * 128], stg.t[:, kb, c4 * 128:(c4 + 1) * 128], ident_b.t[:], [stg.k, ident_b.k], [psT.k])
            cx.copy("act" if kb % 2 else "dve", kT_.t[:, :, kb * 128:(kb + 1) * 128], psT.t[:, 0:512].rearrange("p (k t) -> p k t", k=4), [psT.k], [kT_.k])
        cx.dma("pool", stg.t[:], cv[c].rearrange("(b p) f -> p b f", p=128), writes=[stg.k])
        cx.copy("act", va_.t[:, :, :, 0:64], stg.t[:].rearrange("p b (h d) -> p b h d", h=8), [stg.k], [va_.k])
        ocb = occ[c % 2]
        for h in range(8):
            c4, par = h // 2, h % 2
            pss, pd = psSb[it % 2], psD[it % 2]
            pe_, pm_, rd = pe_t[it % 2], pm_t[it % 2], rden[it % 2]
            it += 1
            rhs = qTb.t[:, c4, par, 8 * c:8 * c + 8]
            for kb in range(16):
                cx.mm(pss.t[:, kb * 8:(kb + 1) * 8], kT_.t[:, c4, kb * 128:(kb + 1) * 128], rhs, True, True, [kT_.k, qTb.k], [pss.k])
            cx.mm(pss.t[:, 128:136], kTs.t[:, c4, :], rhs, True, True, [kTs.k, qTb.k], [pss.k])
            cx.act(pe_.t[:], pss.t[:, 0:136], AF.Exp, [pss.k], [pe_.k], scale=0.125)
            cx.tt("dve", pm_.t[:, 0:128], pe_.t[:, 0:128], ms.t[:, h, :], ALU.mult, [pe_.k, ms.k], [pm_.k])
            cx.tt("dve", pm_.t[:, 128:136], pe_.t[:, 128:136], mn.t[:, h, c, :], ALU.mult, [pe_.k, mn.k], [pm_.k])
            for kb in range(16):
                cx.mm(pd.t[0:8, 0:65], pm_.t[:, kb * 8:(kb + 1) * 8], va_.t[:, kb, h, :], kb == 0, False, [pm_.k, va_.k], [pd.k])
            cx.mm(pd.t[0:8, 0:65], pm_.t[:, 128:136], Vs.t[:, h, :], False, True, [pm_.k, Vs.k], [pd.k])
            S.add("dve", lambda e, rd=rd, pd=pd: e.reciprocal(out=rd.t[:], in_=pd.t[0:8, 64:65]), [pd.k], [rd.k])
            cx.ts("dve", ocb.t[:, h * 64:(h + 1) * 64], pd.t[0:8, 0:64], rd.t[:], None, ALU.mult, None, [pd.k, rd.k], [ocb.k])
        cx.mm(psSel.t[:], sel.t[:, c, :], ocb.t[:], c == 0, c == NSS - 1, [sel.k, ocb.k], [psSel.k])
    cx.copy("act", oc.t[:, 512:1024], psSel.t[:], [psSel.k], [oc.k])

    transpose8(oc, oT)
    for half in range(2):
        ps = next_ps()
        for k in range(8):
            cx.mm(ps.t[:], oT.t[:, k, :], wout.t[:, k, half * 512:(half + 1) * 512], k == 0, k == 7, [oT.k, wout.k], [ps.k])
        cx.tt("dve", xb.t[:, half * 512:(half + 1) * 512], ps.t[:], xb.t[:, half * 512:(half + 1) * 512], ALU.add, [ps.k, xb.k], [xb.k])
    cx.dma("sp", h1s_d, xb.t[:], reads=[xb.k], out_dram=True)
    cx.close()


_NC_CACHE = {}


def kernel(**inputs):
    n = 8
    if "nc" not in _NC_CACHE:
        _NC_CACHE["nc"] = build(NT=NT_FULL, nseq=NSEQ, debug=False, phases="ASBC")
    nc = _NC_CACHE["nc"]
    f32 = lambda a: np.ascontiguousarray(np.asarray(a, dtype=np.float32))
    I = {k: np.asarray(v) for k, v in inputs.items()}
    shared = {
        "g_norm1": f32(I["g_norm1"]), "w_in": f32(I["w_in"][0]), "w_gk2": f32(I["w_gk2"][0]),
        "b_gk": f32(I["b_gk"]), "g_gla_out": f32(I["g_gla_out"]), "w_out": f32(I["w_out"][0]),
        "g_norm2": f32(I["g_norm2"]), "g_mem": f32(I["g_mem"]), "g_norm3": f32(I["g_norm3"]),
        "w_cq": f32(I["w_cq"][0]), "w_mk": f32(I["w_mk"][0]), "w_mv": f32(I["w_mv"][0]), "w_co": f32(I["w_co"][0]),
        "w_r": f32(np.concatenate([I["w_gr"][0], I["w_er"][0]], axis=1)),
        "b_r": f32(np.concatenate([I["b_gr"], I["b_er"]], axis=1)),
        "w_e1": f32(I["w_e1"][0]).reshape(32 * D, 512), "w_e3": f32(I["w_e3"][0]).reshape(32 * D, 512),
        "w_e2": f32(I["w_e2"][0]).reshape(32 * 512, D), "g_final": f32(I["g_final"]).reshape(1, D),
    }
    shared.update(host_consts())
    shared.update(host_consts_b())
    shared.update(host_consts_c())
    shared.update(host_consts_s())
    in_maps = []
    for i in range(n):
        m = dict(shared)
        ps, ss = slice(NSEQ * i, NSEQ * (i + 1)), slice(NSS * i, NSS * (i + 1))
        m["xp"] = f32(I["x_prompt"][ps])
        m["memp"] = f32(I["mem_prompt"][ps])
        m["xs"] = f32(I["x_sample"][ss]).reshape(128, D)
        m["ck"] = f32(I["cache_swa_k"][0, ss]).reshape(NSS, 2048, 512)
        m["cv"] = f32(I["cache_swa_v"][0, ss]).reshape(NSS, 2048, 512)
        m["sg"] = f32(I["state_gla"][0, ss])
        m["cmk"] = f32(I["cache_mem_k"][0, ss]).reshape(NSS, 256, D)
        m["cmv"] = f32(I["cache_mem_v"][0, ss]).reshape(NSS, 256, D)
        in_maps.append(m)
    res = run_bass_kernel_spmd(nc, in_maps, core_ids=list(range(n)))
    rs = res.results
    npr = NSEQ * NT_FULL * 128
    cat = lambda key: np.concatenate([np.asarray(r[key], dtype=np.float32) for r in rs], axis=0)
    y_prompt = np.concatenate([np.asarray(r["y"][:npr], dtype=np.float32).reshape(NSEQ, NT_FULL * 128, D) for r in rs], axis=0)
    y_sample = np.concatenate([np.asarray(r["y"][npr:npr + 128], dtype=np.float32).reshape(NSS, 8, D) for r in rs], axis=0)
    return (y_prompt, y_sample,
            cat("o_swak").reshape(1, 16, 2048, 8, 64), cat("o_swav").reshape(1, 16, 2048, 8, 64),
            cat("o_glas").reshape(1, 16, 4, 64, 128),
            cat("o_memk").reshape(1, 16, 256, 4, 256), cat("o_memv").reshape(1, 16, 256, 4, 256),
            cat("o_swak_s").reshape(1, 128, 8, 8, 64), cat("o_swav_s").reshape(1, 128, 8, 8, 64),
            cat("o_glas_s").reshape(1, 128, 4, 64, 128))
```

```python
from contextlib import ExitStack
import numpy as np
import concourse.bass as bass
import concourse.mybir as mybir
from concourse.bass_utils import run_bass_kernel_spmd

F32 = mybir.dt.float32
BF16 = mybir.dt.bfloat16
I32 = mybir.dt.int32
U32 = mybir.dt.uint32
AF = mybir.ActivationFunctionType
ALU = mybir.AluOpType
AX = mybir.AxisListType

ENGS = ("pe", "act", "dve", "pool", "sp")
SEM_LIMIT = 30000
DMA_RING = 8


class Tok:
    __slots__ = ("w", "rs", "name", "excl")

    def __init__(self, name="", excl=False):
        self.w = None
        self.rs = []
        self.name = name
        self.excl = excl


class Op:
    __slots__ = ("eng", "fn", "dma", "deps", "signal", "sem", "val", "gate")

    def __init__(self, eng, fn, dma):
        self.eng = eng
        self.fn = fn
        self.dma = dma
        self.deps = []
        self.signal = dma
        self.sem = None
        self.val = None
        self.gate = None


class Sched:
    def __init__(self, nc):
        self.nc = nc
        self.ops = {e: [] for e in ENGS}
        self.dma_ops = {e: [] for e in ENGS}
        self.out_dmas = []
        import os
        self.cut = int(os.environ.get("KCUT", "0")) or None
        self.total = 0

    def add(self, eng, fn, reads=(), writes=(), dma=False, out=False):
        self.total += 1
        if self.cut is not None and self.total > self.cut:
            return None
        op = Op(eng, fn, dma)
        deps = []
        ex = [t for t in reads if t.excl]
        if ex:
            reads = [t for t in reads if not t.excl]
            writes = list(writes) + [t for t in ex if t not in writes]
        for t in reads:
            if t.w is not None:
                deps.append(t.w)
        for t in writes:
            deps.extend(t.rs)
            if t.w is not None:
                deps.append(t.w)
        seen = set()
        flat = []
        for d in deps:
            if d.fn is None:
                flat.extend(d.deps)
            else:
                flat.append(d)
        for d in flat:
            if id(d) in seen:
                continue
            seen.add(id(d))
            if fn is None or d.dma or d.eng != eng or eng != "pe":
                op.deps.append(d)
                d.signal = True
        for t in reads:
            t.rs.append(op)
        for t in writes:
            t.w = op
            t.rs = []
        if dma:
            lst = self.dma_ops[eng]
            if len(lst) >= DMA_RING:
                op.gate = lst[len(lst) - DMA_RING]
            lst.append(op)
            if out:
                self.out_dmas.append(op)
        if fn is not None:
            self.ops[eng].append(op)
        return op

    def finish(self):
        op = Op("sp", None, False)
        op.deps = list(self.out_dmas)
        self.ops["sp"].append(op)

    def emit(self):
        nc = self.nc
        with ExitStack() as st:
            nsig = {e: sum(1 for o in self.ops[e] if o.signal and not o.dma) for e in ENGS}
            esems = {}
            for e in ENGS:
                k = nsig[e] // SEM_LIMIT + 1
                esems[e] = [st.enter_context(nc.semaphore(f"s_{e}_{i}")) for i in range(k)]
            dsems = {}
            for e in ENGS:
                if self.dma_ops[e]:
                    dsems[e] = [st.enter_context(nc.semaphore(f"d_{e}_{i}")) for i in range(DMA_RING)]
            for e in ENGS:
                c = 0
                for o in self.ops[e]:
                    if o.dma:
                        continue
                    if o.signal:
                        o.sem = esems[e][c // SEM_LIMIT]
                        o.val = c % SEM_LIMIT + 1
                        c += 1
                for n, o in enumerate(self.dma_ops[e]):
                    o.sem = dsems[e][n % DMA_RING]
                    o.val = 16 * (n // DMA_RING + 1)
            block = st.enter_context(nc.Block())

            def run(e, name):
                waited = {}
                for o in self.ops[name]:
                    ds = list(o.deps)
                    if o.gate is not None:
                        ds.append(o.gate)
                    for d in ds:
                        k = id(d.sem)
                        if waited.get(k, 0) >= d.val:
                            continue
                        waited[k] = d.val
                        e.wait_ge(d.sem, d.val)
                    if o.fn is None:
                        continue
                    ins = o.fn(e)
                    if o.signal:
                        ins.then_inc(o.sem, 16 if o.dma else 1)

            @block.tensor
            def _(e):
                run(e, "pe")

            @block.scalar
            def _(e):
                run(e, "act")

            @block.vector
            def _(e):
                run(e, "dve")

            @block.gpsimd
            def _(e):
                run(e, "pool")

            @block.sync
            def _(e):
                run(e, "sp")


class Buf:
    def __init__(self, t, name=""):
        self.t = t
        self.k = Tok(name)


class Ctx:
    def __init__(self, nc):
        self.nc = nc
        self.st = ExitStack()
        self.S = Sched(nc)
        self.n = 0

    def sb(self, shape, dt, name=None):
        self.n += 1
        name = name or f"sb{self.n}"
        return Buf(self.st.enter_context(self.nc.sbuf_tensor(name, list(shape), dt)), name)

    def ps(self, shape, dt, name=None):
        self.n += 1
        name = name or f"ps{self.n}"
        b = Buf(self.st.enter_context(self.nc.psum_tensor(name, list(shape), dt)), name)
        b.k.excl = True
        return b

    def dma(self, q, out, in_, reads=(), writes=(), out_dram=False, **kw):
        return self.S.add(q, lambda e: e.dma_start(out=out, in_=in_, **kw), reads, writes, dma=True, out=out_dram)

    def mm(self, out, lhsT, rhs, start, stop, reads, writes):
        return self.S.add("pe", lambda e: e.matmul(out, lhsT=lhsT, rhs=rhs, start=start, stop=stop), reads, writes)

    def tr(self, out, in_, ident, reads, writes):
        return self.S.add("pe", lambda e: e.transpose(out, in_, ident), reads, writes)

    def act(self, out, in_, func, reads, writes, **kw):
        return self.S.add("act", lambda e: e.activation(out=out, in_=in_, func=func, **kw), reads, writes)

    def copy(self, eng, out, in_, reads, writes):
        if eng == "act":
            return self.S.add("act", lambda e: e.copy(out=out, in_=in_), reads, writes)
        return self.S.add(eng, lambda e: e.tensor_copy(out=out, in_=in_), reads, writes)

    def tt(self, eng, out, in0, in1, op, reads, writes):
        return self.S.add(eng, lambda e: e.tensor_tensor(out=out, in0=in0, in1=in1, op=op), reads, writes)

    def ts(self, eng, out, in0, s1, s2, op0, op1, reads, writes, **kw):
        if s2 is None:
            return self.S.add(eng, lambda e: e.tensor_scalar(out=out, in0=in0, scalar1=s1, scalar2=None, op0=op0, **kw), reads, writes)
        return self.S.add(eng, lambda e: e.tensor_scalar(out=out, in0=in0, scalar1=s1, scalar2=s2, op0=op0, op1=op1, **kw), reads, writes)

    def stt(self, eng, out, in0, scalar, in1, op0, op1, reads, writes):
        return self.S.add(eng, lambda e: e.scalar_tensor_tensor(out=out, in0=in0, scalar=scalar, in1=in1, op0=op0, op1=op1), reads, writes)

    def close(self):
        self.S.finish()
        self.S.emit()
        self.st.close()


def interleave(gens, width=2):
    active = []
    for g in gens:
        active.append(g)
        while len(active) >= width:
            for a in list(active):
                try:
                    next(a)
                except StopIteration:
                    active.remove(a)
    while active:
        for a in list(active):
            try:
                next(a)
            except StopIteration:
                active.remove(a)

D = 1024
DIN = 3088
NT_FULL = 32
NSEQ = 2
NSS = 16
NR = 18
EPS = 1e-6
C_QA, C_KA, C_VA, C_GK, C_RA, C_QB, C_KB, C_VB = 0, 256, 512, 1024, 1040, 1552, 2064, 2576


def host_consts():
    t = np.arange(128)
    ch = t // 64
    same = ch[:, None] == ch[None, :]
    ucum = np.where(same & (t[:, None] <= t[None, :]), -1.0 / 16, 0.0).astype(np.float32)
    lrev = np.where(same & (t[:, None] > t[None, :]), -1.0 / 16, 0.0).astype(np.float32)
    amask = np.where(same & (t[:, None] <= t[None, :]), 1.0, 0.0).astype(np.float32)
    dm = np.zeros((17, 128, 128), np.float32)
    for db in range(17):
        dist = db * 128 + t[None, :] - t[:, None]
        m = ((dist >= 0) & (dist <= 128)).astype(np.float32)
        m += ((dist >= 0) & (dist <= 512) & (dist % 4 == 0))
        m += ((dist >= 0) & (dist <= 2048) & (dist % 16 == 0))
        dm[db] = m
    slopes = np.array([2.0 ** (-8.0 * (h + 1) / 8) for h in range(8)], np.float64)
    ab = np.zeros((128, 8, 17), np.float32)
    for h in range(8):
        for db in range(17):
            ab[:, h, db] = slopes[h] * (t - 64 - 128 * db)
    dmf = np.zeros((128, 8, 17, 128), np.float32)
    for db in range(17):
        dist = db * 128 + t[None, :] - t[:, None]
        for h in range(8):
            dmf[:, h, 16 - db, :] = dm[db] * np.exp(-slopes[h] * np.maximum(dist, 0))
    return {
        "c_ident": np.eye(128, dtype=np.float32),
        "c_ucum": ucum, "c_lrev": lrev, "c_amask": amask,
        "c_dmf": dmf.reshape(128, 8 * 17 * 128),
    }


def build(NT=NT_FULL, nseq=NSEQ, debug=False, phases="AB"):
    nc = bass.Bass("TRN2", target_bir_lowering=False)

    def din(name, shape, dt=F32):
        return nc.dram_tensor(name, list(shape), dt, kind="ExternalInput").ap()

    def dout(name, shape, dt=F32):
        return nc.dram_tensor(name, list(shape), dt, kind="ExternalOutput").ap()

    T = NT * 128
    c_ident = din("c_ident", [128, 128])
    samp = "S" in phases
    if "A" in phases:
        h1_d = dout("h1", [nseq, T, D]) if debug else nc.dram_tensor("h1", [nseq, T, D], F32).ap()
        phase_a(nc, din, dout, NT, nseq, h1_d, c_ident)
    else:
        h1_d = din("h1", [nseq, T, D])
    h1s_d = None
    if samp:
        h1s_d = dout("h1s", [128, D]) if debug else nc.dram_tensor("h1s", [128, D], F32).ap()
        phase_as(nc, din, dout, h1s_d, c_ident)
    NTOT = nseq * NT + (1 if samp else 0)
    if "B" in phases:
        h2_d, xn_d, route_d, cnt_d = phase_b(nc, din, dout, NT, nseq, h1_d, c_ident, debug, h1s_d)
    elif "C" in phases:
        h2_d = din("h2", [NTOT * 128, D])
        xn_d = din("xn", [NTOT * 128, D], BF16)
        route_d = din("route", [128, NTOT, 8])
        cnt_d = din("cnt", [128, 32])
    if "C" in phases:
        y_d = dout("y", [NTOT * 128, D])
        phase_c(nc, din, dout, NTOT, h2_d, xn_d, route_d, cnt_d, c_ident, y_d)
    return nc


def phase_a(nc, din, dout, NT, nseq, h1_d, c_ident):
    T = NT * 128
    KEEP = min(2048, T)
    KT0 = NT - KEEP // 128
    xp = din("xp", [nseq, T, D])
    g1 = nc_input(nc, din, "g_norm1", [1, D])
    w_in = nc_input(nc, din, "w_in", [D, DIN])
    w_gk2 = nc_input(nc, din, "w_gk2", [16, 256])
    b_gk = nc_input(nc, din, "b_gk", [1, 256])
    g_gla = nc_input(nc, din, "g_gla_out", [1, 128])
    w_out = nc_input(nc, din, "w_out", [D, D])
    c_ucum = din("c_ucum", [128, 128])
    c_lrev = din("c_lrev", [128, 128])
    c_amask = din("c_amask", [128, 128])
    c_dmf = din("c_dmf", [128, 8 * 17 * 128])

    o_swak = dout("o_swak", [nseq, KEEP, 512])
    o_swav = dout("o_swav", [nseq, KEEP, 512])
    o_glas = dout("o_glas", [nseq, 4, 64, 128])
    cx = Ctx(nc)
    S = cx.S
    ident_b = cx.sb([128, 128], BF16, "ident_b")
    ucum = cx.sb([128, 128], F32, "ucum")
    lrev = cx.sb([128, 128], F32, "lrev")
    amask = cx.sb([128, 128], F32, "amask")
    dmf = cx.sb([128, 8, 17, 128], BF16, "dmf")
    g1b = cx.sb([128, D], F32, "g1b")
    gglab = cx.sb([128, 128], F32, "gglab")
    win = cx.sb([128, 8, DIN], BF16, "win")
    wout = cx.sb([128, 8, D], BF16, "wout")
    wgk = cx.sb([32, 256], BF16, "wgk")
    kTr = cx.sb([128, 4, NR * 128], BF16, "kTr")
    Vr = cx.sb([128, NR, 8, 65], BF16, "Vr")
    gklrT2 = [cx.sb([32, 128], BF16, f"gklrT{i}") for i in range(2)]
    Sst = cx.sb([128, 2, 128], F32, "Sst")
    Sb = [cx.sb([128, 2, 128], BF16, f"Sb{i}") for i in range(8)]

    cx.dma("pool", ident_b.t[:], c_ident, writes=[ident_b.k])
    cx.dma("sp", ucum.t[:], c_ucum, writes=[ucum.k])
    cx.dma("sp", lrev.t[:], c_lrev, writes=[lrev.k])
    cx.dma("sp", amask.t[:], c_amask, writes=[amask.k])
    c_dmf_v = c_dmf.rearrange("p (h r q) -> p h r q", h=8, r=17)
    for h in range(8):
        cx.dma("pool", dmf.t[:, h, 0:9, :], c_dmf_v[:, h, 0:9, :], writes=[dmf.k])
        cx.dma("pool", dmf.t[:, h, 9:17, :], c_dmf_v[:, h, 9:17, :], writes=[dmf.k])
    cx.dma("sp", g1b.t[:], g1.partition_broadcast(128), writes=[g1b.k])
    cx.dma("sp", gglab.t[:], g_gla.partition_broadcast(128), writes=[gglab.k])
    w_in_v = w_in.rearrange("(k p) f -> p k f", p=128)
    for k in range(8):
        for hh in range(2):
            c0 = hh * 1544
            cx.dma("pool", win.t[:, k, c0:c0 + 1544], w_in_v[:, k, c0:c0 + 1544], writes=[win.k])
    w_out_v = w_out.rearrange("(k p) f -> p k f", p=128)
    for k in range(8):
        cx.dma("pool", wout.t[:, k, :], w_out_v[:, k, :], writes=[wout.k])
    cx.dma("pool", wgk.t[0:16, :], w_gk2, writes=[wgk.k])
    cx.dma("pool", wgk.t[16:17, :], b_gk, writes=[wgk.k])
    for g_ in gklrT2:
        S.add("pool", lambda e, g_=g_: e.memset(g_.t[:], 1.0), (), [g_.k])
    S.add("pool", lambda e: e.memset(Vr.t[:], 1.0), (), [Vr.k])
    S.add("pool", lambda e: e.memset(kTr.t[:], 0.0), (), [kTr.k])

    kT_k = [Tok(f"kT{i}") for i in range(NR)]
    V_k = [Tok(f"V{i}") for i in range(NR)]
    psT = cx.ps([128, 1024], BF16, "psT")
    psR = [cx.ps([128, 512], F32, f"psR{i}") for i in range(2)]
    psSb = [cx.ps([128, 512], F32, f"psS{i}") for i in range(2)]
    psD = [cx.ps([128, 512], F32, f"psD{i}") for i in range(2)]
    psGO = cx.ps([128, 4, 128], F32, "psGO")
    rr = [0]

    def next_ps():
        b = psR[rr[0] % 2]
        rr[0] += 1
        return b

    def rot(n, shape, dt, name):
        return [cx.sb(shape, dt, f"{name}{i}") for i in range(n)]

    x_t = rot(2, [128, D], F32, "x")
    junk = cx.sb([128, D], BF16, "junk")
    st_t = rot(2, [128, 8], F32, "stat")
    n_t = rot(1, [128, D], BF16, "n") * 2
    nT_t = rot(2, [128, 8, 128], BF16, "nT")
    qkTa = rot(2, [128, 4, 128], BF16, "qkTa")
    qTb = rot(2, [128, 4, 2, 128], BF16, "qTb")
    va_t = rot(2, [128, 512], BF16, "va")
    ka_t = rot(2, [128, 256], F32, "ka")
    sr_t = rot(2, [128, 512], BF16, "sr")
    kbf = rot(1, [128, 512], F32, "kbf") * 2
    vbf = rot(1, [128, 512], F32, "vbf") * 2
    e1_t = rot(1, [128, 256], F32, "e1") * 2
    g_t = rot(2, [128, 256], F32, "g")
    ebT = rot(2, [128, 2, 128], F32, "ebT")
    enbT = rot(2, [128, 2, 128], F32, "enbT")
    ed_t = rot(2, [128, 256], F32, "ed")
    qtT = rot(2, [128, 2, 2, 128], BF16, "qtT")
    ktT = rot(2, [128, 2, 128], BF16, "ktT")
    khat = rot(2, [128, 2, 256], BF16, "khat")
    Am = rot(8, [128, 128], BF16, "Am")
    gst = rot(2, [128, 8], F32, "gst")
    gtmp = rot(2, [128, 128], F32, "gtmp")
    ocat = rot(2, [128, D], BF16, "ocat")
    pe_t = rot(2, [128, 512], BF16, "pe")
    pm_t = rot(2, [128, 512], BF16, "pm")
    rden = rot(4, [128, 1], F32, "rden")
    oT_t = rot(1, [128, 8, 128], BF16, "oT") * 2
    cnt = {"s": 0, "a": 0, "p": 0, "r": 0}
    for b_ in qTb + qtT + khat:
        S.add("pool", lambda e, b_=b_: e.memset(b_.t[:], 0.0), (), [b_.k])

    def rmsnorm_to_bf16(xb, gb, nb, stb):
        cx.act(junk.t[:], xb.t[:], AF.Square, [xb.k], [junk.k, stb.k], accum_out=stb.t[:, 0:1])
        cx.act(stb.t[:, 1:2], stb.t[:, 0:1], AF.Ln, [stb.k], [stb.k], scale=1.0 / D, bias=EPS)
        cx.act(stb.t[:, 2:3], stb.t[:, 1:2], AF.Exp, [stb.k], [stb.k], scale=-0.5)
        cx.stt("dve", nb.t[:], xb.t[:], stb.t[:, 2:3], gb.t[:], ALU.mult, ALU.mult, [xb.k, stb.k, gb.k], [nb.k])

    def transpose8(src, dst):
        for k in range(8):
            cx.tr(psT.t[:, k * 128:(k + 1) * 128], src.t[:, k * 128:(k + 1) * 128], ident_b.t[:], [src.k, ident_b.k], [psT.k])
        cx.copy("act", dst.t[:].rearrange("p k t -> p (k t)"), psT.t[:], [psT.k], [dst.k])

    def proj_fm(nT, col0, nchunk, ps, m=128):
        for j in range(nchunk):
            for k in range(8):
                cx.mm(ps.t[0:m, j * 128:(j + 1) * 128], win.t[:, k, col0 + j * 128: col0 + j * 128 + m], nT.t[:, k, :],
                      k == 0, k == 7, [win.k, nT.k], [ps.k])

    def proj_tm(nT, col0, ncol, ps):
        for k in range(8):
            cx.mm(ps.t[:, 0:ncol], nT.t[:, k, :], win.t[:, k, col0:col0 + ncol], k == 0, k == 7, [win.k, nT.k], [ps.k])

    sbi = [0]

    def tile_gen(s, qb):
        i2 = qb % 2
        gklrT = gklrT2[i2]
        xb, nb, nT, stb = x_t[i2], n_t[i2], nT_t[i2], st_t[i2]
        cx.dma("sp", xb.t[:], xp[s, qb * 128:(qb + 1) * 128, :], writes=[xb.k])
        rmsnorm_to_bf16(xb, g1b, nb, stb)
        transpose8(nb, nT)
        slot = qb % NR
        ps = next_ps()
        proj_fm(nT, C_QA, 4, ps)
        cx.copy("act", qkTa[i2].t[:].rearrange("p k t -> p (k t)"), ps.t[:], [ps.k], [qkTa[i2].k])
        ps = next_ps()
        proj_fm(nT, C_QB, 4, ps)
        psv = ps.t[:].rearrange("p (k t) -> p k t", k=4)
        cx.copy("act", qTb[i2].t[0:64, :, 0, :], psv[0:64], [ps.k], [qTb[i2].k])
        cx.copy("act", qTb[i2].t[64:128, :, 1, :], psv[64:128], [ps.k], [qTb[i2].k])
        ps = next_ps()
        proj_fm(nT, C_KB, 4, ps)
        cx.copy("dve", kTr.t[:, :, slot * 128:(slot + 1) * 128], ps.t[:].rearrange("p (k t) -> p k t", k=4), [ps.k, kTr.k], [kT_k[slot]])
        ps = next_ps()
        proj_fm(nT, C_GK, 1, ps, m=16)
        cx.copy("dve", gklrT.t[0:16, :], ps.t[0:16, 0:128], [ps.k], [gklrT.k])
        yield
        ps = next_ps()
        proj_tm(nT, C_VA, 512, ps)
        cx.copy("act", va_t[i2].t[:], ps.t[:], [ps.k], [va_t[i2].k])
        ps = next_ps()
        proj_tm(nT, C_KA, 256, ps)
        cx.copy("dve", ka_t[i2].t[:], ps.t[:, 0:256], [ps.k], [ka_t[i2].k])
        ps = next_ps()
        proj_tm(nT, C_RA, 512, ps)
        cx.act(sr_t[i2].t[:], ps.t[:], AF.Silu, [ps.k], [sr_t[i2].k])
        if qb >= KT0:
            ps = next_ps()
            proj_tm(nT, C_KB, 512, ps)
            cx.copy("act", kbf[i2].t[:], ps.t[:], [ps.k], [kbf[i2].k])
            cx.dma("sp", o_swak[s, (qb - KT0) * 128:(qb - KT0 + 1) * 128, :], kbf[i2].t[:], reads=[kbf[i2].k], out_dram=True)
        ps = next_ps()
        proj_tm(nT, C_VB, 512, ps)
        cx.copy("dve", Vr.t[:, slot, :, 0:64], ps.t[:].rearrange("p (h d) -> p h d", h=8), [ps.k, Vr.k], [V_k[slot]])
        if qb >= KT0:
            cx.copy("act", vbf[i2].t[:], ps.t[:], [ps.k], [vbf[i2].k])
            cx.dma("sp", o_swav[s, (qb - KT0) * 128:(qb - KT0 + 1) * 128, :], vbf[i2].t[:], reads=[vbf[i2].k], out_dram=True)

        yield
        if qb == 0:
            S.add("dve", lambda e: e.memset(Sst.t[:], 0.0), (), [Sst.k])
            sbi[0] = 0
            S.add("pool", lambda e: e.memset(Sb[0].t[:], 0.0), (), [Sb[0].k])
        ps = next_ps()
        cx.mm(ps.t[:, 0:256], gklrT.t[0:17, :], wgk.t[0:17, :], True, True, [gklrT.k, wgk.k], [ps.k])
        cx.act(e1_t[i2].t[:], ps.t[:, 0:256], AF.Exp, [ps.k], [e1_t[i2].k], scale=-1.0)
        cx.act(g_t[i2].t[:], e1_t[i2].t[:], AF.Ln, [e1_t[i2].k], [g_t[i2].k], bias=1.0, scale=1.0)
        gb_ = g_t[i2]
        ps = next_ps()
        for p in range(2):
            cx.mm(ps.t[:, p * 128:(p + 1) * 128], gb_.t[:, p * 128:(p + 1) * 128], ucum.t[:], True, True, [gb_.k, ucum.k], [ps.k])
        cx.mm(ps.t[:, 256:512], lrev.t[:], gb_.t[:], True, True, [gb_.k, lrev.k], [ps.k])
        cx.act(ebT[i2].t[:].rearrange("p k t -> p (k t)"), ps.t[:, 0:256], AF.Exp, [ps.k], [ebT[i2].k])
        cx.act(enbT[i2].t[:].rearrange("p k t -> p (k t)"), ps.t[:, 0:256], AF.Exp, [ps.k], [enbT[i2].k], scale=-1.0)
        cx.act(ed_t[i2].t[:], ps.t[:, 256:512], AF.Exp, [ps.k], [ed_t[i2].k])
        for hh in range(2):
            pr = slice(hh * 64, hh * 64 + 64)
            cx.stt("dve", qtT[i2].t[pr, :, hh, :], qkTa[i2].t[pr, 0:2, :], 0.125, ebT[i2].t[pr], ALU.mult, ALU.mult,
                   [qkTa[i2].k, ebT[i2].k], [qtT[i2].k])
        cx.tt("dve", ktT[i2].t[:], qkTa[i2].t[:, 2:4, :], enbT[i2].t[:], ALU.mult, [qkTa[i2].k, enbT[i2].k], [ktT[i2].k])
        for c in range(2):
            pr = slice(c * 64, c * 64 + 64)
            cx.tt("dve", khat[i2].t[pr, c, :], ka_t[i2].t[pr], ed_t[i2].t[pr], ALU.mult, [ka_t[i2].k, ed_t[i2].k], [khat[i2].k])
        yield
        ams = []
        for h in range(4):
            p, base = h // 2, (h % 2) * 64
            pss = psSb[cnt["s"] % 2]
            cnt["s"] += 1
            cx.mm(pss.t[:, 0:128], ktT[i2].t[:, p, :], qtT[i2].t[:, p, h % 2, :], True, True,
                  [ktT[i2].k, qtT[i2].k], [pss.k])
            am = Am[i2 * 4 + h]
            cx.tt("dve", am.t[:], pss.t[:, 0:128], amask.t[:], ALU.mult, [pss.k, amask.k], [am.k])
            ams.append(am)
        psU = next_ps()
        for c in range(2):
            for h in range(4):
                p, base = h // 2, (h % 2) * 64
                r0 = c * 64
                col = (c * 2 + p) * 128
                cx.mm(psU.t[base:base + 64, col:col + 128], khat[i2].t[:, c, h * 64:(h + 1) * 64],
                      va_t[i2].t[:, h * 128:(h + 1) * 128], True, True, [khat[i2].k, va_t[i2].k], [psU.k])
        sbs = [Sb[sbi[0] % 8]]
        for c in range(2):
            for p in range(2):
                col = (c * 2 + p) * 128
                cx.stt("dve", Sst.t[:, p, :], Sst.t[:, p, :], ebT[i2].t[:, p, c * 64 + 63:c * 64 + 64], psU.t[:, col:col + 128],
                       ALU.mult, ALU.add, [Sst.k, ebT[i2].k, psU.k], [Sst.k])
            sbi[0] += 1
            nsb = Sb[sbi[0] % 8]
            cx.copy("act", nsb.t[:], Sst.t[:], [Sst.k], [nsb.k])
            sbs.append(nsb)
        yield
        for h in range(4):
            p, base = h // 2, (h % 2) * 64
            cx.mm(psGO.t[:, h, :], ams[h].t[:], va_t[i2].t[:, h * 128:(h + 1) * 128], True, False, [ams[h].k, va_t[i2].k], [psGO.k])
            cx.mm(psGO.t[0:64, h, :], qtT[i2].t[:, p, h % 2, 0:64], sbs[0].t[:, p, :], False, True,
                  [qtT[i2].k, sbs[0].k], [psGO.k])
            cx.mm(psGO.t[64:128, h, :], qtT[i2].t[:, p, h % 2, 64:128], sbs[1].t[:, p, :], False, True,
                  [qtT[i2].k, sbs[1].k], [psGO.k])
        gs = gst[i2]
        for h in range(4):
            cx.act(junk.t[:, 0:128], psGO.t[:, h, :], AF.Square, [psGO.k], [junk.k, gs.k], accum_out=gs.t[:, h:h + 1])
        cx.act(gs.t[:, 4:8], gs.t[:, 0:4], AF.Ln, [gs.k], [gs.k], scale=1.0 / 128, bias=EPS)
        cx.act(gs.t[:, 0:4], gs.t[:, 4:8], AF.Exp, [gs.k], [gs.k], scale=-0.5)
        oc = ocat[i2]
        for h in range(4):
            gt = gtmp[h % 2]
            cx.stt("dve", gt.t[:], psGO.t[:, h, :], gs.t[:, h:h + 1], gglab.t[:], ALU.mult, ALU.mult, [psGO.k, gs.k, gglab.k], [gt.k])
            cx.tt("dve", oc.t[:, h * 128:(h + 1) * 128], gt.t[:], sr_t[i2].t[:, h * 128:(h + 1) * 128], ALU.mult,
                  [gt.k, sr_t[i2].k], [oc.k])
        if qb == NT - 1:
            cx.dma("sp", o_glas[s].rearrange("(p h) k v -> (h k) p v", h=2), Sst.t[:], reads=[Sst.k], out_dram=True)

        yield
        kbs = list(range(max(0, qb - 16), qb + 1))
        groups = [kbs[i:i + 4] for i in range(0, len(kbs), 4)]
        steps = [(h, gi) for h in range(8) for gi in range(len(groups))]
        stt_ = {}

        def emit_scores(i):
            h, gi = steps[i]
            grp = groups[gi]
            c4 = h // 2
            pss = psSb[cnt["s"] % 2]
            cnt["s"] += 1
            for j, kb in enumerate(grp):
                ks = kb % NR
                cx.mm(pss.t[:, j * 128:(j + 1) * 128], kTr.t[:, c4, ks * 128:(ks + 1) * 128], qTb[i2].t[:, c4, h % 2, :],
                      True, True, [kT_k[ks], qTb[i2].k], [pss.k])
            stt_[i] = pss

        def emit_rest(i):
            h, gi = steps[i]
            grp = groups[gi]
            ng = len(grp)
            pss = stt_.pop(i)
            pd = psD[h % 2]
            pe_ = pe_t[cnt["p"] % 2]
            pm_ = pm_t[cnt["p"] % 2]
            cnt["p"] += 1
            cx.act(pe_.t[:, 0:ng * 128], pss.t[:, 0:ng * 128], AF.Exp, [pss.k], [pe_.k], scale=0.125)
            r0 = 16 - (qb - grp[0])
            cx.tt("dve", pm_.t[:, 0:ng * 128], pe_.t[:, 0:ng * 128], dmf.t[:, h, r0:r0 + ng, :].rearrange("p r q -> p (r q)"), ALU.mult,
                  [pe_.k, dmf.k], [pm_.k])
            for j, kb in enumerate(grp):
                ks = kb % NR
                first = gi == 0 and j == 0
                last = gi == len(groups) - 1 and j == ng - 1
                cx.mm(pd.t[:, 0:65], pm_.t[:, j * 128:(j + 1) * 128], Vr.t[:, ks, h, :], first, last, [pm_.k, V_k[ks]], [pd.k])
            if gi == len(groups) - 1:
                rd = rden[cnt["r"] % 4]
                cnt["r"] += 1
                S.add("dve", lambda e, rd=rd, pd=pd: e.reciprocal(out=rd.t[:], in_=pd.t[:, 64:65]), [pd.k], [rd.k])
                cx.ts("dve", oc.t[:, 512 + h * 64:512 + (h + 1) * 64], pd.t[:, 0:64], rd.t[:], None, ALU.mult, None,
                      [pd.k, rd.k], [oc.k])

        ng_ = len(groups)
        for h in range(8):
            emit_scores(h * ng_)
            for gi in range(ng_):
                i = h * ng_ + gi
                if gi + 1 < ng_:
                    emit_scores(i + 1)
                emit_rest(i)
            yield

        transpose8(oc, oT_t[i2])
        hb = xb
        for half in range(2):
            ps = next_ps()
            for k in range(8):
                cx.mm(ps.t[:], oT_t[i2].t[:, k, :], wout.t[:, k, half * 512:(half + 1) * 512], k == 0, k == 7,
                      [oT_t[i2].k, wout.k], [ps.k])
            cx.tt("dve", hb.t[:, half * 512:(half + 1) * 512], ps.t[:], xb.t[:, half * 512:(half + 1) * 512], ALU.add,
                  [ps.k, xb.k], [hb.k])
        cx.dma("sp", h1_d[s, qb * 128:(qb + 1) * 128, :], hb.t[:], reads=[hb.k], out_dram=True)
        yield

    interleave((tile_gen(s_, qb_) for s_ in range(nseq) for qb_ in range(NT)), width=2)
    cx.close()


def host_consts_b():
    t = np.arange(128)
    return {
        "c_tri": (t[:, None] < t[None, :]).astype(np.float32),
        "c_iota32": np.broadcast_to(np.arange(32, dtype=np.float32), (128, 32)).copy(),
        "c_iota4": np.broadcast_to(np.arange(4, dtype=np.float32), (128, 4)).copy(),
    }


def phase_b(nc, din, dout, NT, nseq, h1_d, c_ident, debug, h1s_d=None):
    T = NT * 128
    NTOT = nseq * NT + (1 if h1s_d is not None else 0)
    if h1s_d is not None:
        cmk = din("cmk", [NSS, 256, D])
        cmv = din("cmv", [NSS, 256, D])
        c_sel = nc_input(nc, din, "c_sel", [8, 2048])
    memp = din("memp", [nseq, 256, D])
    g2 = din("g_norm2", [1, D])
    gm = din("g_mem", [1, D])
    g3 = din("g_norm3", [1, D])
    w_cq = din("w_cq", [D, D])
    w_mk = din("w_mk", [D, D])
    w_mv = din("w_mv", [D, D])
    w_co = din("w_co", [D, D])
    w_r = din("w_r", [D, 36])
    b_r = din("b_r", [1, 36])
    c_tri = din("c_tri", [128, 128])
    c_iota32 = nc_input(nc, din, "c_iota32", [128, 32])
    c_iota4 = din("c_iota4", [128, 4])
    o_memk = dout("o_memk", [nseq, 256, D])
    o_memv = dout("o_memv", [nseq, 256, D])
    mk_out = dout if debug else (lambda n, sh, dt=F32: nc.dram_tensor(n, list(sh), dt).ap())
    h2_d = mk_out("h2", [NTOT * 128, D])
    xn_d = mk_out("xn", [NTOT * 128, D], BF16)
    route_d = mk_out("route", [128, NTOT, 8])
    cnt_d = mk_out("cnt", [128, 32])

    cx = Ctx(nc)
    S = cx.S
    ident_b = cx.sb([128, 128], BF16, "b_ident_b")
    ident_f = cx.sb([128, 128], F32, "b_ident_f")
    tri_b = cx.sb([128, 128], BF16, "b_tri")
    ones_b = cx.sb([128, 128], BF16, "b_ones")
    iota32 = cx.sb([128, 32], F32, "b_iota32")
    iota4 = cx.sb([128, 4], F32, "b_iota4")
    g2b = cx.sb([128, D], F32, "g2b")
    g3b = cx.sb([128, D], F32, "g3b")
    gmb = cx.sb([128, D], F32, "gmb")
    brb = cx.sb([128, 36], F32, "brb")
    wcq = cx.sb([128, 8, D], BF16, "wcq")
    wco = cx.sb([128, 8, D], BF16, "wco")
    wmk = cx.sb([128, 8, D], BF16, "wmk")
    wmv = cx.sb([128, 8, D], BF16, "wmv")
    wr = cx.sb([128, 8, 36], F32, "wr")
    mT = cx.sb([128, 8, 256], BF16, "mT")
    mkT = cx.sb([128, 8, 256], BF16, "mkT")
    mva = cx.sb([128, 2, 4, 257], BF16, "mva")
    route = cx.sb([128, NTOT, 8], F32, "route_sb")
    cntb = cx.sb([128, 32], F32, "cntb")

    if h1s_d is not None:
        sel = cx.sb([8, 16, 128], BF16, "b_sel")
        cx.dma("pool", sel.t[:].rearrange("p c t -> p (c t)"), c_sel, writes=[sel.k])
        stgk = [cx.sb([128, 2, D], BF16, f"b_stgk{i}") for i in range(2)]
        mkTs = [cx.sb([128, 8, 256], BF16, f"b_mkTs{i}") for i in range(2)]
        mvas = [cx.sb([128, 2, 4, 257], BF16, f"b_mvas{i}") for i in range(2)]
        PTs = [cx.sb([128, 8, 8], BF16, f"b_PTs{i}") for i in range(2)]
        ocs = [cx.sb([8, D], BF16, f"b_ocs{i}") for i in range(2)]
        rdens = [cx.sb([8, 1], F32, f"b_rdens{i}") for i in range(2)]
        for v_ in mvas:
            S.add("pool", lambda e, v_=v_: e.memset(v_.t[:], 1.0), (), [v_.k])
    cx.dma("pool", ident_b.t[:], c_ident, writes=[ident_b.k])
    cx.dma("sp", ident_f.t[:], c_ident, writes=[ident_f.k])
    cx.dma("pool", tri_b.t[:], c_tri, writes=[tri_b.k])
    cx.dma("sp", iota32.t[:], c_iota32, writes=[iota32.k])
    cx.dma("sp", iota4.t[:], c_iota4, writes=[iota4.k])
    cx.dma("sp", g2b.t[:], g2.partition_broadcast(128), writes=[g2b.k])
    cx.dma("sp", g3b.t[:], g3.partition_broadcast(128), writes=[g3b.k])
    cx.dma("sp", gmb.t[:], gm.partition_broadcast(128), writes=[gmb.k])
    cx.dma("sp", brb.t[:], b_r.partition_broadcast(128), writes=[brb.k])
    cx.dma("sp", wr.t[:], w_r.rearrange("(k p) f -> p k f", p=128), writes=[wr.k])
    for wsb, wd in ((wmk, w_mk), (wmv, w_mv), (wcq, w_cq), (wco, w_co)):
        v = wd.rearrange("(k p) f -> p k f", p=128)
        for k in range(8):
            cx.dma("pool", wsb.t[:, k, :], v[:, k, :], writes=[wsb.k])
    S.add("pool", lambda e: e.memset(ones_b.t[:], 1.0), (), [ones_b.k])
    S.add("pool", lambda e: e.memset(mva.t[:], 1.0), (), [mva.k])
    S.add("dve", lambda e: e.memset(cntb.t[:], 0.0), (), [cntb.k])
    S.add("dve", lambda e: e.memset(route.t[:], 0.0), (), [route.k])

    psT = cx.ps([128, 1024], BF16, "b_psT")
    psF = [cx.ps([128, 512], F32, f"b_psF{i}") for i in range(2)]
    psR = [cx.ps([128, 512], F32, f"b_psR{i}") for i in range(2)]
    psD = [cx.ps([128, 512], F32, f"b_psD{i}") for i in range(2)]
    psX = cx.ps([128, 512], F32, "b_psX")
    rr = [0]

    def next_ps():
        b = psR[rr[0] % 2]
        rr[0] += 1
        return b

    def rot(n, shape, dt, name):
        return [cx.sb(shape, dt, f"b_{name}{i}") for i in range(n)]

    h_t = rot(2, [128, D], F32, "h")
    junk = cx.sb([128, D], BF16, "b_junk")
    st_t = rot(2, [128, 8], F32, "stat")
    n_t = rot(2, [128, D], BF16, "n")
    nT_t = rot(2, [128, 8, 128], BF16, "nT")
    qT_t = rot(2, [128, 8, 128], BF16, "qT")
    PT_t = rot(2, [128, 8, 128], BF16, "PT")
    oc_t = rot(2, [128, D], BF16, "oc")
    oT_t = rot(2, [128, 8, 128], BF16, "oT")
    rden = rot(4, [128, 1], F32, "rden")
    mo_t = rot(2, [128, 512], F32, "mo")
    xf_t = rot(2, [128, D], F32, "xf")
    xb_t = rot(2, [128, D], BF16, "xb")
    xfT = cx.sb([128, 8, 128], F32, "b_xfT")
    rt_t = rot(2, [128, 64], F32, "rt")
    mx8 = rot(2, [128, 8], F32, "mx8")
    ix8 = rot(2, [128, 8], U32, "ix8")
    ixf = rot(2, [128, 8], F32, "ixf")
    O0_t = rot(2, [128, 32], F32, "O0")
    O1_t = rot(2, [128, 32], F32, "O1")
    Ob_t = rot(2, [128, 32], BF16, "Ob")
    rk_t = rot(2, [128, 32], F32, "rk")
    t32 = rot(2, [128, 32], F32, "t32")
    cnt = {"r": 0}

    def rmsnorm(xb, gb, out, stb, eng="dve"):
        cx.act(junk.t[:], xb.t[:], AF.Square, [xb.k], [junk.k, stb.k], accum_out=stb.t[:, 0:1])
        cx.act(stb.t[:, 1:2], stb.t[:, 0:1], AF.Ln, [stb.k], [stb.k], scale=1.0 / D, bias=EPS)
        cx.act(stb.t[:, 2:3], stb.t[:, 1:2], AF.Exp, [stb.k], [stb.k], scale=-0.5)
        cx.stt(eng, out.t[:], xb.t[:], stb.t[:, 2:3], gb.t[:], ALU.mult, ALU.mult, [xb.k, stb.k, gb.k], [out.k])

    def transpose8(src, dst_ap, dst_k):
        for k in range(8):
            cx.tr(psT.t[:, k * 128:(k + 1) * 128], src.t[:, k * 128:(k + 1) * 128], ident_b.t[:], [src.k, ident_b.k], [psT.k])
        cx.copy("act", dst_ap, psT.t[:].rearrange("p (k t) -> p k t", k=8), [psT.k], [dst_k])

    gt = 0
    units = [(s_, False) for s_ in range(nseq)] + ([(0, True)] if h1s_d is not None else [])
    for s, is_samp in units:
      if not is_samp:
          for mt in range(2):
              hb, nb, stb = h_t[mt], n_t[mt], st_t[mt]
              cx.dma("sp", hb.t[:], memp[s, mt * 128:(mt + 1) * 128, :], writes=[hb.k])
              rmsnorm(hb, gmb, nb, stb)
              transpose8(nb, mT.t[:, :, mt * 128:(mt + 1) * 128], mT.k)
          for mt in range(2):
              for wsb, od, isv in ((wmk, o_memk, False), (wmv, o_memv, True)):
                  for half in range(2):
                      ps = next_ps()
                      for k in range(8):
                          cx.mm(ps.t[:], mT.t[:, k, mt * 128:(mt + 1) * 128], wsb.t[:, k, half * 512:(half + 1) * 512], k == 0, k == 7,
                                [mT.k, wsb.k], [ps.k])
                      mo = mo_t[rr[0] % 2]
                      cx.copy("act", mo.t[:], ps.t[:], [ps.k], [mo.k])
                      if isv:
                          cx.copy("dve", mva.t[:, mt, 2 * half:2 * half + 2, 0:256], ps.t[:].rearrange("p (h d) -> p h d", h=2), [ps.k], [mva.k])
                      cx.dma("sp", od[s, mt * 128:(mt + 1) * 128, half * 512:(half + 1) * 512], mo.t[:], reads=[mo.k], out_dram=True)
          for c2 in range(4):
              ps = next_ps()
              for j in range(2):
                  c8 = c2 * 2 + j
                  for k in range(8):
                      cx.mm(ps.t[:, j * 256:(j + 1) * 256], wmk.t[:, k, c8 * 128:(c8 + 1) * 128], mT.t[:, k, :], k == 0, k == 7,
                            [wmk.k, mT.k], [ps.k])
              cx.copy("act", mkT.t[:, c2 * 2:c2 * 2 + 2, :], ps.t[:].rearrange("p (j m) -> p j m", j=2), [ps.k], [mkT.k])

      def tile_b(s, is_samp, qb, gt):
            i2 = gt % 2
            hb, nb, nT, stb = h_t[i2], n_t[i2], nT_t[i2], st_t[i2]
            cx.dma("sp", hb.t[:], h1s_d if is_samp else h1_d[s, qb * 128:(qb + 1) * 128, :], writes=[hb.k])
            rmsnorm(hb, g2b, nb, stb)
            transpose8(nb, nT.t[:], nT.k)
            qT = qT_t[i2]
            for half in range(2):
                ps = next_ps()
                for j in range(4):
                    c8 = half * 4 + j
                    for k in range(8):
                        cx.mm(ps.t[:, j * 128:(j + 1) * 128], wcq.t[:, k, c8 * 128:(c8 + 1) * 128], nT.t[:, k, :], k == 0, k == 7,
                              [wcq.k, nT.k], [ps.k])
                cx.copy("act", qT.t[:, half * 4:half * 4 + 4, :], ps.t[:].rearrange("p (j t) -> p j t", j=4), [ps.k], [qT.k])
            yield
            if is_samp:
                oc = oc_t[i2]
                for c in range(NSS):
                    sk, mkT_, mva_, PT_, oc_, rd_ = stgk[c % 2], mkTs[c % 2], mvas[c % 2], PTs[c % 2], ocs[c % 2], rdens[c % 2]
                    cx.dma("pool", sk.t[:], cmk[c].rearrange("(m p) f -> p m f", p=128), writes=[sk.k])
                    for mb in range(2):
                        cx.dma("pool", mva_.t[:, mb, :, 0:256], cmv[c, mb * 128:(mb + 1) * 128, :].rearrange("p (h d) -> p h d", h=4), writes=[mva_.k])
                    for mb in range(2):
                        for c8 in range(8):
                            cx.tr(psT.t[:, c8 * 128:(c8 + 1) * 128], sk.t[:, mb, c8 * 128:(c8 + 1) * 128], ident_b.t[:], [sk.k, ident_b.k], [psT.k])
                        cx.copy("act", mkT_.t[:, :, mb * 128:(mb + 1) * 128], psT.t[:].rearrange("p (k t) -> p k t", k=8), [psT.k], [mkT_.k])
                    ps = next_ps()
                    for h in range(4):
                        for mb in range(2):
                            idx = h * 2 + mb
                            for j in range(2):
                                cx.mm(ps.t[:, idx * 8:(idx + 1) * 8], mkT_.t[:, 2 * h + j, mb * 128:(mb + 1) * 128], qT.t[:, 2 * h + j, 8 * c:8 * c + 8],
                                      j == 0, j == 1, [mkT_.k, qT.k], [ps.k])
                    cx.act(PT_.t[:].rearrange("p a b -> p (a b)"), ps.t[:, 0:64], AF.Exp, [ps.k], [PT_.k], scale=1.0 / 16)
                    for h in range(4):
                        pd = psD[h % 2]
                        for mb in range(2):
                            cx.mm(pd.t[0:8, 0:257], PT_.t[:, h * 2 + mb, :], mva_.t[:, mb, h, :], mb == 0, mb == 1, [PT_.k, mva_.k], [pd.k])
                        S.add("dve", lambda e, rd_=rd_, pd=pd: e.reciprocal(out=rd_.t[:], in_=pd.t[0:8, 256:257]), [pd.k], [rd_.k])
                        cx.ts("dve", oc_.t[:, h * 256:(h + 1) * 256], pd.t[0:8, 0:256], rd_.t[:], None, ALU.mult, None, [pd.k, rd_.k], [oc_.k])
                    for half in range(2):
                        cx.mm(psF[half].t[:], sel.t[:, c, :], oc_.t[:, half * 512:(half + 1) * 512], c == 0, c == NSS - 1, [sel.k, oc_.k], [psF[half].k])
                for half in range(2):
                    cx.copy("act", oc.t[:, half * 512:(half + 1) * 512], psF[half].t[:], [psF[half].k], [oc.k])
            else:
                PT = PT_t[i2]
                for hp in range(2):
                    ps = next_ps()
                    for hh in range(2):
                        h = hp * 2 + hh
                        for mb in range(2):
                            idx = hh * 2 + mb
                            for j in range(2):
                                cx.mm(ps.t[:, idx * 128:(idx + 1) * 128], mkT.t[:, 2 * h + j, mb * 128:(mb + 1) * 128], qT.t[:, 2 * h + j, :],
                                      j == 0, j == 1, [mkT.k, qT.k], [ps.k])
                    cx.act(PT.t[:, hp * 4:hp * 4 + 4, :], ps.t[:].rearrange("p (j t) -> p j t", j=4), AF.Exp, [ps.k], [PT.k], scale=1.0 / 16)
                oc = oc_t[i2]
                for h in range(4):
                    pd = psD[h % 2]
                    for mb in range(2):
                        cx.mm(pd.t[:, 0:257], PT.t[:, h * 2 + mb, :], mva.t[:, mb, h, :], mb == 0, mb == 1, [PT.k, mva.k], [pd.k])
                    rd = rden[cnt["r"] % 4]
                    cnt["r"] += 1
                    S.add("dve", lambda e, rd=rd, pd=pd: e.reciprocal(out=rd.t[:], in_=pd.t[:, 256:257]), [pd.k], [rd.k])
                    cx.ts("dve", oc.t[:, h * 256:(h + 1) * 256], pd.t[:, 0:256], rd.t[:], None, ALU.mult, None, [pd.k, rd.k], [oc.k])
            yield
            oT = oT_t[i2]
            transpose8(oc, oT.t[:], oT.k)
            for half in range(2):
                ps = next_ps()
                for k in range(8):
                    cx.mm(ps.t[:], oT.t[:, k, :], wco.t[:, k, half * 512:(half + 1) * 512], k == 0, k == 7, [oT.k, wco.k], [ps.k])
                cx.tt("dve", hb.t[:, half * 512:(half + 1) * 512], ps.t[:], hb.t[:, half * 512:(half + 1) * 512], ALU.add, [ps.k, hb.k], [hb.k])
            cx.dma("sp", h2_d[gt * 128:(gt + 1) * 128, :], hb.t[:], reads=[hb.k], out_dram=True)
            xf, xb = xf_t[i2], xb_t[i2]
            rmsnorm(hb, g3b, xf, stb)
            cx.copy("act", xb.t[:], xf.t[:], [xf.k], [xb.k])
            cx.dma("sp", xn_d[gt * 128:(gt + 1) * 128, :], xb.t[:], reads=[xb.k], out_dram=True)
            yield
            for half in range(2):
                for j in range(4):
                    k = half * 4 + j
                    cx.tr(psF[half].t[:, j * 128:(j + 1) * 128], xf.t[:, k * 128:(k + 1) * 128], ident_f.t[:], [xf.k, ident_f.k], [psF[half].k])
                cx.copy("act", xfT.t[:, half * 4:half * 4 + 4, :], psF[half].t[:].rearrange("p (j t) -> p j t", j=4), [psF[half].k], [xfT.k])
            for k in range(8):
                cx.mm(psX.t[:, 0:36], xfT.t[:, k, :], wr.t[:, k, :], k == 0, k == 7, [xfT.k, wr.k], [psX.k])
            rt = rt_t[i2]
            R_ = [rt.k]
            c = lambda a, b=None: rt.t[:, a:(a + 1 if b is None else b)]
            cx.tt("dve", c(0, 36), psX.t[:, 0:36], brb.t[:], ALU.add, [psX.k, brb.k], R_)
            S.add("dve", lambda e, rt=rt: e.tensor_reduce(out=rt.t[:, 36:37], in_=rt.t[:, 0:4], axis=AX.X, op=ALU.max), R_, R_)
            cx.ts("dve", c(37), c(36), -1.0, None, ALU.mult, None, R_, R_)
            cx.act(c(45, 49), c(0, 4), AF.Exp, R_, R_, bias=c(37), scale=1.0, accum_out=c(38))
            S.add("dve", lambda e, rt=rt: e.reciprocal(out=rt.t[:, 39:40], in_=rt.t[:, 38:39]), R_, R_)
            cx.ts("dve", c(40, 44), c(0, 4), c(36), None, ALU.is_equal, None, R_, R_)
            cx.tt("dve", c(45, 49), c(40, 44), iota4.t[:], ALU.mult, R_ + [iota4.k], R_)
            S.add("dve", lambda e, rt=rt: e.tensor_reduce(out=rt.t[:, 44:45], in_=rt.t[:, 45:49], axis=AX.X, op=ALU.add), R_, R_)
            cx.ts("dve", c(49, 57), c(4, 12), c(40), None, ALU.mult, None, R_, R_)
            for g in range(1, 4):
                cx.stt("dve", c(49, 57), c(4 + 8 * g, 12 + 8 * g), c(40 + g), c(49, 57), ALU.mult, ALU.add, R_, R_)
            m8, i8, i8f = mx8[i2], ix8[i2], ixf[i2]
            S.add("dve", lambda e, rt=rt, m8=m8: e.max(out=m8.t[:], in_=rt.t[:, 49:57]), R_, [m8.k])
            S.add("dve", lambda e, rt=rt, m8=m8, i8=i8: e.max_index(out=i8.t[:], in_max=m8.t[:], in_values=rt.t[:, 49:57]), R_ + [m8.k], [i8.k])
            cx.copy("dve", i8f.t[:], i8.t[:], [i8.k], [i8f.k])
            cx.tt("dve", c(57), m8.t[:, 1:2], m8.t[:, 0:1], ALU.subtract, [m8.k], R_)
            cx.act(c(58), c(57), AF.Exp, R_, R_)
            cx.ts("dve", c(59), c(58), 1.0, None, ALU.add, None, R_, R_)
            S.add("dve", lambda e, rt=rt: e.reciprocal(out=rt.t[:, 59:60], in_=rt.t[:, 59:60]), R_, R_)
            cx.tt("dve", c(60), c(59), c(39), ALU.mult, R_, R_)
            cx.tt("dve", c(61), c(60), c(58), ALU.mult, R_, R_)
            cx.stt("dve", c(62), c(44), 8.0, i8f.t[:, 0:1], ALU.mult, ALU.add, R_ + [i8f.k], R_)
            cx.stt("dve", c(63), c(44), 8.0, i8f.t[:, 1:2], ALU.mult, ALU.add, R_ + [i8f.k], R_)
            O0, O1, Ob, rk, tt32 = O0_t[i2], O1_t[i2], Ob_t[i2], rk_t[i2], t32[i2]
            cx.ts("dve", O0.t[:], iota32.t[:], c(62), None, ALU.is_equal, None, R_ + [iota32.k], [O0.k])
            cx.ts("dve", O1.t[:], iota32.t[:], c(63), None, ALU.is_equal, None, R_ + [iota32.k], [O1.k])
            cx.tt("dve", Ob.t[:], O0.t[:], O1.t[:], ALU.add, [O0.k, O1.k], [Ob.k])
            cx.mm(psX.t[:, 64:96], tri_b.t[:], Ob.t[:], True, True, [tri_b.k, Ob.k], [psX.k])
            cx.mm(psX.t[:, 96:128], ones_b.t[:], Ob.t[:], True, True, [ones_b.k, Ob.k], [psX.k])
            cx.tt("dve", rk.t[:], psX.t[:, 64:96], cntb.t[:], ALU.add, [psX.k, cntb.k], [rk.k])
            cx.tt("dve", cntb.t[:], psX.t[:, 96:128], cntb.t[:], ALU.add, [psX.k, cntb.k], [cntb.k])
            cx.tt("dve", tt32.t[:], O0.t[:], rk.t[:], ALU.mult, [O0.k, rk.k], [tt32.k])
            S.add("dve", lambda e, tt32=tt32, gt=gt: e.tensor_reduce(out=route.t[:, gt, 2:3], in_=tt32.t[:], axis=AX.X, op=ALU.add), [tt32.k], [route.k])
            cx.tt("dve", tt32.t[:], O1.t[:], rk.t[:], ALU.mult, [O1.k, rk.k], [tt32.k])
            S.add("dve", lambda e, tt32=tt32, gt=gt: e.tensor_reduce(out=route.t[:, gt, 3:4], in_=tt32.t[:], axis=AX.X, op=ALU.add), [tt32.k], [route.k])
            cx.copy("dve", route.t[:, gt, 0:2], c(62, 64), R_, [route.k])
            cx.copy("dve", route.t[:, gt, 4:6], c(60, 62), R_, [route.k])
            yield

      ntl = 1 if is_samp else NT
      interleave((tile_b(s, is_samp, qb_, gt + qb_) for qb_ in range(ntl)), width=2)
      gt += ntl
    cx.dma("sp", route_d, route.t[:], reads=[route.k], out_dram=True)
    cx.dma("sp", cnt_d, cntb.t[:], reads=[cntb.k], out_dram=True)
    cx.close()
    return h2_d, xn_d, route_d, cnt_d


BLK = 256
_BREGS = {}


def _breg(e, val):
    key = (id(e), val)
    if key not in _BREGS:
        _BREGS[key] = e.to_reg(val)
    return _BREGS[key]


def host_consts_c():
    p = np.arange(128, dtype=np.float32)
    return {
        "c_thr": (p * BLK).reshape(128, 1).astype(np.float32),
        "c_kp": (np.arange(8, dtype=np.float32)[None, :] * 128 + p[:, None]).astype(np.float32),
    }


def phase_c(nc, din, dout, NTOT, h2_d, xn_d, route_d, cnt_d, c_ident, y_d):
    NTOK = NTOT * 128
    NB = -(-2 * NTOK // BLK) + 32
    assert NB <= 128
    R = NB * BLK
    w_e1 = din("w_e1", [32 * D, 512])
    w_e3 = din("w_e3", [32 * D, 512])
    w_e2 = din("w_e2", [32 * 512, D])
    gf = din("g_final", [1, D])
    c_iota32 = nc_input(nc, din, "c_iota32", [128, 32])
    c_thr = din("c_thr", [128, 1])
    c_kp = din("c_kp", [128, 8])
    Xs = nc.dram_tensor("moe_xs", [R, D], BF16).ap()
    Ys = nc.dram_tensor("moe_ys", [R, D], F32).ap()
    Xs_k, Ys_k = Tok("Xs"), Tok("Ys")
    xs_parts, ys_parts = [], []

    cx = Ctx(nc)
    S = cx.S
    ident_b = cx.sb([128, 128], BF16, "c_ident_b")
    ident_f = cx.sb([128, 128], F32, "c_ident_f")
    ones_f = cx.sb([128, 128], F32, "c_ones_f")
    iota32 = cx.sb([128, 32], F32, "c_iota32s")
    thr = cx.sb([128, 1], F32, "c_thrs")
    kp = cx.sb([128, 8], F32, "c_kps")
    gfb = cx.sb([128, D], F32, "gfb")
    route = cx.sb([128, NTOT, 8], F32, "c_route")
    cntb = cx.sb([128, 32], F32, "c_cnt")
    sm = cx.sb([128, 8, 32], F32, "c_sm")
    z32 = cx.sb([128, 32], F32, "c_z32")
    becol = cx.sb([128, 2], F32, "c_becol")
    dgb = cx.sb([128, 128], F32, "c_dgb")
    bebc = cx.sb([128, 128], F32, "c_bebc")
    idf = cx.sb([128, NB, 12], F32, "c_idf")
    idi = cx.sb([128, NB, 12], I32, "c_idi")
    dstf = cx.sb([128, NTOT, 2], F32, "c_dstf")
    dsti = cx.sb([128, NTOT, 2], I32, "c_dsti")
    psT = cx.ps([128, 1024], BF16, "c_psT")
    psH1 = [cx.ps([128, 512], F32, f"c_psH1{i}") for i in range(2)]
    psH3 = [cx.ps([128, 512], F32, f"c_psH3{i}") for i in range(2)]
    psY = [cx.ps([128, 512], F32, f"c_psY{i}") for i in range(2)]
    psX = cx.ps([128, 512], F32, "c_psX")

    cx.dma("pool", ident_b.t[:], c_ident, writes=[ident_b.k])
    cx.dma("sp", ident_f.t[:], c_ident, writes=[ident_f.k])
    cx.dma("sp", iota32.t[:], c_iota32, writes=[iota32.k])
    cx.dma("sp", thr.t[:], c_thr, writes=[thr.k])
    cx.dma("sp", kp.t[:], c_kp, writes=[kp.k])
    cx.dma("sp", gfb.t[:], gf.partition_broadcast(128), writes=[gfb.k])
    cx.dma("sp", route.t[:], route_d, writes=[route.k])
    cx.dma("sp", cntb.t[:], cnt_d, writes=[cntb.k])
    S.add("dve", lambda e: e.memset(ones_f.t[:], 1.0), (), [ones_f.k])
    S.add("dve", lambda e: e.memset(z32.t[:], 0.0), (), [z32.k])
    K_ = [sm.k]
    padded, pend, pstart, cmp_ = sm.t[:, 0, :], sm.t[:, 1, :], sm.t[:, 2, :], sm.t[:, 3, :]
    tmpa, tmpb = sm.t[:, 4, :], sm.t[:, 5, :]
    cx.ts("dve", tmpa, cntb.t[:], 0.0, None, ALU.is_gt, None, [cntb.k], K_)
    for j in range(1, NTOK // BLK + 1):
        cx.stt("dve", tmpa, cntb.t[:], float(BLK * j), tmpa, ALU.is_gt, ALU.add, [cntb.k] + K_, K_)
    cx.ts("dve", padded, tmpa, float(BLK), None, ALU.mult, None, K_, K_)
    S.add("dve", lambda e: e.tensor_tensor_scan(out=pend, data0=padded, data1=z32.t[:], initial=0.0, op0=ALU.add, op1=ALU.add), K_ + [z32.k], K_)
    cx.tt("dve", pstart, pend, padded, ALU.subtract, K_, K_)
    cx.ts("dve", cmp_, pend, thr.t[:], None, ALU.is_le, None, K_ + [thr.k], K_)
    S.add("dve", lambda e: e.tensor_reduce(out=becol.t[:, 0:1], in_=cmp_, axis=AX.X, op=ALU.add), K_, [becol.k])
    cx.ts("dve", becol.t[:, 1:2], becol.t[:, 0:1], 31.0, None, ALU.min, None, [becol.k], [becol.k])
    cx.ts("dve", dgb.t[:], ident_f.t[:], becol.t[:, 1:2], None, ALU.mult, None, [ident_f.k, becol.k], [dgb.k])
    cx.mm(psX.t[:, 0:128], ones_f.t[:], dgb.t[:], True, True, [ones_f.k, dgb.k], [psX.k])
    cx.copy("dve", bebc.t[:], psX.t[:, 0:128], [psX.k], [bebc.k])
    for k in range(8):
        cx.ts("dve", idf.t[:, :, k], bebc.t[:, 0:NB], 1024.0, kp.t[:, k:k + 1], ALU.mult, ALU.add, [bebc.k, kp.k], [idf.k])
    for k in range(4):
        cx.ts("dve", idf.t[:, :, 8 + k], bebc.t[:, 0:NB], 512.0, kp.t[:, k:k + 1], ALU.mult, ALU.add, [bebc.k, kp.k], [idf.k])
    same = cx.sb([128, 128], F32, "c_same")
    S.add("dve", lambda e: e.memset(same.t[:], 0.0), (), [same.k])
    cx.tt("dve", same.t[:, 2:NB], bebc.t[:, 2:NB], bebc.t[:, 0:NB - 2], ALU.is_equal, [bebc.k, same.k], [same.k])
    for k in range(12):
        cx.stt("dve", idf.t[:, :, k], same.t[:, 0:NB], 1.0e6, idf.t[:, :, k], ALU.mult, ALU.add, [same.k, idf.k], [idf.k])
    cx.copy("dve", idi.t[:], idf.t[:], [idf.k], [idi.k])

    def rot(n, shape, dt, name):
        return [cx.sb(shape, dt, f"c_{name}{i}") for i in range(n)]

    zt = cx.sb([128, 8, D], BF16, "c_zero")
    S.add("pool", lambda e: e.memset(zt.t[:], 0.0), (), [zt.k])
    r0 = 0
    while r0 < R:
        nr = min(1024, R - r0)
        zk = Tok("xz")
        cx.dma("sp", Xs[r0:r0 + nr, :].rearrange("(s p) f -> p s f", p=128), zt.t[:, 0:nr // 128, :], reads=[zt.k], writes=[zk], out_dram=True)
        xs_parts.append(zk)
        r0 += nr
    S.add("pool", None, xs_parts, [Xs_k])
    xs_parts = []
    xn_t = rot(3, [128, D], BF16, "xn")
    o_t = rot(2, [128, 32], F32, "o32")
    for gt in range(NTOT):
        xb = xn_t[gt % 3]
        cx.dma("sp", xb.t[:], xn_d[gt * 128:(gt + 1) * 128, :], writes=[xb.k])
        for sl in range(2):
            o32 = o_t[sl]
            cx.ts("dve", o32.t[:], iota32.t[:], route.t[:, gt, sl:sl + 1], None, ALU.is_equal, None, [iota32.k, route.k], [o32.k])
            cx.tt("dve", o32.t[:], o32.t[:], pstart, ALU.mult, [o32.k] + K_, [o32.k])
            S.add("dve", lambda e, o32=o32, gt=gt, sl=sl: e.tensor_reduce(out=dstf.t[:, gt, sl:sl + 1], in_=o32.t[:], axis=AX.X, op=ALU.add),
                  [o32.k], [dstf.k])
        cx.tt("dve", dstf.t[:, gt, :], dstf.t[:, gt, :], route.t[:, gt, 2:4], ALU.add, [dstf.k, route.k], [dstf.k])
        cx.copy("dve", dsti.t[:, gt, :], dstf.t[:, gt, :], [dstf.k], [dsti.k])
        for sl in range(2):
            S.add("pool", lambda e, xb=xb, gt=gt, sl=sl: e.indirect_dma_start(
                out=Xs, out_offset=bass.IndirectOffsetOnAxis(ap=dsti.t[:, gt, sl:sl + 1], axis=0), in_=xb.t[:, :], in_offset=None),
                [xb.k, dsti.k, Xs_k], [xs_parts.append(Tok("xsc")) or xs_parts[-1]], dma=True, out=True)

    S.add("sp", None, xs_parts, [Xs_k])
    xs_t = rot(2, [128, 2, D], BF16, "xs")
    xsT_t = rot(2, [128, 8, 256], BF16, "xsT")
    w1_t = rot(2, [128, 8, 512], BF16, "w1")
    w3_t = rot(2, [128, 8, 512], BF16, "w3")
    w2_t = rot(2, [128, 4, D], BF16, "w2")
    sg_t = rot(2, [128, 512], F32, "sg")
    hT_t = rot(2, [128, 4, 256], BF16, "hT")
    yo_t = rot(2, [128, D], F32, "yo")
    for b in range(NB):
        i2 = b % 2
        xs, xsT, w1, w3, w2, hT = xs_t[i2], xsT_t[i2], w1_t[i2], w3_t[i2], w2_t[i2], hT_t[i2]
        for k in range(8):
            S.add("pool", lambda e, w1=w1, k=k, b=b: e.indirect_dma_start(
                out=w1.t[:, k, :], out_offset=None, in_=w_e1, in_offset=bass.IndirectOffsetOnAxis(ap=idi.t[:, b, k:k + 1], axis=0),
                bounds_check=_breg(e, 32 * D - 1), oob_is_err=False),
                [idi.k], [w1.k], dma=True)
            S.add("pool", lambda e, w3=w3, k=k, b=b: e.indirect_dma_start(
                out=w3.t[:, k, :], out_offset=None, in_=w_e3, in_offset=bass.IndirectOffsetOnAxis(ap=idi.t[:, b, k:k + 1], axis=0),
                bounds_check=_breg(e, 32 * D - 1), oob_is_err=False),
                [idi.k], [w3.k], dma=True)
        for k in range(4):
            S.add("pool", lambda e, w2=w2, k=k, b=b: e.indirect_dma_start(
                out=w2.t[:, k, :], out_offset=None, in_=w_e2, in_offset=bass.IndirectOffsetOnAxis(ap=idi.t[:, b, 8 + k:9 + k], axis=0),
                bounds_check=_breg(e, 32 * 512 - 1), oob_is_err=False),
                [idi.k], [w2.k], dma=True)
        cx.dma("sp", xs.t[:], Xs[b * BLK:(b + 1) * BLK, :].rearrange("(s p) f -> p s f", p=128), reads=[Xs_k], writes=[xs.k])
        for sub in range(2):
            for k in range(8):
                cx.tr(psT.t[:, k * 128:(k + 1) * 128], xs.t[:, sub, k * 128:(k + 1) * 128], ident_b.t[:], [xs.k, ident_b.k], [psT.k])
            cx.copy("act", xsT.t[:, :, sub * 128:(sub + 1) * 128], psT.t[:].rearrange("p (k t) -> p k t", k=8), [psT.k], [xsT.k])
        for pr in range(2):
            for wt, pst in ((w1, psH1[pr]), (w3, psH3[pr])):
                for j in range(2):
                    fc = pr * 2 + j
                    for k in range(8):
                        cx.mm(pst.t[:, j * 256:(j + 1) * 256], wt.t[:, k, fc * 128:(fc + 1) * 128], xsT.t[:, k, :], k == 0, k == 7,
                              [wt.k, xsT.k], [pst.k])
            sg = sg_t[pr]
            cx.act(sg.t[:], psH1[pr].t[:], AF.Silu, [psH1[pr].k], [sg.k])
            cx.tt("dve", hT.t[:, pr * 2:pr * 2 + 2, :], sg.t[:].rearrange("p (j t) -> p j t", j=2),
                  psH3[pr].t[:].rearrange("p (j t) -> p j t", j=2), ALU.mult, [sg.k, psH3[pr].k], [hT.k])
        for sub in range(2):
            yo = yo_t[sub]
            for half in range(2):
                py = psY[half]
                for fk in range(4):
                    cx.mm(py.t[:], hT.t[:, fk, sub * 128:(sub + 1) * 128], w2.t[:, fk, half * 512:(half + 1) * 512], fk == 0, fk == 3,
                          [hT.k, w2.k], [py.k])
                cx.copy("act" if half else "dve", yo.t[:, half * 512:(half + 1) * 512], py.t[:], [py.k], [yo.k])
            cx.dma("sp", Ys[b * BLK + sub * 128:b * BLK + (sub + 1) * 128, :], yo.t[:], reads=[yo.k], writes=[ys_parts.append(Tok("ysp")) or ys_parts[-1]], out_dram=True)

    S.add("pool", None, ys_parts, [Ys_k])
    h_t = rot(2, [128, D], F32, "h")
    y0_t = rot(2, [128, D], F32, "y0")
    y1_t = rot(2, [128, D], F32, "y1")
    st_t = rot(2, [128, 8], F32, "stat")
    junk = cx.sb([128, D], BF16, "c_junk")
    for gt in range(NTOT):
        i2 = gt % 2
        hb, y0, y1, stb = h_t[i2], y0_t[i2], y1_t[i2], st_t[i2]
        cx.dma("sp", hb.t[:], h2_d[gt * 128:(gt + 1) * 128, :], writes=[hb.k])
        for sl, yy in ((0, y0), (1, y1)):
            S.add("pool", lambda e, yy=yy, gt=gt, sl=sl: e.indirect_dma_start(
                out=yy.t[:, :], out_offset=None, in_=Ys, in_offset=bass.IndirectOffsetOnAxis(ap=dsti.t[:, gt, sl:sl + 1], axis=0)),
                [Ys_k, dsti.k], [yy.k], dma=True)
        cx.stt("dve", hb.t[:], y0.t[:], route.t[:, gt, 4:5], hb.t[:], ALU.mult, ALU.add, [y0.k, route.k, hb.k], [hb.k])
        cx.stt("dve", hb.t[:], y1.t[:], route.t[:, gt, 5:6], hb.t[:], ALU.mult, ALU.add, [y1.k, route.k, hb.k], [hb.k])
        cx.act(junk.t[:], hb.t[:], AF.Square, [hb.k], [junk.k, stb.k], accum_out=stb.t[:, 0:1])
        cx.act(stb.t[:, 1:2], stb.t[:, 0:1], AF.Ln, [stb.k], [stb.k], scale=1.0 / D, bias=EPS)
        cx.act(stb.t[:, 2:3], stb.t[:, 1:2], AF.Exp, [stb.k], [stb.k], scale=-0.5)
        cx.stt("dve", y0.t[:], hb.t[:], stb.t[:, 2:3], gfb.t[:], ALU.mult, ALU.mult, [hb.k, stb.k, gfb.k], [y0.k])
        cx.dma("sp", y_d[gt * 128:(gt + 1) * 128, :], y0.t[:], reads=[y0.k], out_dram=True)
    cx.close()


_DIN_CACHE = {}


def nc_input(nc, din, name, shape):
    key = (id(nc), name)
    if key not in _DIN_CACHE:
        _DIN_CACHE[key] = din(name, shape)
    return _DIN_CACHE[key]


def host_consts_s():
    t = np.arange(128)
    ch = t // 8
    same = ch[:, None] == ch[None, :]
    ucum = np.where(same & (t[:, None] <= t[None, :]), -1.0 / 16, 0.0).astype(np.float32)
    lrev = np.where(same & (t[:, None] > t[None, :]), -1.0 / 16, 0.0).astype(np.float32)
    amask = np.where(same & (t[:, None] <= t[None, :]), 1.0, 0.0).astype(np.float32)
    rowmask = (ch[:, None] == np.arange(16)[None, :]).astype(np.float32)
    slopes = np.array([2.0 ** (-8.0 * (h + 1) / 8) for h in range(8)], np.float64)

    def mfun(dist):
        m = ((dist >= 0) & (dist <= 128)).astype(np.float64)
        m += ((dist >= 0) & (dist <= 512) & (dist % 4 == 0))
        m += ((dist >= 0) & (dist <= 2048) & (dist % 16 == 0))
        return m
    ki = t[:, None, None, None]
    kb = np.arange(16)[None, None, :, None]
    j = np.arange(8)[None, None, None, :]
    dist = 2048 + j - (kb * 128 + ki)
    ms = mfun(dist) * np.exp(-slopes[None, :, None, None] * dist)
    c = np.arange(16)[None, None, :, None]
    jp = ki - 8 * c
    dist2 = j - jp
    mn = np.where((jp >= 0) & (jp < 8), mfun(dist2) * np.exp(-slopes[None, :, None, None] * np.maximum(dist2, 0)), 0.0)
    sel = np.zeros((8, 16, 128), np.float32)
    for cc in range(16):
        for jj in range(8):
            sel[jj, cc, 8 * cc + jj] = 1.0
    return {"c_ucum8": ucum, "c_lrev8": lrev, "c_amask8": amask, "c_rowmask": rowmask,
            "c_ms": ms.astype(np.float32).reshape(128, 8 * 16 * 8), "c_mn": mn.astype(np.float32).reshape(128, 8 * 16 * 8),
            "c_sel": sel.reshape(8, 16 * 128)}


def phase_as(nc, din, dout, h1s_d, c_ident):
    xs = din("xs", [128, D])
    ck = din("ck", [NSS, 2048, 512])
    cv = din("cv", [NSS, 2048, 512])
    sg = din("sg", [NSS, 4, 64, 128])
    g1 = nc_input(nc, din, "g_norm1", [1, D])
    w_in = nc_input(nc, din, "w_in", [D, DIN])
    w_gk2 = nc_input(nc, din, "w_gk2", [16, 256])
    b_gk = nc_input(nc, din, "b_gk", [1, 256])
    g_gla = nc_input(nc, din, "g_gla_out", [1, 128])
    w_out = nc_input(nc, din, "w_out", [D, D])
    c_ucum = din("c_ucum8", [128, 128])
    c_lrev = din("c_lrev8", [128, 128])
    c_amask = din("c_amask8", [128, 128])
    c_rowmask = din("c_rowmask", [128, 16])
    c_ms = din("c_ms", [128, 1024])
    c_mn = din("c_mn", [128, 1024])
    c_sel = nc_input(nc, din, "c_sel", [8, 2048])
    o_swak = dout("o_swak_s", [128, 512])
    o_swav = dout("o_swav_s", [128, 512])
    o_glas = dout("o_glas_s", [NSS, 4, 64, 128])

    cx = Ctx(nc)
    S = cx.S
    ident_b = cx.sb([128, 128], BF16, "s_ident_b")
    ident_f = cx.sb([128, 128], F32, "s_ident_f")
    ucum = cx.sb([128, 128], F32, "s_ucum")
    lrev = cx.sb([128, 128], F32, "s_lrev")
    amask = cx.sb([128, 128], F32, "s_amask")
    rowmask = cx.sb([128, 16], F32, "s_rowmask")
    ms = cx.sb([128, 8, 128], BF16, "s_ms")
    mn = cx.sb([128, 8, 16, 8], BF16, "s_mn")
    sel = cx.sb([8, 16, 128], BF16, "s_sel")
    g1b = cx.sb([128, D], F32, "s_g1b")
    gglab = cx.sb([128, 128], F32, "s_gglab")
    win = cx.sb([128, 8, DIN], BF16, "s_win")
    wout = cx.sb([128, 8, D], BF16, "s_wout")
    wgk = cx.sb([32, 256], BF16, "s_wgk")
    gklrT = cx.sb([32, 128], BF16, "s_gklrT")
    S0f = cx.sb([128, NSS, 2, 128], F32, "s_S0f")
    S0b = cx.sb([128, NSS, 2, 128], BF16, "s_S0b")
    stg = cx.sb([128, 16, 512], BF16, "s_stg")
    kcT = [cx.sb([128, 4, 2048], BF16, f"s_kcT{i}") for i in range(1)]
    vca = [cx.sb([128, 16, 8, 65], BF16, f"s_vca{i}") for i in range(1)]
    Vs = cx.sb([128, 8, 65], BF16, "s_Vs")

    cx.dma("pool", ident_b.t[:], c_ident, writes=[ident_b.k])
    cx.dma("sp", ident_f.t[:], c_ident, writes=[ident_f.k])
    cx.dma("sp", ucum.t[:], c_ucum, writes=[ucum.k])
    cx.dma("sp", lrev.t[:], c_lrev, writes=[lrev.k])
    cx.dma("sp", amask.t[:], c_amask, writes=[amask.k])
    cx.dma("sp", rowmask.t[:], c_rowmask, writes=[rowmask.k])
    cx.dma("pool", ms.t[:].rearrange("p h x -> p (h x)"), c_ms, writes=[ms.k])
    cx.dma("pool", mn.t[:].rearrange("p h c j -> p (h c j)"), c_mn, writes=[mn.k])
    cx.dma("pool", sel.t[:].rearrange("p c t -> p (c t)"), c_sel, writes=[sel.k])
    cx.dma("sp", g1b.t[:], g1.partition_broadcast(128), writes=[g1b.k])
    cx.dma("sp", gglab.t[:], g_gla.partition_broadcast(128), writes=[gglab.k])
    w_in_v = w_in.rearrange("(k p) f -> p k f", p=128)
    for k in range(8):
        for hh in range(2):
            c0 = hh * 1544
            cx.dma("pool", win.t[:, k, c0:c0 + 1544], w_in_v[:, k, c0:c0 + 1544], writes=[win.k])
    w_out_v = w_out.rearrange("(k p) f -> p k f", p=128)
    for k in range(8):
        cx.dma("pool", wout.t[:, k, :], w_out_v[:, k, :], writes=[wout.k])
    cx.dma("pool", wgk.t[0:16, :], w_gk2, writes=[wgk.k])
    cx.dma("pool", wgk.t[16:17, :], b_gk, writes=[wgk.k])
    S.add("pool", lambda e: e.memset(gklrT.t[:], 1.0), (), [gklrT.k])
    S.add("pool", lambda e: e.memset(Vs.t[:], 1.0), (), [Vs.k])
    for v_ in vca:
        S.add("pool", lambda e, v_=v_: e.memset(v_.t[:], 1.0), (), [v_.k])
    cx.dma("sp", S0f.t[:], sg.rearrange("c (p h) k v -> (h k) c p v", h=2), writes=[S0f.k])
    cx.copy("act", S0b.t[:], S0f.t[:], [S0f.k], [S0b.k])

    psT = cx.ps([128, 1024], BF16, "s_psT")
    psR = [cx.ps([128, 512], F32, f"s_psR{i}") for i in range(2)]
    psSb = [cx.ps([128, 512], F32, f"s_psS{i}") for i in range(2)]
    psD = [cx.ps([128, 512], F32, f"s_psD{i}") for i in range(2)]
    psSel = cx.ps([128, 512], F32, "s_psSel")
    rr = [0]

    def next_ps():
        b = psR[rr[0] % 2]
        rr[0] += 1
        return b

    xb = cx.sb([128, D], F32, "s_x")
    junk = cx.sb([128, D], BF16, "s_junk")
    stb = cx.sb([128, 8], F32, "s_stat")
    nb = cx.sb([128, D], BF16, "s_n")
    nT = cx.sb([128, 8, 128], BF16, "s_nT")
    qkTa = cx.sb([128, 4, 128], BF16, "s_qkTa")
    qTb = cx.sb([128, 4, 2, 128], BF16, "s_qTb")
    kTs = cx.sb([128, 4, 128], BF16, "s_kTs")
    va = cx.sb([128, 512], BF16, "s_va")
    ka = cx.sb([128, 256], F32, "s_ka")
    sr = cx.sb([128, 512], F32, "s_sr")
    kbf = cx.sb([128, 512], F32, "s_kbf")
    vbf = cx.sb([128, 512], F32, "s_vbf")
    e1 = cx.sb([128, 256], F32, "s_e1")
    gg = cx.sb([128, 256], F32, "s_g")
    ebT = cx.sb([128, 2, 128], F32, "s_ebT")
    enbT = cx.sb([128, 2, 128], F32, "s_enbT")
    ed = cx.sb([128, 256], F32, "s_ed")
    qtT = cx.sb([128, 2, 2, 128], BF16, "s_qtT")
    ktT = cx.sb([128, 2, 128], BF16, "s_ktT")
    khat = cx.sb([128, 256], BF16, "s_khat")
    khm = [cx.sb([128, 256], BF16, f"s_khm{i}") for i in range(2)]
    Am = [cx.sb([128, 128], BF16, f"s_Am{i}") for i in range(4)]
    oTf = cx.sb([128, 4, 128], F32, "s_oTf")
    gs = cx.sb([128, 8], F32, "s_gs")
    gtmp = [cx.sb([128, 128], F32, f"s_gtmp{i}") for i in range(2)]
    oc = cx.sb([128, D], BF16, "s_ocat")
    Sn = [cx.sb([128, 2, 128], F32, f"s_Sn{i}") for i in range(2)]
    pe_t = [cx.sb([128, 136], BF16, f"s_pe{i}") for i in range(2)]
    pm_t = [cx.sb([128, 136], BF16, f"s_pm{i}") for i in range(2)]
    rden = [cx.sb([8, 1], F32, f"s_rden{i}") for i in range(2)]
    occ = [cx.sb([8, 512], BF16, f"s_occ{i}") for i in range(2)]
    oT = cx.sb([128, 8, 128], BF16, "s_oT")
    for b_ in (qTb, qtT):
        S.add("pool", lambda e, b_=b_: e.memset(b_.t[:], 0.0), (), [b_.k])

    def transpose8(src, dst):
        for k in range(8):
            cx.tr(psT.t[:, k * 128:(k + 1) * 128], src.t[:, k * 128:(k + 1) * 128], ident_b.t[:], [src.k, ident_b.k], [psT.k])
        cx.copy("act", dst.t[:].rearrange("p k t -> p (k t)"), psT.t[:], [psT.k], [dst.k])

    def proj_fm(col0, nchunk, ps, m=128):
        for j in range(nchunk):
            for k in range(8):
                cx.mm(ps.t[0:m, j * 128:(j + 1) * 128], win.t[:, k, col0 + j * 128: col0 + j * 128 + m], nT.t[:, k, :],
                      k == 0, k == 7, [win.k, nT.k], [ps.k])

    def proj_tm(col0, ncol, ps):
        for k in range(8):
            cx.mm(ps.t[:, 0:ncol], nT.t[:, k, :], win.t[:, k, col0:col0 + ncol], k == 0, k == 7, [win.k, nT.k], [ps.k])

    cx.dma("sp", xb.t[:], xs, writes=[xb.k])
    cx.act(junk.t[:], xb.t[:], AF.Square, [xb.k], [junk.k, stb.k], accum_out=stb.t[:, 0:1])
    cx.act(stb.t[:, 1:2], stb.t[:, 0:1], AF.Ln, [stb.k], [stb.k], scale=1.0 / D, bias=EPS)
    cx.act(stb.t[:, 2:3], stb.t[:, 1:2], AF.Exp, [stb.k], [stb.k], scale=-0.5)
    cx.stt("dve", nb.t[:], xb.t[:], stb.t[:, 2:3], g1b.t[:], ALU.mult, ALU.mult, [xb.k, stb.k, g1b.k], [nb.k])
    transpose8(nb, nT)
    ps = next_ps()
    proj_fm(C_QA, 4, ps)
    cx.copy("act", qkTa.t[:].rearrange("p k t -> p (k t)"), ps.t[:], [ps.k], [qkTa.k])
    ps = next_ps()
    proj_fm(C_QB, 4, ps)
    psv = ps.t[:].rearrange("p (k t) -> p k t", k=4)
    cx.copy("act", qTb.t[0:64, :, 0, :], psv[0:64], [ps.k], [qTb.k])
    cx.copy("act", qTb.t[64:128, :, 1, :], psv[64:128], [ps.k], [qTb.k])
    ps = next_ps()
    proj_fm(C_KB, 4, ps)
    cx.copy("dve", kTs.t[:].rearrange("p k t -> p (k t)"), ps.t[:], [ps.k], [kTs.k])
    ps = next_ps()
    proj_fm(C_GK, 1, ps, m=16)
    cx.copy("dve", gklrT.t[0:16, :], ps.t[0:16, 0:128], [ps.k], [gklrT.k])
    ps = next_ps()
    proj_tm(C_VA, 512, ps)
    cx.copy("act", va.t[:], ps.t[:], [ps.k], [va.k])
    ps = next_ps()
    proj_tm(C_KA, 256, ps)
    cx.copy("dve", ka.t[:], ps.t[:, 0:256], [ps.k], [ka.k])
    ps = next_ps()
    proj_tm(C_RA, 512, ps)
    cx.act(sr.t[:], ps.t[:], AF.Silu, [ps.k], [sr.k])
    ps = next_ps()
    proj_tm(C_KB, 512, ps)
    cx.copy("act", kbf.t[:], ps.t[:], [ps.k], [kbf.k])
    cx.dma("sp", o_swak, kbf.t[:], reads=[kbf.k], out_dram=True)
    ps = next_ps()
    proj_tm(C_VB, 512, ps)
    cx.copy("dve", Vs.t[:, :, 0:64], ps.t[:].rearrange("p (h d) -> p h d", h=8), [ps.k], [Vs.k])
    cx.copy("act", vbf.t[:], ps.t[:], [ps.k], [vbf.k])
    cx.dma("sp", o_swav, vbf.t[:], reads=[vbf.k], out_dram=True)

    ps = next_ps()
    cx.mm(ps.t[:, 0:256], gklrT.t[0:17, :], wgk.t[0:17, :], True, True, [gklrT.k, wgk.k], [ps.k])
    cx.act(e1.t[:], ps.t[:, 0:256], AF.Exp, [ps.k], [e1.k], scale=-1.0)
    cx.act(gg.t[:], e1.t[:], AF.Ln, [e1.k], [gg.k], bias=1.0, scale=1.0)
    ps = next_ps()
    for p in range(2):
        cx.mm(ps.t[:, p * 128:(p + 1) * 128], gg.t[:, p * 128:(p + 1) * 128], ucum.t[:], True, True, [gg.k, ucum.k], [ps.k])
    cx.mm(ps.t[:, 256:512], lrev.t[:], gg.t[:], True, True, [gg.k, lrev.k], [ps.k])
    cx.act(ebT.t[:].rearrange("p k t -> p (k t)"), ps.t[:, 0:256], AF.Exp, [ps.k], [ebT.k])
    cx.act(enbT.t[:].rearrange("p k t -> p (k t)"), ps.t[:, 0:256], AF.Exp, [ps.k], [enbT.k], scale=-1.0)
    cx.act(ed.t[:], ps.t[:, 256:512], AF.Exp, [ps.k], [ed.k])
    for hh in range(2):
        pr = slice(hh * 64, hh * 64 + 64)
        cx.stt("dve", qtT.t[pr, :, hh, :], qkTa.t[pr, 0:2, :], 0.125, ebT.t[pr], ALU.mult, ALU.mult, [qkTa.k, ebT.k], [qtT.k])
    cx.tt("dve", ktT.t[:], qkTa.t[:, 2:4, :], enbT.t[:], ALU.mult, [qkTa.k, enbT.k], [ktT.k])
    cx.tt("dve", khat.t[:], ka.t[:], ed.t[:], ALU.mult, [ka.k, ed.k], [khat.k])
    for h in range(4):
        p = h // 2
        pss = psSb[h % 2]
        cx.mm(pss.t[:, 0:128], ktT.t[:, p, :], qtT.t[:, p, h % 2, :], True, True, [ktT.k, qtT.k], [pss.k])
        cx.tt("dve", Am[h].t[:], pss.t[:, 0:128], amask.t[:], ALU.mult, [pss.k, amask.k], [Am[h].k])
    psO = psD[0]
    for h in range(4):
        p = h // 2
        cx.mm(psO.t[:, h * 128:(h + 1) * 128], va.t[:, h * 128:(h + 1) * 128], Am[h].t[:], True, False, [va.k, Am[h].k], [psO.k])
        for c in range(NSS):
            cx.mm(psO.t[:, h * 128 + 8 * c:h * 128 + 8 * c + 8], S0b.t[:, c, p, :], qtT.t[:, p, h % 2, 8 * c:8 * c + 8], False, c == NSS - 1,
                  [S0b.k, qtT.k], [psO.k])
    cx.copy("act", oTf.t[:].rearrange("p h t -> p (h t)"), psO.t[:], [psO.k], [oTf.k])
    psGO = psD[1]
    for h in range(4):
        cx.tr(psGO.t[:, h * 128:(h + 1) * 128], oTf.t[:, h, :], ident_f.t[:], [oTf.k, ident_f.k], [psGO.k])
    for h in range(4):
        cx.act(junk.t[:, 0:128], psGO.t[:, h * 128:(h + 1) * 128], AF.Square, [psGO.k], [junk.k, gs.k], accum_out=gs.t[:, h:h + 1])
    cx.act(gs.t[:, 4:8], gs.t[:, 0:4], AF.Ln, [gs.k], [gs.k], scale=1.0 / 128, bias=EPS)
    cx.act(gs.t[:, 0:4], gs.t[:, 4:8], AF.Exp, [gs.k], [gs.k], scale=-0.5)
    for h in range(4):
        gt_ = gtmp[h % 2]
        cx.stt("dve", gt_.t[:], psGO.t[:, h * 128:(h + 1) * 128], gs.t[:, h:h + 1], gglab.t[:], ALU.mult, ALU.mult, [psGO.k, gs.k, gglab.k], [gt_.k])
        cx.tt("dve", oc.t[:, h * 128:(h + 1) * 128], gt_.t[:], sr.t[:, h * 128:(h + 1) * 128], ALU.mult, [gt_.k, sr.k], [oc.k])
    for c in range(NSS):
        km = khm[c % 2]
        cx.ts("dve", km.t[:], khat.t[:], rowmask.t[:, c:c + 1], None, ALU.mult, None, [khat.k, rowmask.k], [km.k])
        psU = next_ps()
        for h in range(4):
            p, base = h // 2, (h % 2) * 64
            cx.mm(psU.t[base:base + 64, p * 128:(p + 1) * 128], km.t[:, h * 64:(h + 1) * 64], va.t[:, h * 128:(h + 1) * 128], True, True,
                  [km.k, va.k], [psU.k])
        sn = Sn[c % 2]
        for p in range(2):
            cx.stt("dve", sn.t[:, p, :], S0f.t[:, c, p, :], ebT.t[:, p, 8 * c + 7:8 * c + 8], psU.t[:, p * 128:(p + 1) * 128],
                   ALU.mult, ALU.add, [S0f.k, ebT.k, psU.k], [sn.k])
        cx.dma("sp", o_glas[c].rearrange("(p h) k v -> (h k) p v", h=2), sn.t[:], reads=[sn.k], out_dram=True)

    it = 0
    for c in range(NSS):
        kT_, va_ = kcT[0], vca[0]
        cx.dma("pool", stg.t[:], ck[c].rearrange("(b p) f -> p b f", p=128), writes=[stg.k])
        for kb in range(16):
            for c4 in range(4):
                cx.tr(psT.t[:, c4 * 128:(c4 + 1) * 128], stg.t[:, kb, c4 * 128:(c4 + 1) * 128], ident_b.t[:], [stg.k, ident_b.k], [psT.k])
            cx.copy("act" if kb % 2 else "dve", kT_.t[:, :, kb * 128:(kb + 1) * 128], psT.t[:, 0:512].rearrange("p (k t) -> p k t", k=4), [psT.k], [kT_.k])
        cx.dma("pool", stg.t[:], cv[c].rearrange("(b p) f -> p b f", p=128), writes=[stg.k])
        cx.copy("act", va_.t[:, :, :, 0:64], stg.t[:].rearrange("p b (h d) -> p b h d", h=8), [stg.k], [va_.k])
        ocb = occ[c % 2]
        for h in range(8):
            c4, par = h // 2, h % 2
            pss, pd = psSb[it % 2], psD[it % 2]
            pe_, pm_, rd = pe_t[it % 2], pm_t[it % 2], rden[it % 2]
            it += 1
            rhs = qTb.t[:, c4, par, 8 * c:8 * c + 8]
            for kb in range(16):
                cx.mm(pss.t[:, kb * 8:(kb + 1) * 8], kT_.t[:, c4, kb * 128:(kb + 1) * 128], rhs, True, True, [kT_.k, qTb.k], [pss.k])
            cx.mm(pss.t[:, 128:136], kTs.t[:, c4, :], rhs, True, True, [kTs.k, qTb.k], [pss.k])
            cx.act(pe_.t[:], pss.t[:, 0:136], AF.Exp, [pss.k], [pe_.k], scale=0.125)
            cx.tt("dve", pm_.t[:, 0:128], pe_.t[:, 0:128], ms.t[:, h, :], ALU.mult, [pe_.k, ms.k], [pm_.k])
            cx.tt("dve", pm_.t[:, 128:136], pe_.t[:, 128:136], mn.t[:, h, c, :], ALU.mult, [pe_.k, mn.k], [pm_.k])
            for kb in range(16):
                cx.mm(pd.t[0:8, 0:65], pm_.t[:, kb * 8:(kb + 1) * 8], va_.t[:, kb, h, :], kb == 0, False, [pm_.k, va_.k], [pd.k])
            cx.mm(pd.t[0:8, 0:65], pm_.t[:, 128:136], Vs.t[:, h, :], False, True, [pm_.k, Vs.k], [pd.k])
            S.add("dve", lambda e, rd=rd, pd=pd: e.reciprocal(out=rd.t[:], in_=pd.t[0:8, 64:65]), [pd.k], [rd.k])
            cx.ts("dve", ocb.t[:, h * 64:(h + 1) * 64], pd.t[0:8, 0:64], rd.t[:], None, ALU.mult, None, [pd.k, rd.k], [ocb.k])
        cx.mm(psSel.t[:], sel.t[:, c, :], ocb.t[:], c == 0, c == NSS - 1, [sel.k, ocb.k], [psSel.k])
    cx.copy("act", oc.t[:, 512:1024], psSel.t[:], [psSel.k], [oc.k])

    transpose8(oc, oT)
    for half in range(2):
        ps = next_ps()
        for k in range(8):
            cx.mm(ps.t[:], oT.t[:, k, :], wout.t[:, k, half * 512:(half + 1) * 512], k == 0, k == 7, [oT.k, wout.k], [ps.k])
        cx.tt("dve", xb.t[:, half * 512:(half + 1) * 512], ps.t[:], xb.t[:, half * 512:(half + 1) * 512], ALU.add, [ps.k, xb.k], [xb.k])
    cx.dma("sp", h1s_d, xb.t[:], reads=[xb.k], out_dram=True)
    cx.close()


_NC_CACHE = {}


def kernel(**inputs):
    n = 8
    if "nc" not in _NC_CACHE:
        _NC_CACHE["nc"] = build(NT=NT_FULL, nseq=NSEQ, debug=False, phases="ASBC")
    nc = _NC_CACHE["nc"]
    f32 = lambda a: np.ascontiguousarray(np.asarray(a, dtype=np.float32))
    I = {k: np.asarray(v) for k, v in inputs.items()}
    shared = {
        "g_norm1": f32(I["g_norm1"]), "w_in": f32(I["w_in"][0]), "w_gk2": f32(I["w_gk2"][0]),
        "b_gk": f32(I["b_gk"]), "g_gla_out": f32(I["g_gla_out"]), "w_out": f32(I["w_out"][0]),
        "g_norm2": f32(I["g_norm2"]), "g_mem": f32(I["g_mem"]), "g_norm3": f32(I["g_norm3"]),
        "w_cq": f32(I["w_cq"][0]), "w_mk": f32(I["w_mk"][0]), "w_mv": f32(I["w_mv"][0]), "w_co": f32(I["w_co"][0]),
        "w_r": f32(np.concatenate([I["w_gr"][0], I["w_er"][0]], axis=1)),
        "b_r": f32(np.concatenate([I["b_gr"], I["b_er"]], axis=1)),
        "w_e1": f32(I["w_e1"][0]).reshape(32 * D, 512), "w_e3": f32(I["w_e3"][0]).reshape(32 * D, 512),
        "w_e2": f32(I["w_e2"][0]).reshape(32 * 512, D), "g_final": f32(I["g_final"]).reshape(1, D),
    }
    shared.update(host_consts())
    shared.update(host_consts_b())
    shared.update(host_consts_c())
    shared.update(host_consts_s())
    in_maps = []
    for i in range(n):
        m = dict(shared)
        ps, ss = slice(NSEQ * i, NSEQ * (i + 1)), slice(NSS * i, NSS * (i + 1))
        m["xp"] = f32(I["x_prompt"][ps])
        m["memp"] = f32(I["mem_prompt"][ps])
        m["xs"] = f32(I["x_sample"][ss]).reshape(128, D)
        m["ck"] = f32(I["cache_swa_k"][0, ss]).reshape(NSS, 2048, 512)
        m["cv"] = f32(I["cache_swa_v"][0, ss]).reshape(NSS, 2048, 512)
        m["sg"] = f32(I["state_gla"][0, ss])
        m["cmk"] = f32(I["cache_mem_k"][0, ss]).reshape(NSS, 256, D)
        m["cmv"] = f32(I["cache_mem_v"][0, ss]).reshape(NSS, 256, D)
        in_maps.append(m)
    res = run_bass_kernel_spmd(nc, in_maps, core_ids=list(range(n)))
    rs = res.results
    npr = NSEQ * NT_FULL * 128
    cat = lambda key: np.concatenate([np.asarray(r[key], dtype=np.float32) for r in rs], axis=0)
    y_prompt = np.concatenate([np.asarray(r["y"][:npr], dtype=np.float32).reshape(NSEQ, NT_FULL * 128, D) for r in rs], axis=0)
    y_sample = np.concatenate([np.asarray(r["y"][npr:npr + 128], dtype=np.float32).reshape(NSS, 8, D) for r in rs], axis=0)
    return (y_prompt, y_sample,
            cat("o_swak").reshape(1, 16, 2048, 8, 64), cat("o_swav").reshape(1, 16, 2048, 8, 64),
            cat("o_glas").reshape(1, 16, 4, 64, 128),
            cat("o_memk").reshape(1, 16, 256, 4, 256), cat("o_memv").reshape(1, 16, 256, 4, 256),
            cat("o_swak_s").reshape(1, 128, 8, 8, 64), cat("o_swav_s").reshape(1, 128, 8, 8, 64),
            cat("o_glas_s").reshape(1, 128, 4, 64, 128))
```
